# Optimizing a Trainium2 kernel written in Bass

```python
import math
import jax, jax.numpy as jnp
from jax import lax
import numpy as np

D_MODEL = 1024
BATCH = 16
SEQ = 2048
DEPTH = 2

CTX_LEN = 256
GRID_W = 64
NORM_EPS = 1e-6

GDN_HEADS = 8
GDN_HEAD_DIM = 128
GDN_WIDTH = GDN_HEADS * GDN_HEAD_DIM
GDN_CHUNK = 64
QKV_CONV = 5
CMLP_GROUPS = 4
CMLP_CHUNK = 128
CMLP_WIDTH = 512
CMLP_GROUP_DIM = CMLP_WIDTH // CMLP_GROUPS
POOL_WINDOWS = (2, 4, 8, 16)
POOL_GROUPS = 4
POOL_WIDTH = 512
POOL_GROUP_DIM = POOL_WIDTH // POOL_GROUPS
N_BRANCH = 3
IN_SIZES = (GDN_WIDTH, GDN_WIDTH, GDN_WIDTH, GDN_WIDTH, 2 * GDN_HEADS, 2 * GDN_HEADS,
            CMLP_WIDTH, CMLP_WIDTH, POOL_WIDTH, N_BRANCH * D_MODEL)
IN_COLS = sum(IN_SIZES)
N_EXPERTS = 16
EXPERT_FF = 1024
EC_CAPACITY = 2

kernel_name = 'hybrid_gdn_cmlp_pool_ecmoe_dit'


def rms_norm(x, w):
    xf = x.astype(jnp.float32)
    y = xf * lax.rsqrt(jnp.mean(xf * xf, axis=-1, keepdims=True) + NORM_EPS)
    return (y * w.astype(jnp.float32)).astype(x.dtype)


def layer_norm(x, w, b):
    xf = x.astype(jnp.float32)
    mu = jnp.mean(xf, axis=-1, keepdims=True)
    var = jnp.mean(jnp.square(xf - mu), axis=-1, keepdims=True)
    y = (xf - mu) * lax.rsqrt(var + NORM_EPS) * w.astype(jnp.float32) + b.astype(jnp.float32)
    return y.astype(x.dtype)


def l2_normalize(t):
    return t * lax.rsqrt(jnp.sum(t * t, axis=-1, keepdims=True) + NORM_EPS)


def depthwise_conv_centred(x, w):
    pad = w.shape[0] // 2
    return lax.conv_general_dilated(x, w[:, None, :], window_strides=(1,), padding=[(pad, pad)],
                                    dimension_numbers=('NWC', 'WIO', 'NWC'),
                                    feature_group_count=x.shape[-1])


def gated_delta_chunked(q, k, v, g, beta, s0):
    B, L, H, dk = q.shape
    dv = v.shape[-1]
    C = GDN_CHUNK
    N = L // C

    def to_chunks(t):
        t = t.reshape((B, N, C, H) + t.shape[3:])
        return jnp.moveaxis(t, (1, 3), (0, 2))

    qc, kc, vc, gr, bc = (to_chunks(t) for t in (q, k, v, g, beta))
    gc = jnp.cumsum(gr, axis=-1)
    incl = np.tril(np.ones((C, C), dtype=bool))
    strict = np.tril(np.ones((C, C), dtype=bool), -1)
    decay = jnp.exp(jnp.where(incl, gc[..., :, None] - gc[..., None, :], -jnp.inf))
    kb = kc * bc[..., None]
    m = jnp.where(strict, jnp.einsum('nbhik,nbhjk->nbhij', kb, kc) * decay, 0.0)
    a = m + jnp.eye(C, dtype=m.dtype)
    rhs = jnp.concatenate([vc * bc[..., None], kb * jnp.exp(gc)[..., None]], axis=-1)
    sol = lax.linalg.triangular_solve(a, rhs, left_side=True, lower=True, unit_diagonal=True)
    uc, wc = sol[..., :dv], sol[..., dv:]
    attn = jnp.einsum('nbhik,nbhjk->nbhij', qc, kc) * decay

    def step(s, xs):
        q_, k_, u_, w_, g_, at_ = xs
        v_new = u_ - jnp.einsum('bhck,bhkv->bhcv', w_, s)
        o_ = (jnp.einsum('bhck,bhkv->bhcv', q_ * jnp.exp(g_)[..., None], s)
              + jnp.einsum('bhij,bhjv->bhiv', at_, v_new))
        g_last = g_[..., -1:]
        s = (s * jnp.exp(g_last)[..., None]
             + jnp.einsum('bhck,bhcv->bhkv', k_ * jnp.exp(g_last - g_)[..., None], v_new))
        return s, o_

    s_fin, o = lax.scan(step, s0, (qc, kc, uc, wc, gc, attn))
    o = jnp.moveaxis(o, (0, 2), (1, 3)).reshape(B, L, H, dv)
    return o, s_fin


def gdn_branch(q, k, v, z, b_raw, a_raw, init_states, lp):
    B, L, _ = q.shape
    f32 = jnp.float32
    qkv = jax.nn.silu(depthwise_conv_centred(jnp.concatenate([q, k, v], axis=-1), lp['qkv_conv_w']))
    q, k, v = jnp.split(qkv.astype(f32), 3, axis=-1)
    heads = lambda t: t.reshape(B, L, GDN_HEADS, GDN_HEAD_DIM)
    q = l2_normalize(heads(q)) * (GDN_HEAD_DIM ** -0.5)
    k = l2_normalize(heads(k))
    v = heads(v)
    beta = jax.nn.sigmoid(b_raw.astype(f32).reshape(B, L, 2, GDN_HEADS))
    g = (-jnp.exp(lp['gdn_a_log'].astype(f32))
         * jax.nn.softplus(a_raw.astype(f32).reshape(B, L, 2, GDN_HEADS) + lp['gdn_dt_bias'].astype(f32)))
    o_f, s_f = gated_delta_chunked(q, k, v, g[:, :, 0], beta[:, :, 0], init_states[0])
    rev = lambda t: jnp.flip(t, axis=1)
    o_b, s_b = gated_delta_chunked(rev(q), rev(k), rev(v), rev(g[:, :, 1]), rev(beta[:, :, 1]), init_states[1])
    o = o_f + rev(o_b)
    o = o * lax.rsqrt(jnp.mean(o * o, axis=-1, keepdims=True) + NORM_EPS) * lp['gdn_norm_w'].astype(f32)
    o = o * jax.nn.silu(heads(z.astype(f32)))
    return o.reshape(B, L, GDN_WIDTH).astype(z.dtype), (s_f, s_b)


def chunk_mlp_branch(u, vg, lp):
    B, L, _ = u.shape
    n_chunks = L // CMLP_CHUNK
    u = jax.nn.gelu(u)
    vg = layer_norm(jax.nn.gelu(vg), lp['cmlp_ln_w'], lp['cmlp_ln_b'])
    vr = vg.reshape(B, n_chunks, CMLP_CHUNK, CMLP_GROUPS, CMLP_GROUP_DIM)
    mixed = jnp.einsum('gpq,bnqgc->bnpgc', lp['cmlp_w_s'], vr) + lp['cmlp_b_s'].T[:, :, None]
    return u * mixed.reshape(B, L, CMLP_WIDTH)


def pool_branch(p, n_rows, lp):
    B, L, _ = p.shape
    seg = L // n_rows
    pf = p.astype(jnp.float32).reshape(B, n_rows, seg, POOL_GROUPS, POOL_GROUP_DIM)
    cs = jnp.pad(jnp.cumsum(pf, axis=2), ((0, 0), (0, 0), (1, 0), (0, 0), (0, 0)))
    t = np.arange(seg)
    means = []
    for gi, w in enumerate(POOL_WINDOWS):
        lo = np.clip(t - w // 2, 0, seg)
        hi = np.clip(t + w // 2, 0, seg)
        csg = cs[:, :, :, gi]
        win_sum = jnp.take(csg, hi, axis=2) - jnp.take(csg, lo, axis=2)
        means.append(win_sum / (hi - lo).astype(np.float32)[:, None])
    pooled = jnp.stack(means, axis=3) - pf
    mixed = jnp.einsum('brsgc,gcd->brsgd', pooled, lp['pool_w'].astype(jnp.float32))
    return (mixed.reshape(B, L, POOL_WIDTH) * lp['pool_scale'].astype(jnp.float32)).astype(p.dtype)


def hybrid_mixer(h, init_states, n_rows, lp, states_only):
    splits = [int(s) for s in np.cumsum(IN_SIZES)[:-1]]
    proj = jnp.einsum('bld,de->ble', h, lp['w_in'])
    q, k, v, z, b_raw, a_raw, u, vg, p, gate_raw = jnp.split(proj, splits, axis=-1)
    y_a, states = gdn_branch(q, k, v, z, b_raw, a_raw, init_states, lp)
    if states_only:
        return None, states
    y_b = chunk_mlp_branch(u, vg, lp)
    y_c = pool_branch(p, n_rows, lp)
    gates = jax.nn.sigmoid(gate_raw.astype(jnp.float32)).astype(h.dtype)
    g_a, g_b, g_c = jnp.split(gates, N_BRANCH, axis=-1)
    merged = (g_a * jnp.einsum('blc,cd->bld', y_a, lp['w_br_a'])
              + g_b * jnp.einsum('blc,cd->bld', y_b, lp['w_br_b'])
              + g_c * jnp.einsum('blc,cd->bld', y_c, lp['w_br_c']))
    return jnp.einsum('bld,de->ble', merged, lp['w_out']), states


def expert_choice_ffn(h, lp):
    B, n, D = h.shape
    cap = EC_CAPACITY * n // N_EXPERTS
    aff = jax.nn.softmax(jnp.einsum('bnd,de->bne', h, lp['w_router']).astype(jnp.float32), axis=-1)
    top_w, top_i = lax.top_k(jnp.swapaxes(aff, 1, 2), cap)
    xe = jax.vmap(lambda hb, ib: hb[ib])(h, top_i)
    hid = (jax.nn.silu(jnp.einsum('becd,edf->becf', xe, lp['w_gate']))
           * jnp.einsum('becd,edf->becf', xe, lp['w_up']))
    ye = jnp.einsum('becf,efd->becd', hid, lp['w_down']) * top_w[..., None].astype(h.dtype)
    return jax.vmap(lambda ib, yb: jnp.zeros((n, D), yb.dtype).at[ib.reshape(-1)].add(yb.reshape(-1, D)))(top_i, ye)


def setup_inputs(seed: int = 0) -> dict:
    key = jax.random.key(seed)
    ks = iter(jax.random.split(key, 40))
    nrm = lambda shape, scale: jax.random.normal(next(ks), shape, jnp.float32) * scale
    L, D = DEPTH, D_MODEL
    x = nrm((BATCH, SEQ, D), 1.0)
    c = nrm((BATCH, D), 1.0)
    ctx = nrm((BATCH, CTX_LEN, D), 1.0)
    c_ctx = nrm((D,), 1.0)
    w_ada = nrm((L, D, 6 * D), 0.5 * D ** -0.5)
    b_ada = nrm((L, 6 * D), 0.02)
    norm1_w = 1.0 + nrm((L, D), 0.05)
    norm2_w = 1.0 + nrm((L, D), 0.05)
    w_in = nrm((L, D, IN_COLS), D ** -0.5)
    qkv_conv_w = nrm((L, QKV_CONV, 3 * GDN_WIDTH), QKV_CONV ** -0.5)
    gdn_a_log = jnp.log(jax.random.uniform(next(ks), (L, 2, GDN_HEADS), jnp.float32, 1.0, 16.0))
    dt = jnp.exp(jax.random.uniform(next(ks), (L, 2, GDN_HEADS), jnp.float32, math.log(1e-3), math.log(0.1)))
    gdn_dt_bias = dt + jnp.log(-jnp.expm1(-dt))
    gdn_norm_w = 1.0 + nrm((L, GDN_HEAD_DIM), 0.05)
    cmlp_ln_w = 1.0 + nrm((L, CMLP_WIDTH), 0.05)
    cmlp_ln_b = nrm((L, CMLP_WIDTH), 0.02)
    cmlp_w_s = nrm((L, CMLP_GROUPS, CMLP_CHUNK, CMLP_CHUNK), CMLP_CHUNK ** -0.5)
    cmlp_b_s = 1.0 + nrm((L, CMLP_GROUPS, CMLP_CHUNK), 0.05)
    pool_w = nrm((L, POOL_GROUPS, POOL_GROUP_DIM, POOL_GROUP_DIM), POOL_GROUP_DIM ** -0.5)
    pool_scale = 1.0 + nrm((L, POOL_WIDTH), 0.05)
    w_br_a = nrm((L, GDN_WIDTH, D), GDN_WIDTH ** -0.5)
    w_br_b = nrm((L, CMLP_WIDTH, D), CMLP_WIDTH ** -0.5)
    w_br_c = nrm((L, POOL_WIDTH, D), POOL_WIDTH ** -0.5)
    w_out = nrm((L, D, D), D ** -0.5)
    w_router = nrm((L, D, N_EXPERTS), D ** -0.5)
    w_gate = nrm((L, N_EXPERTS, D, EXPERT_FF), D ** -0.5)
    w_up = nrm((L, N_EXPERTS, D, EXPERT_FF), D ** -0.5)
    w_down = nrm((L, N_EXPERTS, EXPERT_FF, D), EXPERT_FF ** -0.5)
    final_norm_w = 1.0 + nrm((D,), 0.05)
    return {'x': x, 'c': c, 'ctx': ctx, 'c_ctx': c_ctx, 'w_ada': w_ada, 'b_ada': b_ada,
            'norm1_w': norm1_w, 'norm2_w': norm2_w, 'w_in': w_in, 'qkv_conv_w': qkv_conv_w,
            'gdn_a_log': gdn_a_log, 'gdn_dt_bias': gdn_dt_bias, 'gdn_norm_w': gdn_norm_w,
            'cmlp_ln_w': cmlp_ln_w, 'cmlp_ln_b': cmlp_ln_b, 'cmlp_w_s': cmlp_w_s, 'cmlp_b_s': cmlp_b_s,
            'pool_w': pool_w, 'pool_scale': pool_scale, 'w_br_a': w_br_a, 'w_br_b': w_br_b,
            'w_br_c': w_br_c, 'w_out': w_out, 'w_router': w_router, 'w_gate': w_gate, 'w_up': w_up,
            'w_down': w_down, 'final_norm_w': final_norm_w}


def reference(x, c, ctx, c_ctx, w_ada, b_ada, norm1_w, norm2_w, w_in, qkv_conv_w, gdn_a_log,
              gdn_dt_bias, gdn_norm_w, cmlp_ln_w, cmlp_ln_b, cmlp_w_s, cmlp_b_s, pool_w, pool_scale,
              w_br_a, w_br_b, w_br_c, w_out, w_router, w_gate, w_up, w_down, final_norm_w):
    B = x.shape[0]
    ROWS = x.shape[1] // GRID_W
    zero_states = (jnp.zeros((B, GDN_HEADS, GDN_HEAD_DIM, GDN_HEAD_DIM), jnp.float32),) * 2
    for l in range(DEPTH):
        last = l == DEPTH - 1
        lp = {'w_in': w_in[l], 'qkv_conv_w': qkv_conv_w[l], 'gdn_a_log': gdn_a_log[l],
              'gdn_dt_bias': gdn_dt_bias[l], 'gdn_norm_w': gdn_norm_w[l], 'cmlp_ln_w': cmlp_ln_w[l],
              'cmlp_ln_b': cmlp_ln_b[l], 'cmlp_w_s': cmlp_w_s[l], 'cmlp_b_s': cmlp_b_s[l],
              'pool_w': pool_w[l], 'pool_scale': pool_scale[l], 'w_br_a': w_br_a[l],
              'w_br_b': w_br_b[l], 'w_br_c': w_br_c[l], 'w_out': w_out[l], 'w_router': w_router[l],
              'w_gate': w_gate[l], 'w_up': w_up[l], 'w_down': w_down[l]}
        mod_x = (jax.nn.silu(c) @ w_ada[l] + b_ada[l])[:, None, :]
        mod_c = (jax.nn.silu(c_ctx) @ w_ada[l] + b_ada[l]).reshape(1, 1, -1)
        sh1x, sc1x, g1x, sh2x, sc2x, g2x = jnp.split(mod_x, 6, axis=-1)
        sh1c, sc1c, g1c, sh2c, sc2c, g2c = jnp.split(mod_c, 6, axis=-1)

        hc = rms_norm(ctx, norm1_w[l]) * (1.0 + sc1c) + sh1c
        ctx_mix, ctx_states = hybrid_mixer(hc, zero_states, 1, lp, states_only=last)
        hx = rms_norm(x, norm1_w[l]) * (1.0 + sc1x) + sh1x
        x_mix, _ = hybrid_mixer(hx, ctx_states, ROWS, lp, states_only=False)
        x = x + g1x * x_mix

        hx2 = rms_norm(x, norm2_w[l]) * (1.0 + sc2x) + sh2x
        x = x + g2x * expert_choice_ffn(hx2, lp)
        if not last:
            ctx = ctx + g1c * ctx_mix
            hc2 = rms_norm(ctx, norm2_w[l]) * (1.0 + sc2c) + sh2c
            ctx = ctx + g2c * expert_choice_ffn(hc2, lp)
    return rms_norm(x, final_norm_w)
```

```python
import numpy as np
import concourse.bass as bass
import concourse.mybir as mybir
from concourse.bass_utils import run_bass_kernel_spmd

F32 = mybir.dt.float32
BF16 = mybir.dt.bfloat16
AF = mybir.ActivationFunctionType
ALU = mybir.AluOpType
AX = mybir.AxisListType

ENGS = ("pe", "act", "dve", "pool", "sp")


def _box(ap):
    t = ap.tensor
    name = t.name
    space = str(ap.space)
    dims = ap.ap
    off = ap.offset
    if space == "DRAM":
        lo = off
        hi = off + sum((c - 1) * abs(s) for s, c in dims) + 1
        return (name, 0, 1, lo, hi)
    pstride = dims[0][0]
    if pstride == 0:
        pstride = 1 << 40
    tshape = t.shape
    fsz = 1
    for s in list(tshape)[1:]:
        fsz *= s
    p0 = off // fsz
    f0 = off % fsz
    npart = dims[0][1]
    f1 = f0 + sum((c - 1) * abs(s) for s, c in dims[1:]) + 1
    return (name, p0, p0 + npart, f0, f1)


def _overlap(a, b):
    return a[1] < b[2] and b[1] < a[2] and a[3] < b[4] and b[3] < a[4]


def _contains(a, b):
    return a[1] <= b[1] and b[2] <= a[2] and a[3] <= b[3] and b[4] <= a[4]


class Op:
    __slots__ = ("eng", "fn", "idx", "deps", "is_dma", "semkey", "has_dep", "tick",
                 "group", "pos")

    def __init__(self, eng, fn, is_dma=False, semkey=None):
        self.eng = eng
        self.fn = fn
        self.is_dma = is_dma
        self.semkey = semkey
        self.deps = set()
        self.has_dep = False
        self.tick = None
        self.group = None


class Prog:
    def __init__(self, nc, n_dma_sems=80):
        self.nc = nc
        self.ops = []
        self.acc = {}
        self.n_dma_sems = n_dma_sems
        self.same_engine_sync = True

    def _track(self, op, reads, writes):
        ekey = ("dma", op.semkey) if op.is_dma else op.eng
        preads = [ap for ap in reads if str(ap.space) == "PSUM"]
        reads = [ap for ap in reads if str(ap.space) != "PSUM"]
        writes = list(writes) + preads
        for ap in reads:
            b = _box(ap)
            ent = self.acc.setdefault(b[0], {"w": [], "r": {}})
            for (ob, oop) in ent["w"]:
                if _overlap(ob, b):
                    op.deps.add(oop)
            ent["r"][(b, ekey)] = op
        for ap in writes:
            b = _box(ap)
            if str(ap.space) == "PSUM":
                b = (b[0], 0, 128, 0, 1 << 30)
            ent = self.acc.setdefault(b[0], {"w": [], "r": {}})
            keep = []
            for (ob, oop) in ent["w"]:
                if oop is op:
                    keep.append((ob, oop))
                    continue
                if _overlap(ob, b):
                    op.deps.add(oop)
                    if _contains(b, ob):
                        continue
                keep.append((ob, oop))
            keep.append((b, op))
            ent["w"] = keep
            rk = {}
            for (ob, ek), oop in ent["r"].items():
                if oop is op:
                    rk[(ob, ek)] = oop
                    continue
                if _overlap(ob, b):
                    op.deps.add(oop)
                    if _contains(b, ob):
                        continue
                rk[(ob, ek)] = oop
            ent["r"] = rk
        op.deps.discard(op)

    def add(self, eng, fn, reads=(), writes=()):
        op = Op(eng, fn)
        op.idx = len(self.ops)
        self.ops.append(op)
        self._track(op, reads, writes)
        return op

    def dma(self, out, in_, eng="sp", semkey=None, **kw):
        sb = out if str(out.space) != "DRAM" else in_
        if semkey is None:
            semkey = sb.tensor.name
        op = Op(eng, lambda e, out=out, in_=in_, kw=kw: e.dma_start(out=out, in_=in_, **kw),
                is_dma=True, semkey=semkey)
        op.idx = len(self.ops)
        self.ops.append(op)
        self._track(op, [in_], [out])
        return op

    def mm(self, out, lhsT, rhs, start=True, stop=True, **kw):
        reads = [lhsT, rhs] + ([] if start else [out])
        return self.add("pe", lambda e: e.matmul(out, lhsT, rhs, start=start, stop=stop, **kw),
                        reads, [out])

    def transpose(self, out, in_, ident):
        return self.add("pe", lambda e: e.transpose(out, in_, ident), [in_, ident], [out])

    def act(self, out, in_, func, bias=None, scale=1.0, accum_out=None, eng="act"):
        reads = [in_]
        if bias is not None and not isinstance(bias, (int, float)):
            reads.append(bias)
        if not isinstance(scale, (int, float)):
            reads.append(scale)
        writes = [out] + ([accum_out] if accum_out is not None else [])
        kw = {}
        if accum_out is not None:
            kw["accum_out"] = accum_out
        if bias is not None:
            kw["bias"] = bias
        return self.add(eng, lambda e: e.activation(out=out, in_=in_, func=func, scale=scale, **kw),
                        reads, writes)

    def tt(self, out, in0, in1, op, eng="dve"):
        return self.add(eng, lambda e: e.tensor_tensor(out=out, in0=in0, in1=in1, op=op),
                        [in0, in1], [out])

    def ts(self, out, in0, s1, s2, op0, op1=None, eng="dve", accum_out=None):
        reads = [in0] + [s for s in (s1, s2) if s is not None and not isinstance(s, (int, float))]
        writes = [out] + ([accum_out] if accum_out is not None else [])
        kw = {}
        if op1 is not None:
            kw["op1"] = op1
        if accum_out is not None:
            kw["accum_out"] = accum_out
        return self.add(eng, lambda e: e.tensor_scalar(out=out, in0=in0, scalar1=s1, scalar2=s2,
                                                       op0=op0, **kw), reads, writes)

    def stt(self, out, in0, scalar, in1, op0, op1, eng="dve", accum_out=None):
        reads = [in0, in1] + ([scalar] if not isinstance(scalar, (int, float)) else [])
        writes = [out] + ([accum_out] if accum_out is not None else [])
        kw = {}
        if eng == "pool":
            eng = "dve"
        if accum_out is not None:
            kw["accum_out"] = accum_out
        return self.add(eng, lambda e: e.scalar_tensor_tensor(out=out, in0=in0, scalar=scalar, in1=in1,
                                                              op0=op0, op1=op1, **kw), reads, writes)

    def copy(self, out, in_, eng="dve"):
        if eng == "act":
            return self.add("act", lambda e: e.copy(out=out, in_=in_), [in_], [out])
        return self.add(eng, lambda e: e.tensor_copy(out=out, in_=in_), [in_], [out])

    def memset(self, ap, val, eng="pool"):
        return self.add(eng, lambda e: e.memset(ap, val), [], [ap])

    def reduce(self, out, in_, op, axis=None, eng="dve"):
        axis = axis or AX.X
        return self.add(eng, lambda e: e.tensor_reduce(out=out, in_=in_, axis=axis, op=op), [in_], [out])

    def emit(self, final_wait_ops=()):
        nc = self.nc
        ops = self.ops
        streams = {e: [] for e in ENGS}
        for op in ops:
            op.pos = len(streams[op.eng])
            streams[op.eng].append(op)
        for op in ops:
            red = {}
            dm = []
            for d in op.deps:
                if d.is_dma:
                    dm.append(d)
                else:
                    if d.eng == op.eng and (d.eng == "pe" or not self.same_engine_sync) and not op.is_dma:
                        continue
                    if d.eng not in red or red[d.eng].idx < d.idx:
                        red[d.eng] = d
            op.deps = list(red.values()) + dm
            for d in op.deps:
                d.has_dep = True
        for op in final_wait_ops:
            op.has_dep = True
        LIM = 30000
        cnt = {e: 0 for e in ENGS}
        for op in ops:
            if not op.is_dma and op.has_dep:
                cnt[op.eng] += 1
                op.tick = ((cnt[op.eng] - 1) // LIM, (cnt[op.eng] - 1) % LIM + 1)
        keys = []
        for op in ops:
            if op.is_dma and op.semkey not in keys:
                keys.append(op.semkey)
        nsem = min(len(keys), self.n_dma_sems)
        key2sem = {k: i % max(nsem, 1) for i, k in enumerate(keys)}
        semstate = {}
        dma_wait_prev = {}
        consumers = {}
        for op in ops:
            for d in op.deps:
                if d.is_dma:
                    consumers.setdefault(d, []).append(op)
        first_consumer_idx = {}
        for d, cl in consumers.items():
            first_consumer_idx[d] = min(c.idx for c in cl)
        for op in final_wait_ops:
            if op.is_dma:
                first_consumer_idx.setdefault(op, len(ops))
        groups = []
        for op in ops:
            if not op.is_dma:
                continue
            s = key2sem[op.semkey]
            st = semstate.setdefault(s, {"total": 0, "open": None, "close_at": None, "prev_final": 0})
            g = st["open"]
            if g is not None and st["close_at"] is not None and st["close_at"] <= op.idx:
                st["prev_final"] = g["final"]
                g = None
            if g is None:
                g = {"sem": s, "members": [], "final": st["total"]}
                groups.append(g)
                st["open"] = g
                st["close_at"] = None
                if st["prev_final"] > 0:
                    dma_wait_prev[op] = (s, st["prev_final"])
            st["total"] += 16
            g["members"].append(op)
            g["final"] = st["total"]
            op.group = g
            fc = first_consumer_idx.get(op)
            if fc is not None:
                st["close_at"] = fc if st["close_at"] is None else min(st["close_at"], fc)
        self.stats = {"n_ops": len(ops), "n_dma_sems": nsem, "incs": dict(cnt)}
        from contextlib import ExitStack
        es = ExitStack()
        esem = {}
        for e in ENGS:
            if e == "sp":
                continue
            for gen in range((cnt[e] + LIM - 1) // LIM + 1):
                esem[(e, gen)] = es.enter_context(nc.semaphore("c_%s%d" % (e, gen)))
        dsem = [es.enter_context(nc.semaphore("d_%d" % i)) for i in range(nsem)]
        known = {e: {} for e in ENGS}
        nwaits = 0

        def emit_stream(ename, eobj):
            nonlocal nwaits
            kn = known[ename]
            for op in streams[ename]:
                waits = {}
                for d in op.deps:
                    if d.is_dma:
                        g = d.group
                        key = ("d", g["sem"])
                        val = g["final"]
                    else:
                        key = ("e", d.eng, d.tick[0])
                        val = d.tick[1]
                    if waits.get(key, 0) < val:
                        waits[key] = val
                if op in dma_wait_prev:
                    s, v = dma_wait_prev[op]
                    key = ("d", s)
                    if waits.get(key, 0) < v:
                        waits[key] = v
                for key, val in waits.items():
                    if kn.get(key, 0) >= val:
                        continue
                    kn[key] = val
                    sem = dsem[key[1]] if key[0] == "d" else esem[(key[1], key[2])]
                    eobj.wait_ge(sem, val)
                    nwaits += 1
                ins = op.fn(eobj)
                if op.is_dma:
                    ins.then_inc(dsem[op.group["sem"]], 16)
                elif op.has_dep:
                    ins.then_inc(esem[(ename, op.tick[0])], 1)
            if ename == "sp":
                for op in final_wait_ops:
                    g = op.group
                    eobj.wait_ge(dsem[g["sem"]], g["final"])

        with nc.Block() as block:
            @block.tensor
            def _(e):
                emit_stream("pe", e)

            @block.scalar
            def _(e):
                emit_stream("act", e)

            @block.vector
            def _(e):
                emit_stream("dve", e)

            @block.gpsimd
            def _(e):
                emit_stream("pool", e)

            @block.sync
            def _(e):
                emit_stream("sp", e)
        self.stats["nwaits"] = nwaits
        es.close()


from contextlib import ExitStack

D = 1024
KC = 8
H = 8
NL = 2
OFF_Q, OFF_K, OFF_V, OFF_Z, OFF_B, OFF_A, OFF_U, OFF_VG, OFF_P, OFF_G = (
    0, 1024, 2048, 3072, 4096, 4112, 4128, 4640, 5152, 5664)
IN_COLS = 8736
EPS = 1e-6
NEG = -30000.0
TX = 2048
TC = 256
NE = 16

C_ID, C_U, C_L, C_NMF, C_NMB, C_SL, C_SU, C_ONE = [i * 128 for i in range(8)]
C_IOTA = 1024
C_RCX = 1280
C_RCC = 1536
NCST = 2560


def make_consts():
    c = np.zeros((128, NCST), np.float32)
    i = np.arange(128)[:, None]
    j = np.arange(128)[None, :]
    c[:, C_ID:C_ID + 128] = (i == j)
    c[:, C_U:C_U + 128] = (i <= j)
    c[:, C_L:C_L + 128] = (i >= j)
    c[:, C_NMF:C_NMF + 128] = np.where(i >= j, 0.0, NEG)
    c[:, C_NMB:C_NMB + 128] = np.where(i <= j, 0.0, NEG)
    c[:, C_SL:C_SL + 128] = (i > j)
    c[:, C_SU:C_SU + 128] = (i < j)
    c[:, C_ONE:C_ONE + 128] = 1.0
    c[:, C_IOTA:C_IOTA + 256] = np.arange(1, 257)[None, :]
    for seg, off in ((64, C_RCX), (256, C_RCC)):
        t = np.arange(seg)
        for gi, w in enumerate((2, 4, 8, 16)):
            lo = np.clip(t - w // 2, 0, seg)
            hi = np.clip(t + w // 2, 0, seg)
            c[:, off + gi * seg: off + (gi + 1) * seg] = (1.0 / (hi - lo).astype(np.float32))[None, :]
    return c


def tokblocks(T):
    bs = min(512, T)
    return [(s, bs) for s in range(0, T, bs)]


class K:
    pass


def build(cfg):
    nc = bass.Bass("TRN2", target_bir_lowering=False)
    k = K()
    k.nc = nc
    k.cfg = cfg
    P = Prog(nc)
    k.P = P
    es = ExitStack()
    k.fin = []
    k.dbg = {}

    def din(name, shape, dt=F32):
        return nc.dram_tensor(name, list(shape), dt, kind="ExternalInput").ap()

    def dscr(name, shape, dt=F32):
        return nc.dram_tensor(name, list(shape), dt).ap()

    def dout(name, shape, dt=F32):
        return nc.dram_tensor(name, list(shape), dt, kind="ExternalOutput").ap()

    def sb(name, shape, dt=F32):
        return es.enter_context(nc.sbuf_tensor(name, list(shape), dt))

    def ps(name, shape, dt=F32):
        return es.enter_context(nc.psum_tensor(name, list(shape), dt))

    k.dout = dout
    I = K()
    k.I = I
    I.x = din("x", [2, TX, D])
    I.ctx = din("ctx", [2, TC, D])
    I.ccol = din("ccol", [128, KC, 3])
    I.w_ada = din("w_ada", [NL, D, 6 * D])
    I.b_ada_col = din("b_ada_col", [NL, 128, 48])
    I.n1col = din("n1col", [NL, 128, KC])
    I.n2col = din("n2col", [NL, 128, KC])
    I.fnw_bc = din("fnw_bc", [128, D])
    I.w_in = din("w_in", [NL, D, IN_COLS])
    I.convw = din("convw", [NL, 128, 24, 5])
    I.alog_bc = din("alog_bc", [NL, 128, 16])
    I.dtb_bc = din("dtb_bc", [NL, 128, 16])
    I.gnw_bc = din("gnw_bc", [NL, 128, 128])
    I.lnw_bc = din("lnw_bc", [NL, 128, 512])
    I.lnb_bc = din("lnb_bc", [NL, 128, 512])
    I.wsT = din("wsT", [NL, 4, 128, 128])
    I.bs_col = din("bs_col", [NL, 128, 4])
    I.pool_w = din("pool_w", [NL, 4, 128, 128])
    I.psc_col = din("psc_col", [NL, 128, 4])
    I.w_br_a = din("w_br_a", [NL, 1024, D])
    I.w_br_b = din("w_br_b", [NL, 512, D])
    I.w_br_c = din("w_br_c", [NL, 512, D])
    I.w_out = din("w_out", [NL, D, D])
    I.w_router = din("w_router", [NL, D, NE])
    I.w_gate = din("w_gate", [NL, NE, D, D])
    I.w_up = din("w_up", [NL, NE, D, D])
    I.w_down = din("w_down", [NL, NE, D, D])
    I.cst = din("cst", [128, NCST])
    k.out = dout("out", [2, TX, D])
    S = K()
    k.S = S
    S.xcur = dscr("xcur", [2, TX, D])
    S.ccur = dscr("ccur", [2, TC, D])
    S.gates = dscr("gatesD", [24, 128, TX], BF16)
    B = K()
    k.B = B
    B.cst = sb("cst_sb", [128, 1280])
    B.cstb = sb("cstb", [128, 1024 + 256], BF16)
    B.AB = sb("AB", [128, 49152], BF16)
    B.AF = sb("AF", [128, 16384], F32)
    B.xt = [sb("xt%d" % i, [128, D]) for i in range(2)]
    B.xn = sb("xn", [128, D], BF16)
    B.st = sb("st", [128, 64])
    B.wst = [sb("wst%d" % i, [128, KC, 256], BF16) for i in range(2)]
    B.wsm = [sb("wsm%d" % i, [128, KC, 128], BF16) for i in range(4)]
    B.mod = sb("mod", [128, NL, 3, 48])
    B.modA = sb("modA", [128, NL, 3, 2, KC])
    B.sc3 = sb("sc3", [128, KC, 3])
    B.small = sb("small", [128, 2048])
    B.states = B.AF[:, 10240:12288].rearrange("p (a c) -> p a c", c=128)
    B.gsc = B.AF[:, 12288:14592].rearrange("p (a b c) -> p a b c", a=9, b=16)
    B.junk = sb("junk", [128, D], BF16)
    k.pf = [ps("pf%d" % i, [128, 512]) for i in range(6)]
    k.pb = [ps("pb%d" % i, [128, 1024], BF16) for i in range(2)]

    def cst(off, n=128):
        return B.cst[:, off:off + n]

    def cstb(off, n=128):
        return B.cstb[:, off:off + n]

    k.c = cst
    k.cb = cstb
    P.dma(B.cst[:], I.cst[:, 0:1280])
    P.copy(B.cstb[:, 0:1024], B.cst[:, 0:1024])
    P.copy(B.cstb[:, 1024:1280], B.cst[:, C_IOTA:C_IOTA + 256])
    k.rr = [0]

    prologue(k)
    for b in cfg["batches"]:
        for l in cfg["layers"]:
            last = l == NL - 1
            src_c = I.ctx[b] if l == 0 else S.ccur[b]
            src_x = I.x[b] if l == 0 else S.xcur[b]
            if "ctx" in cfg["streams"]:
                mixer(k, b, l, src_c, S.ccur[b], TC, True, last)
                if not last and cfg.get("moe", True):
                    moe(k, b, l, S.ccur[b], S.ccur[b], TC, True)
                if not last:
                    dump_dram(k, "ccur", S.ccur[b], TC)
            if "x" in cfg["streams"]:
                mixer(k, b, l, src_x, S.xcur[b], TX, False, False)
                if cfg.get("moe", True):
                    moe(k, b, l, S.xcur[b], S.xcur[b], TX, False)
                dump_dram(k, "xcur%d" % l, S.xcur[b], TX)
        if cfg.get("final", True):
            final_norm(k, b)
    P.emit(final_wait_ops=k.fin)
    es.close()
    k.stats = P.stats
    return k


def tap(k, name, ap_sb, shape=None, dt=F32):
    if name not in k.cfg.get("taps", ()):
        return
    shp = list(ap_sb.shape)
    o = k.dout("tap_" + name, shp, ap_sb.dtype)
    k.fin.append(k.P.dma(o, ap_sb, semkey="tap"))


def evac_eng(k):
    k.rr[0] += 1
    return "act" if k.rr[0] % 2 else "dve"


def pcopy(k, out, in_, eng=None):
    eng = eng or evac_eng(k)
    k.P.copy(out, in_, eng=eng)


def prologue(k):
    P, B, I = k.P, k.B, k.I
    raw = B.small[:, 0:24].rearrange("p (a b) -> p a b", a=KC)
    P.dma(raw, I.ccol)
    P.act(B.sc3[:], raw, AF.Silu)
    for l in range(NL):
        wv = I.w_ada[l].rearrange("(kk p) c -> p kk c", p=128)
        acc = k.pf[0][:, 0:144].rearrange("p (j v) -> p j v", v=3)
        for cb in range(12):
            wblk = B.AF[:, (cb % 2) * 4096:(cb % 2) * 4096 + 4096].rearrange("p (a b) -> p a b", a=KC)
            P.dma(wblk, wv[:, :, cb * 512:(cb + 1) * 512], semkey=("wada", cb % 2))
            for jj in range(4):
                j = cb * 4 + jj
                for kk in range(KC):
                    P.mm(acc[:, j, :], wblk[:, kk, jj * 128:(jj + 1) * 128], B.sc3[:, kk, :],
                         start=(kk == 0), stop=(kk == KC - 1))
        bcol = B.small[:, 32:80]
        P.dma(bcol, I.b_ada_col[l])
        for v in range(3):
            P.tt(B.mod[:, l, v, :], acc[:, :, v], bcol, ALU.add)
        n1 = B.small[:, 80:88]
        n2 = B.small[:, 88:96]
        P.dma(n1, I.n1col[l])
        P.dma(n2, I.n2col[l])
        for v in range(3):
            P.stt(B.modA[:, l, v, 0, :], B.mod[:, l, v, 8:16], 1.0, n1, ALU.add, ALU.mult)
            P.stt(B.modA[:, l, v, 1, :], B.mod[:, l, v, 32:40], 1.0, n2, ALU.add, ALU.mult)
    tap(k, "mod", B.mod[:].rearrange("p l v j -> p (l v j)"))


def rstd_col(k, out_col, ss_col, n, tmp_col):
    P = k.P
    P.ts(tmp_col, ss_col, 1.0 / n, EPS, ALU.mult, ALU.add)
    P.act(tmp_col, tmp_col, AF.Sqrt)
    P.add("dve", lambda e: e.reciprocal(out=out_col, in_=tmp_col), [tmp_col], [out_col])


def norm_to_T(k, src, T, Acol, Shcol, hT, xn_tok=None):
    P, B = k.P, k.B
    NT = T // 128
    for i in range(NT):
        xt = B.xt[i % 2]
        P.dma(xt[:], src[i * 128:(i + 1) * 128, :], semkey=("xt", i % 2))
        st = B.st[:, (i % 2) * 4:(i % 2) * 4 + 4]
        P.act(B.junk[:], xt[:], AF.Square, accum_out=st[:, 0:1])
        rstd_col(k, st[:, 2:3], st[:, 0:1], D, st[:, 1:2])
        xn = xn_tok[:, i, :] if xn_tok is not None else B.xn[:]
        P.ts(xn, xt[:], st[:, 2:3], None, ALU.mult)
        if hT is None:
            continue
        pb = k.pb[i % 2]
        for kk in range(KC):
            P.transpose(pb[:, kk * 128:(kk + 1) * 128], xn[:, kk * 128:(kk + 1) * 128], k.cb(C_ID))
        pv = pb[:].rearrange("p (a b) -> p a b", a=KC)
        dst = hT[:, :, i * 128:(i + 1) * 128]
        P.tt(dst, pv, Acol.unsqueeze(2).broadcast_to([128, KC, 128]), ALU.mult)
        P.tt(dst, dst, Shcol.unsqueeze(2).broadcast_to([128, KC, 128]), ALU.add, eng="pool")


def load_w_cols(k, dst, l, c0, ncols, key):
    wv = k.I.w_in[l].rearrange("(kk p) c -> p kk c", p=128)
    k.P.dma(dst, wv[:, :, c0:c0 + ncols], eng="pool", semkey=key)


def proj_fm(k, hT, T, w, ncol, consume):
    P = k.P
    for j in range(ncol // 128):
        for bi, (t0, n) in enumerate(tokblocks(T)):
            pf = k.pf[(j * 4 + bi) % 4]
            for kk in range(KC):
                P.mm(pf[:, 0:n], w[:, kk, j * 128:(j + 1) * 128], hT[:, kk, t0:t0 + n],
                     start=(kk == 0), stop=(kk == KC - 1))
            consume(j, t0, n, pf[:, 0:n])


def mixer(k, b, l, src, dst, T, is_ctx, states_only):
    P, B, I, S = k.P, k.B, k.I, k.S
    NT = T // 128
    v = 2 if is_ctx else b
    AB = B.AB
    h1T = AB[:, 0:KC * T].rearrange("p (a t) -> p a t", a=KC)
    yaT = AB[:, 16384:16384 + 8 * T].rearrange("p (a t) -> p a t", a=8)
    ybT = AB[:, 32768:32768 + 4 * T].rearrange("p (a t) -> p a t", a=4)
    ycT = AB[:, 40960:40960 + 4 * T].rearrange("p (a t) -> p a t", a=4)
    norm_to_T(k, src, T, B.modA[:, l, v, 0, :], B.mod[:, l, v, 0:8], h1T)
    tap(k, "h1T", h1T)
    if not states_only:
        gate_phase(k, l, h1T, T)
    gdn(k, b, l, h1T, yaT, T, is_ctx, states_only)
    if states_only:
        return
    tap(k, "yaT", yaT)
    cmlp(k, l, h1T, ybT, T)
    tap(k, "ybT", ybT)
    pool(k, l, h1T, ycT, T, is_ctx)
    tap(k, "ycT", ycT)
    merge_out(k, b, l, v, src, dst, yaT, ybT, ycT, T)


def gate_phase(k, l, h1T, T):
    P, B, S = k.P, k.B, k.S
    for blk in range(12):
        w = B.wst[blk % 2]
        load_w_cols(k, w[:], l, OFF_G + blk * 256, 256, ("wst", blk % 2))

        def consume(j, t0, n, pap, blk=blk):
            stg = B.AB[:, 49152 - 1024 + (j % 2) * 512:49152 - 1024 + (j % 2) * 512 + n]
            P.act(stg, pap, AF.Sigmoid)
            P.dma(S.gates[blk * 2 + j, :, t0:t0 + n], stg, eng="sp", semkey=("gst", j % 2))
        proj_fm(k, h1T, T, w, 256, consume)


GS_BETA, GS_NBETA, GS_G, GS_GC, GS_EGC, GS_EDEC, GS_ETOT, GS_BEXP, GS_TOT = range(9)


def gdn(k, b, l, h1T, yaT, T, is_ctx, states_only):
    P, B, I = k.P, k.B, k.I
    NT = T // 128
    AB, AFa = B.AB, B.AF
    GB = 32768
    QT = AB[:, GB:GB + T]
    KT = AB[:, GB + 2048:GB + 2048 + T]
    VT = AB[:, GB + 4096:GB + 4096 + T]
    Ktok = AB[:, GB + 6144:GB + 6144 + T].rearrange("p (a c) -> p a c", c=128)
    Vtok = AB[:, GB + 8192:GB + 8192 + T].rearrange("p (a c) -> p a c", c=128)
    zs = AB[:, GB + 10240:GB + 10240 + T].rearrange("p (a c) -> p a c", c=128)
    yatok = AB[:, GB + 12288:GB + 12288 + T].rearrange("p (a c) -> p a c", c=128)
    tb = GB + 14336
    attn_b = AB[:, tb:tb + 128]
    attnT = AB[:, tb + 128:tb + 256]
    wT = AB[:, tb + 256:tb + 384]
    qgT = AB[:, tb + 384:tb + 512]
    kdec = AB[:, tb + 512:tb + 640]
    vnew = AB[:, tb + 640:tb + 768]
    Sbf = AB[:, tb + 768:tb + 896]
    cbuf = AFa[:, 0:T + 4]
    cs = AFa[:, 2052:2052 + T]
    oacc = AFa[:, 4100:4100 + T].rearrange("p (a c) -> p a c", c=128)
    fb = 6148
    Rm = AFa[:, fb:fb + 128]
    dec = AFa[:, fb + 128:fb + 256]
    egr = AFa[:, fb + 256:fb + 384]
    nk = [AFa[:, fb + 384 + i * 128:fb + 512 + i * 128] for i in range(2)]
    Pk = [AFa[:, fb + 640 + i * 128:fb + 768 + i * 128] for i in range(2)]
    Xk = [AFa[:, fb + 896 + i * 128:fb + 1024 + i * 128] for i in range(2)]
    rhsu = AFa[:, fb + 1152:fb + 1280]
    rhsw = AFa[:, fb + 1280:fb + 1408]
    usb = AFa[:, fb + 1408:fb + 1536]
    Sst = AFa[:, fb + 1536:fb + 1664]
    sq = AFa[:, fb + 1664:fb + 1664 + 512]
    rinv = AFa[:, fb + 2176:fb + 2176 + 512]
    tmpf = AFa[:, fb + 2688:fb + 2688 + 128]
    ident, identb = k.c(C_ID), k.cb(C_ID)
    ones = k.c(C_ONE)
    gs = B.gsc

    wba = B.wsm[0]
    load_w_cols(k, wba[:, :, 0:32], l, OFF_B, 32, ("wsm", 0))
    sm = B.small
    alog = sm[:, 128:144]
    dtb = sm[:, 144:160]
    negA = sm[:, 160:176]
    gnw = sm[:, 256:384]
    P.dma(alog, I.alog_bc[l])
    P.dma(dtb, I.dtb_bc[l])
    P.dma(gnw, I.gnw_bc[l])
    cw = sm[:, 384:504].rearrange("p (a t) -> p a t", t=5)
    P.dma(cw, I.convw[l])
    P.act(negA, alog, AF.Exp)
    P.ts(negA, negA, -1.0, None, ALU.mult)
    for i in range(NT):
        pf = k.pf[4]
        for kk in range(KC):
            P.mm(pf[:, 0:32], h1T[:, kk, i * 128:(i + 1) * 128], wba[:, kk, 0:32],
                 start=(kk == 0), stop=(kk == KC - 1))
        P.act(gs[:, GS_BETA, i, :], pf[:, 0:16], AF.Sigmoid)
        P.tt(gs[:, GS_G, i, :], pf[:, 16:32], dtb, ALU.add)
    nt = slice(0, NT)
    P.ts(gs[:, GS_NBETA, nt, :], gs[:, GS_BETA, nt, :], -1.0, None, ALU.mult)
    P.act(gs[:, GS_G, nt, :], gs[:, GS_G, nt, :], AF.Exp)
    P.act(gs[:, GS_G, nt, :], gs[:, GS_G, nt, :], AF.Ln, bias=1.0)
    P.tt(gs[:, GS_G, nt, :], gs[:, GS_G, nt, :], negA.unsqueeze(1).broadcast_to([128, NT, 16]), ALU.mult)
    for i in range(NT):
        pf = k.pf[4]
        P.mm(pf[:, 0:8], k.c(C_U), gs[:, GS_G, i, 0:8])
        P.mm(pf[:, 8:16], k.c(C_L), gs[:, GS_G, i, 8:16])
        P.mm(pf[:, 16:32], ones, gs[:, GS_G, i, :])
        pcopy(k, gs[:, GS_GC, i, :], pf[:, 0:16], "dve")
        pcopy(k, gs[:, GS_TOT, i, :], pf[:, 16:32], "act")
    P.act(gs[:, GS_EGC, nt, :], gs[:, GS_GC, nt, :], AF.Exp)
    P.act(gs[:, GS_ETOT, nt, :], gs[:, GS_TOT, nt, :], AF.Exp)
    P.tt(gs[:, GS_EDEC, nt, :], gs[:, GS_TOT, nt, :], gs[:, GS_GC, nt, :], ALU.subtract)
    P.act(gs[:, GS_EDEC, nt, :], gs[:, GS_EDEC, nt, :], AF.Exp)
    P.tt(gs[:, GS_BEXP, nt, :], gs[:, GS_BETA, nt, :], gs[:, GS_EGC, nt, :], ALU.mult)
    tap(k, "gsc", gs)

    P.memset(cbuf[:, 0:2], 0.0)
    P.memset(cbuf[:, T + 2:T + 4], 0.0)
    for h in range(H):
        for ci, (off, dstT) in enumerate(((OFF_Q, QT), (OFF_K, KT), (OFF_V, VT))):
            w = B.wsm[1 + ci]
            load_w_cols(k, w[:], l, off + h * 128, 128, ("wsm", 1 + ci))

            def consume(j, t0, n, pap):
                pcopy(k, cbuf[:, 2 + t0:2 + t0 + n], pap)
            proj_fm(k, h1T, T, w, 128, consume)
            ce = "dve" if ci != 1 else "pool"
            cwc = cw[:, ci * 8 + h, :]
            P.ts(cs, cbuf[:, 0:T], cwc[:, 0:1], None, ALU.mult, eng=ce)
            for tp in range(1, 5):
                P.stt(cs, cbuf[:, tp:tp + T], cwc[:, tp:tp + 1], cs, ALU.mult, ALU.add, eng=ce)
            if ci == 2:
                P.act(dstT, cs, AF.Silu)
                continue
            P.act(cs, cs, AF.Silu)
            for (t0, n) in tokblocks(T):
                P.act(sq[:, 0:n], cs[:, t0:t0 + n], AF.Square)
                pf = k.pf[5]
                P.mm(pf[:, 0:n], ones, sq[:, 0:n])
                P.ts(rinv[:, 0:n], pf[:, 0:n], EPS, None, ALU.add)
                P.act(rinv[:, 0:n], rinv[:, 0:n], AF.Sqrt)
                P.add("dve", lambda e, n=n: e.reciprocal(out=rinv[:, 0:n], in_=rinv[:, 0:n]),
                      [rinv[:, 0:n]], [rinv[:, 0:n]])
                if ci == 0:
                    P.stt(dstT[:, t0:t0 + n], cs[:, t0:t0 + n], 128.0 ** -0.5, rinv[:, 0:n], ALU.mult, ALU.mult)
                else:
                    P.tt(dstT[:, t0:t0 + n], cs[:, t0:t0 + n], rinv[:, 0:n], ALU.mult)
        for (srcT, dtok) in ((KT, Ktok), (VT, Vtok)):
            for i0 in range(0, NT, 8):
                pb = k.pb[(i0 // 8) % 2]
                ni = min(8, NT - i0)
                for i in range(ni):
                    P.transpose(pb[:, i * 128:(i + 1) * 128], srcT[:, (i0 + i) * 128:(i0 + i + 1) * 128], identb)
                pcopy(k, dtok[:, i0:i0 + ni, :], pb[:, 0:ni * 128].rearrange("p (a c) -> p a c", c=128))
        if h == 0:
            tap(k, "QT0", QT)
            tap(k, "KT0", KT)
            tap(k, "Vtok0", Vtok)
        if not states_only:
            wz = B.wsm[0]
            load_w_cols(k, wz[:], l, OFF_Z + h * 128, 128, ("wsm", 0))
            for i in range(NT):
                pf = k.pf[4]
                for kk in range(KC):
                    P.mm(pf[:, 0:128], h1T[:, kk, i * 128:(i + 1) * 128], wz[:, kk, :],
                         start=(kk == 0), stop=(kk == KC - 1))
                P.act(zs[:, i, :], pf[:, 0:128], AF.Silu)
        for dr in range(2):
            col = dr * 8 + h
            msk = k.c(C_U) if dr == 0 else k.c(C_L)
            nm = k.c(C_NMF) if dr == 0 else k.c(C_NMB)
            sm_ = k.c(C_SL) if dr == 0 else k.c(C_SU)
            if is_ctx:
                P.memset(Sst, 0.0)
            else:
                P.copy(Sst, B.states[:, col, :], eng="pool")
            P.copy(Sbf, Sst, eng="pool")
            order = range(NT) if dr == 0 else range(NT - 1, -1, -1)
            for c in order:
                tsl = slice(c * 128, (c + 1) * 128)
                sc = lambda kind: gs[:, kind, c, col:col + 1]
                P.ts(Rm, msk, sc(GS_G), -1.0, ALU.mult, ALU.mult, eng="pool")
                pA = k.pf[0]
                P.mm(pA[:, 0:128], ones, Rm)
                P.act(egr, pA[:, 0:128], AF.Exp, scale=-1.0)
                P.tt(dec, pA[:, 0:128], nm, ALU.add)
                P.act(dec, dec, AF.Exp, bias=sc(GS_GC))
                pK = k.pf[1]
                P.mm(pK[:, 0:128], KT[:, tsl], KT[:, tsl])
                P.mm(pK[:, 128:256], QT[:, tsl], KT[:, tsl])
                P.stt(nk[0], pK[:, 0:128], sc(GS_NBETA), dec, ALU.mult, ALU.mult)
                P.tt(nk[0], nk[0], sm_, ALU.mult, eng="pool")
                P.tt(attn_b, pK[:, 128:256], dec, ALU.mult)
                pT = k.pf[2]
                P.transpose(pT[:, 0:128], nk[0], ident)
                P.transpose(k.pb[0][:, 0:128], attn_b, identb)
                pcopy(k, Pk[0], pT[:, 0:128], "act")
                pcopy(k, attnT, k.pb[0][:, 0:128], "act")
                P.tt(Xk[0], pT[:, 0:128], ident, ALU.add)
                cur = 0
                for lev in range(1, 7):
                    nxt = 1 - cur
                    pn = k.pf[3]
                    P.mm(pn[:, 0:128], Pk[cur], nk[cur])
                    if lev < 6:
                        P.mm(pn[:, 128:256], nk[cur], Pk[cur])
                    pcopy(k, nk[nxt], pn[:, 0:128], "act")
                    if lev < 6:
                        pcopy(k, Pk[nxt], pn[:, 128:256], "dve")
                    px = k.pf[2]
                    P.mm(px[:, 0:128], ident, Xk[cur], start=True, stop=False)
                    P.mm(px[:, 0:128], nk[nxt], Xk[cur], start=False, stop=True)
                    pcopy(k, Xk[nxt], px[:, 0:128], "dve")
                    cur = nxt
                X = Xk[cur]
                P.ts(rhsu, Vtok[:, c, :], sc(GS_BETA), None, ALU.mult, eng="pool")
                P.ts(rhsw, Ktok[:, c, :], sc(GS_BEXP), None, ALU.mult, eng="pool")
                pu = k.pf[0]
                P.mm(pu[:, 128:256], X, rhsu)
                P.mm(pu[:, 256:384], rhsw, X)
                pcopy(k, usb, pu[:, 128:256], "act")
                pcopy(k, wT, pu[:, 256:384], "dve")
                P.tt(qgT, QT[:, tsl], egr, ALU.mult, eng="pool")
                P.ts(kdec, Ktok[:, c, :], sc(GS_EDEC), None, ALU.mult, eng="pool")
                pv = k.pf[1]
                P.mm(pv[:, 256:384], wT, Sbf)
                P.tt(vnew, usb, pv[:, 256:384], ALU.subtract)
                po = k.pf[4]
                P.mm(po[:, 0:128], qgT, Sbf, start=True, stop=False)
                P.mm(po[:, 0:128], attnT, vnew, start=False, stop=True)
                if dr == 0:
                    pcopy(k, oacc[:, c, :], po[:, 0:128], "act")
                else:
                    P.tt(oacc[:, c, :], oacc[:, c, :], po[:, 0:128], ALU.add)
                ps_ = k.pf[5]
                P.mm(ps_[:, 0:128], kdec, vnew)
                P.stt(Sst, Sst, sc(GS_ETOT), ps_[:, 0:128], ALU.mult, ALU.add)
                P.copy(Sbf, Sst, eng="act")
            if is_ctx:
                P.copy(B.states[:, col, :], Sst, eng="pool")
        if states_only:
            continue
        if h == 0:
            tap(k, "oacc0", oacc)
        P.act(cs[:, 0:T].rearrange("p (a c) -> p a c", c=128), oacc, AF.Square)
        ssv = B.st[:, 16:16 + NT]
        P.reduce(ssv, cs[:, 0:T].rearrange("p (a c) -> p a c", c=128), ALU.add)
        P.ts(ssv, ssv, 1.0 / 128, EPS, ALU.mult, ALU.add)
        P.act(ssv, ssv, AF.Sqrt)
        P.add("dve", lambda e, ssv=ssv: e.reciprocal(out=ssv, in_=ssv), [ssv], [ssv])
        P.tt(oacc, oacc, ssv.unsqueeze(2).broadcast_to([128, NT, 128]), ALU.mult)
        P.tt(oacc, oacc, gnw.unsqueeze(1).broadcast_to([128, NT, 128]), ALU.mult, eng="pool")
        P.tt(yatok, oacc, zs, ALU.mult)
        for i0 in range(0, NT, 8):
            pb = k.pb[(i0 // 8) % 2]
            ni = min(8, NT - i0)
            for i in range(ni):
                P.transpose(pb[:, i * 128:(i + 1) * 128], yatok[:, i0 + i, :], identb)
            pcopy(k, yaT[:, h, i0 * 128:(i0 + ni) * 128], pb[:, 0:ni * 128])
    if is_ctx:
        tap(k, "states", B.states)


def gelu_tanh(k, out, in_, tmp, eng="dve"):
    P = k.P
    P.act(tmp, in_, AF.Square)
    P.ts(tmp, tmp, 0.044715, 1.0, ALU.mult, ALU.add, eng=eng)
    P.tt(tmp, tmp, in_, ALU.mult, eng=eng)
    P.act(tmp, tmp, AF.Sigmoid, scale=1.5957691216057308)
    P.tt(out, tmp, in_, ALU.mult, eng=eng)


def cmlp(k, l, h1T, ybT, T):
    P, B, I = k.P, k.B, k.I
    NT = T // 128
    AF_ = B.AF
    for q in range(2):
        load_w_cols(k, B.wst[q][:], l, OFF_U + q * 256, 256, ("wst", q))
    for q in range(4):
        load_w_cols(k, B.wsm[q][:], l, OFF_VG + q * 128, 128, ("wsm", q))
    sm = B.small
    lnw = sm[:, 512:1024]
    lnb = sm[:, 1024:1536]
    P.dma(lnw, I.lnw_bc[l])
    P.dma(lnb, I.lnb_bc[l])
    bs = sm[:, 176:180]
    P.dma(bs, I.bs_col[l])
    wsT = AF_[:, 8192:8192 + 512].rearrange("p (g c) -> p g c", g=4)
    P.dma(wsT, I.wsT[l].rearrange("g q p -> q g p"))
    ub = AF_[:, 0:512]
    vb = AF_[:, 512:1024]
    t1 = AF_[:, 1024:1536]
    t2 = AF_[:, 1536:2048]
    ybb = B.junk[:, 0:512]
    for i in range(NT):
        pu = k.pf[0]
        pv = k.pf[1]
        for q in range(2):
            for kk in range(KC):
                P.mm(pu[:, q * 256:(q + 1) * 256], h1T[:, kk, i * 128:(i + 1) * 128], B.wst[q][:, kk, :],
                     start=(kk == 0), stop=(kk == KC - 1))
        for q in range(4):
            for kk in range(KC):
                P.mm(pv[:, q * 128:(q + 1) * 128], h1T[:, kk, i * 128:(i + 1) * 128], B.wsm[q][:, kk, :],
                     start=(kk == 0), stop=(kk == KC - 1))
        pcopy(k, ub, pu[:, 0:512], "act")
        pcopy(k, vb, pv[:, 0:512], "dve")
        gelu_tanh(k, ub, ub, t1, eng="pool")
        gelu_tanh(k, vb, vb, t2, eng="dve")
        st = B.st[:, 32:40]
        P.reduce(st[:, 0:1], vb, ALU.add)
        P.ts(st[:, 1:2], st[:, 0:1], -1.0 / 512, None, ALU.mult)
        P.ts(vb, vb, st[:, 1:2], None, ALU.add)
        P.act(t2, vb, AF.Square, accum_out=st[:, 2:3])
        rstd_col(k, st[:, 4:5], st[:, 2:3], 512, st[:, 3:4])
        P.ts(vb, vb, st[:, 4:5], None, ALU.mult)
        P.tt(vb, vb, lnw, ALU.mult, eng="pool")
        P.tt(vb, vb, lnb, ALU.add, eng="pool")
        pm = k.pf[2]
        for g in range(4):
            P.mm(pm[:, g * 128:(g + 1) * 128], wsT[:, g, :], vb[:, g * 128:(g + 1) * 128])
        for g in range(4):
            P.stt(ybb[:, g * 128:(g + 1) * 128], pm[:, g * 128:(g + 1) * 128], bs[:, g:g + 1],
                  ub[:, g * 128:(g + 1) * 128], ALU.add, ALU.mult)
        pb = k.pb[i % 2]
        for g in range(4):
            P.transpose(pb[:, g * 128:(g + 1) * 128], ybb[:, g * 128:(g + 1) * 128], k.cb(C_ID))
        pcopy(k, ybT[:, :, i * 128:(i + 1) * 128], pb[:, 0:512].rearrange("p (g c) -> p g c", g=4))


def pool(k, l, h1T, ycT, T, is_ctx):
    P, B, I = k.P, k.B, k.I
    seg = 256 if is_ctx else 64
    nseg = T // seg
    W = seg + 32
    AF_ = B.AF
    sm = B.small
    psc = sm[:, 180:184]
    P.dma(psc, I.psc_col[l])
    pw = AF_[:, 8192:8192 + 512].rearrange("p (g c) -> p g c", g=4)
    P.dma(pw, I.pool_w[l].rearrange("g c d -> c g d"))
    pwb = B.junk[:, 0:512].rearrange("p (g c) -> p g c", g=4)
    P.copy(pwb, pw, eng="pool")
    rc0 = C_RCC if is_ctx else C_RCX
    rcs = sm[:, 512:512 + 4 * seg]
    P.dma(rcs, I.cst[:, rc0:rc0 + 4 * seg])
    bufs = [AF_[:, i * 3072:(i + 1) * 3072][:, 0:nseg * W].rearrange("p (s w) -> p s w", w=W) for i in range(2)]
    pin = AF_[:, 6144:6144 + T].rearrange("p (s w) -> p s w", w=seg)
    pooled = AF_[:, 8704:8704 + T]
    pooledb = B.AB[:, 49152 - 2048:49152 - 2048 + T]
    for g in range(4):
        def consume(j, t0, n, pap):
            pcopy(k, AF_[:, 6144 + t0:6144 + t0 + n], pap)
        w = B.wsm[g]
        load_w_cols(k, w[:], l, OFF_P + g * 128, 128, ("wsm", g))
        proj_fm(k, h1T, T, w, 128, consume)
        a, bb = bufs
        P.memset(a, 0.0)
        P.memset(bb, 0.0, eng="dve")
        P.copy(a[:, :, 16:16 + seg], pin, eng="pool")
        lo, hi = 8, seg + 24
        P.tt(bb[:, :, lo:hi], a[:, :, lo:hi], a[:, :, lo - 1:hi - 1], ALU.add)
        cur, oth = bb, a
        for lev in range(g):
            sh = 1 << lev
            P.tt(oth[:, :, lo:hi], cur[:, :, lo - sh:hi - sh], cur[:, :, lo + sh:hi + sh], ALU.add, eng="pool")
            cur, oth = oth, cur
        rc = rcs[:, g * seg:(g + 1) * seg]
        pv = pooled.rearrange("p (s w) -> p s w", w=seg)
        P.tt(pv, cur[:, :, 16:16 + seg], rc.unsqueeze(1).broadcast_to([128, nseg, seg]), ALU.mult)
        P.tt(pooledb.rearrange("p (s w) -> p s w", w=seg), pv, pin, ALU.subtract)
        for bi, (t0, n) in enumerate(tokblocks(T)):
            pf = k.pf[4 + bi % 2]
            P.mm(pf[:, 0:n], pwb[:, g, :], pooledb[:, t0:t0 + n])
            P.act(ycT[:, g, t0:t0 + n], pf[:, 0:n], AF.Identity, scale=psc[:, g:g + 1])


def bcast_row(k, out_bc, col8, dg=None):
    P = k.P
    for half in range(2):
        pf = k.pf[half]
        for kk in range(4):
            c = half * 4 + kk
            if dg is None:
                dg = k.B.AF[:, 16384 - 128:16384]
            P.ts(dg, k.c(C_ID), col8[:, c:c + 1], None, ALU.mult)
            P.mm(pf[:, kk * 128:(kk + 1) * 128], k.c(C_ONE), dg)
        pcopy(k, out_bc[:, half * 512:(half + 1) * 512], pf[:, 0:512])


def merge_out(k, b, l, v, src, dst, yaT, ybT, ycT, T):
    P, B, I, S = k.P, k.B, k.I, k.S
    NT = T // 128
    AF_ = B.AF
    mT = B.AB[:, 0:KC * T].rearrange("p (a t) -> p a t", a=KC)
    g1bc = AF_[:, 0:1024]
    bcast_row(k, g1bc, B.mod[:, l, v, 16:24])
    for dc in range(KC):
        wbr = B.wsm[dc % 2]
        wbr2 = B.wsm[2 + dc % 2]
        P.dma(wbr[:], I.w_br_a[l].rearrange("(kk p) c -> p kk c", p=128)[:, :, dc * 128:(dc + 1) * 128],
              eng="pool", semkey=("wsm", dc % 2))
        P.dma(wbr2[:, 0:4, :], I.w_br_b[l].rearrange("(kk p) c -> p kk c", p=128)[:, :, dc * 128:(dc + 1) * 128],
              eng="pool", semkey=("wsm", 2 + dc % 2))
        P.dma(wbr2[:, 4:8, :], I.w_br_c[l].rearrange("(kk p) c -> p kk c", p=128)[:, :, dc * 128:(dc + 1) * 128],
              eng="pool", semkey=("wsm", 2 + dc % 2))
        for bi, (t0, n) in enumerate(tokblocks(T)):
            gts = [AF_[:, 12288 + a * 512:12288 + a * 512 + n] for a in range(3)]
            gtb = [B.junk[:, 0:512], B.junk[:, 512:1024], B.xn[:, 0:512]]
            for a in range(3):
                P.dma(gtb[a][:, 0:n], S.gates[a * 8 + dc, :, t0:t0 + n], semkey=("gld", a))
            acc = AF_[:, 14336:14336 + n]
            for a, (yT, nk_, wsrc, koff) in enumerate(((yaT, 8, wbr, 0), (ybT, 4, wbr2, 0), (ycT, 4, wbr2, 4))):
                pf = k.pf[a]
                for kk in range(nk_):
                    P.mm(pf[:, 0:n], wsrc[:, koff + kk, :], yT[:, kk, t0:t0 + n], start=(kk == 0), stop=(kk == nk_ - 1))
                if a == 0:
                    P.tt(acc, pf[:, 0:n], gtb[a][:, 0:n], ALU.mult)
                else:
                    P.tt(gts[a], pf[:, 0:n], gtb[a][:, 0:n], ALU.mult)
                    P.tt(acc, acc, gts[a], ALU.add, eng="pool")
            P.copy(mT[:, dc, t0:t0 + n], acc, eng="act")
    wv = I.w_out[l].rearrange("(kk p) c -> p kk c", p=128)
    for q in range(4):
        wdst = B.wst[q % 2]
        cs_ = slice(q * 256, (q + 1) * 256)
        for kk in range(KC):
            stg = AF_[:, 2048 + (kk % 2) * 256:2048 + (kk % 2) * 256 + 256]
            P.dma(stg, wv[:, kk, cs_], semkey=("wo", kk % 2))
            P.tt(wdst[:, kk, :], stg, g1bc[:, cs_], ALU.mult)
        for i in range(NT):
            xq = B.xt[i % 2][:, cs_]
            P.dma(xq, src[i * 128:(i + 1) * 128, cs_], semkey=("xt", i % 2))
            pf = k.pf[4 + i % 2]
            for kk in range(KC):
                P.mm(pf[:, 0:256], mT[:, kk, i * 128:(i + 1) * 128], wdst[:, kk, :],
                     start=(kk == 0), stop=(kk == KC - 1))
            P.tt(xq, xq, pf[:, 0:256], ALU.add)
            P.dma(dst[i * 128:(i + 1) * 128, cs_], xq, semkey=("xt", i % 2))


def moe(k, b, l, src, dst, T, is_ctx):
    P, B, I = k.P, k.B, k.I
    NT = T // 128
    cap = 2 * T // NE
    v = 2 if is_ctx else b
    AB, AF_ = B.AB, B.AF
    nsh = (cap + 127) // 128
    sp_ = min(cap, 128)
    xn_tok = AB[:, 0:NT * 1024].rearrange("p (a d) -> p a d", d=1024)
    Sel = AB[:, 16384:16384 + NT * cap].rearrange("p (a s) -> p a s", s=cap)
    SelT = AB[:, 20480:20480 + nsh * T].rearrange("p (a t) -> p a t", a=nsh)
    xeT = AB[:, 24576:24576 + KC * cap].rearrange("p (a s) -> p a s", a=KC)
    hT = AB[:, 26624:26624 + KC * cap].rearrange("p (a s) -> p a s", a=KC)
    ye = AB[:, 28672:28672 + nsh * 1024].rearrange("p (a d) -> p a d", a=nsh)
    wring = [AB[:, 30720 + i * 2048:30720 + (i + 1) * 2048] for i in range(8)]
    ymoe = AF_[:, 0:NT * 1024].rearrange("p (a d) -> p a d", d=1024)
    sm = B.small
    A2 = B.modA[:, l, v, 1, :]
    sh2 = B.mod[:, l, v, 24:32]
    ident, identb = k.c(C_ID), k.cb(C_ID)
    wr = AF_[:, 7168:7296].rearrange("p (a e) -> p a e", a=KC)
    P.dma(wr, I.w_router[l].rearrange("(kk p) e -> p kk e", p=128))
    wrs = AF_[:, 7296:7424].rearrange("p (a e) -> p a e", a=KC)
    P.tt(wrs, wr, A2.unsqueeze(2).broadcast_to([128, KC, NE]), ALU.mult)
    pbias = k.pf[5]
    rep = AF_[:, 7424:7552]
    for kk in range(KC):
        P.ts(rep, k.c(C_ONE), sh2[:, kk:kk + 1], None, ALU.mult)
        P.mm(pbias[:, 0:NE], rep, wr[:, kk, :], start=(kk == 0), stop=(kk == KC - 1))
    rbias = AF_[:, 7552:7568]
    P.copy(rbias, pbias[:, 0:NE])
    afft = sm[:, 1024:1024 + NT * NE].rearrange("p (a e) -> p a e", e=NE)
    maskf = sm[:, 1280:1280 + NT * NE].rearrange("p (a e) -> p a e", e=NE)
    pref = sm[:, 1536:1536 + NT * NE].rearrange("p (a e) -> p a e", e=NE)
    twc = sm[:, 1792:1800]
    xT = AF_[:, 2048:3072].rearrange("p (a c) -> p a c", a=KC)
    xnf = B.xt[0]
    for i in range(NT):
        xt = B.xt[1]
        P.dma(xt[:], src[i * 128:(i + 1) * 128, :], semkey=("xt", 1))
        st = B.st[:, 40:48]
        P.act(B.junk[:], xt[:], AF.Square, accum_out=st[:, 0:1])
        rstd_col(k, st[:, 2:3], st[:, 0:1], D, st[:, 1:2])
        P.ts(xnf[:], xt[:], st[:, 2:3], None, ALU.mult)
        P.copy(xn_tok[:, i, :], xnf[:], eng="pool")
        for half in range(2):
            pt = k.pf[half]
            for kk in range(4):
                c = half * 4 + kk
                P.transpose(pt[:, kk * 128:(kk + 1) * 128], xnf[:, c * 128:(c + 1) * 128], ident)
            pcopy(k, xT[:, half * 4:half * 4 + 4, :], pt[:, 0:512].rearrange("p (a c) -> p a c", a=4))
        plog = k.pf[4]
        for kk in range(KC):
            P.mm(plog[:, 0:NE], xT[:, kk, :], wrs[:, kk, :], start=(kk == 0), stop=(kk == KC - 1))
        lg = B.st[:, 48:64]
        P.tt(lg, plog[:, 0:NE], rbias, ALU.add)
        P.reduce(st[:, 3:4], lg, ALU.max)
        P.ts(st[:, 4:5], st[:, 3:4], -1.0, None, ALU.mult)
        P.act(lg, lg, AF.Exp, bias=st[:, 4:5], accum_out=st[:, 5:6])
        P.add("dve", lambda e, st=st: e.reciprocal(out=st[:, 6:7], in_=st[:, 5:6]), [st[:, 5:6]], [st[:, 6:7]])
        P.ts(afft[:, i, :], lg, st[:, 6:7], None, ALU.mult)
    tap(k, "aff", afft)
    affE = AF_[0:NE, 3072:3072 + T]
    wk = AF_[0:NE, 5120:5120 + T]
    for i0 in range(0, NT, 4):
        pt = k.pf[0]
        ni = min(4, NT - i0)
        for i in range(ni):
            P.transpose(pt[0:NE, i * 128:(i + 1) * 128], afft[:, i0 + i, :], ident)
        pcopy(k, affE[:, i0 * 128:(i0 + ni) * 128], pt[0:NE, 0:ni * 128])
    m8 = B.st[0:NE, 8:16]
    for r in range(cap // 8):
        srcv = affE if r == 0 else wk
        P.add("dve", lambda e, srcv=srcv: e.max(out=m8, in_=srcv), [srcv], [m8])
        P.add("dve", lambda e, srcv=srcv: e.match_replace(out=wk, in_to_replace=m8, in_values=srcv, imm_value=-1.0),
              [m8, srcv], [wk])
    maskE = B.wst[0][:].rearrange("p a c -> p (a c)")[0:NE, 0:T]
    P.tt(maskE, affE, wk, ALU.not_equal)
    pm = k.pb[0]
    for i in range(NT):
        P.transpose(pm[:, i * NE:(i + 1) * NE], maskE[:, i * 128:(i + 1) * 128], identb[0:NE, 0:NE])
    maskb = B.junk[:, 0:NT * NE].rearrange("p (a e) -> p a e", e=NE)
    pcopy(k, maskb, pm[:, 0:NT * NE].rearrange("p (a e) -> p a e", e=NE), "act")
    pcopy(k, maskf, pm[:, 0:NT * NE].rearrange("p (a e) -> p a e", e=NE), "dve")
    tap(k, "mask", maskf)
    hl = B.junk[:, 256:256 + NT * NE * 2].rearrange("p (a e h) -> p a e h", e=NE, h=2)
    P.copy(hl[:, :, :, 0], afft)
    hif = B.st[:, 48:64]
    for i in range(NT):
        P.copy(hif, hl[:, i, :, 0], eng="pool")
        P.tt(hl[:, i, :, 1], afft[:, i, :], hif, ALU.subtract, eng="pool")
    for i in range(NT):
        pp = k.pf[1]
        for i2 in range(i + 1):
            P.mm(pp[:, 0:NE], k.cb(C_U) if i2 == i else k.cb(C_ONE), maskb[:, i2, :],
                 start=(i2 == 0), stop=(i2 == i))
        pcopy(k, pref[:, i, :], pp[:, 0:NE], "dve")
    g2bc = None
    for e in range(NE):
        for i in range(NT):
            P.ts(Sel[:, i, :], k.c(C_IOTA, cap), pref[:, i, e:e + 1], maskf[:, i, e:e + 1],
                 ALU.is_equal, ALU.mult)
        for hh in range(nsh):
            for i0 in range(0, NT, 8):
                pb = k.pb[(i0 // 8) % 2]
                ni = min(8, NT - i0)
                for i in range(ni):
                    P.transpose(pb[0:sp_, i * 128:(i + 1) * 128], Sel[:, i0 + i, hh * 128:hh * 128 + sp_], identb)
                pcopy(k, SelT[0:sp_, hh, i0 * 128:(i0 + ni) * 128], pb[0:sp_, 0:ni * 128])
        for kk in range(KC):
            pg = k.pf[kk % 2]
            for i in range(NT):
                P.mm(pg[:, 0:cap], xn_tok[:, i, kk * 128:(kk + 1) * 128], Sel[:, i, :],
                     start=(i == 0), stop=(i == NT - 1))
            P.act(xeT[:, kk, :], pg[:, 0:cap], AF.Identity, scale=A2[:, kk:kk + 1], bias=sh2[:, kk:kk + 1])
        for hh in range(nsh):
            pw_ = k.pf[2]
            for i in range(NT):
                P.mm(pw_[0:sp_, hh * 2:hh * 2 + 2], Sel[:, i, hh * 128:hh * 128 + sp_], hl[:, i, e, :],
                     start=(i == 0), stop=(i == NT - 1))
            P.reduce(twc[0:sp_, hh:hh + 1], pw_[0:sp_, hh * 2:hh * 2 + 2], ALU.add)
        wg_v = I.w_gate[l, e].rearrange("(kk p) f -> p kk f", p=128)
        wu_v = I.w_up[l, e].rearrange("(kk p) f -> p kk f", p=128)
        wd_v = I.w_down[l, e].rearrange("(m p) d -> p m d", p=128)
        for m2 in range(4):
            gp = wring[(m2 % 2) * 2].rearrange("p (a c) -> p a c", a=KC)
            up = wring[(m2 % 2) * 2 + 1].rearrange("p (a c) -> p a c", a=KC)
            P.dma(gp, wg_v[:, :, m2 * 256:(m2 + 1) * 256], eng="pool", semkey=("wr", (m2 % 2) * 2))
            P.dma(up, wu_v[:, :, m2 * 256:(m2 + 1) * 256], eng="pool", semkey=("wr", (m2 % 2) * 2 + 1))
            for mm_ in range(2):
                m = m2 * 2 + mm_
                pg = k.pf[0]
                pu = k.pf[1]
                for kk in range(KC):
                    P.mm(pg[:, 0:cap], gp[:, kk, mm_ * 128:(mm_ + 1) * 128], xeT[:, kk, :],
                         start=(kk == 0), stop=(kk == KC - 1))
                for kk in range(KC):
                    P.mm(pu[:, 0:cap], up[:, kk, mm_ * 128:(mm_ + 1) * 128], xeT[:, kk, :],
                         start=(kk == 0), stop=(kk == KC - 1))
                sg = sm[:, (m % 2) * 256:(m % 2) * 256 + cap]
                P.act(sg, pg[:, 0:cap], AF.Silu)
                P.tt(hT[:, m, :], sg, pu[:, 0:cap], ALU.mult)
        for m2 in range(4):
            dp = wring[4 + m2].rearrange("p (a d) -> p a d", a=2)
            P.dma(dp, wd_v[:, m2 * 2:m2 * 2 + 2, :], eng="pool", semkey=("wr", 4 + m2))
            for mm_ in range(2):
                m = m2 * 2 + mm_
                for hh in range(nsh):
                    for dh in range(2):
                        pd = k.pf[2 + hh * 2 + dh]
                        P.mm(pd[0:sp_, 0:512], hT[:, m, hh * 128:hh * 128 + sp_], dp[:, mm_, dh * 512:(dh + 1) * 512],
                             start=(m == 0), stop=(m == KC - 1))
        for hh in range(nsh):
            for dh in range(2):
                pd = k.pf[2 + hh * 2 + dh]
                P.act(ye[0:sp_, hh, dh * 512:(dh + 1) * 512], pd[0:sp_, 0:512], AF.Identity,
                      scale=twc[0:sp_, hh:hh + 1])
        for i in range(NT):
            for dh in range(2):
                pc = k.pf[(i * 2 + dh) % 2]
                for hh in range(nsh):
                    P.mm(pc[:, 0:512], SelT[0:sp_, hh, i * 128:(i + 1) * 128], ye[0:sp_, hh, dh * 512:(dh + 1) * 512],
                         start=(hh == 0), stop=(hh == nsh - 1))
                dsty = ymoe[:, i, dh * 512:(dh + 1) * 512]
                if e == 0:
                    pcopy(k, dsty, pc[:, 0:512])
                else:
                    P.tt(dsty, dsty, pc[:, 0:512], ALU.add)
    g2bc = sm[:, 0:1024]
    bcast_row(k, g2bc, B.mod[:, l, v, 40:48], dg=sm[:, 1920:2048])
    for i in range(NT):
        xt = B.xt[i % 2]
        P.dma(xt[:], src[i * 128:(i + 1) * 128, :], semkey=("xt", i % 2))
        P.tt(ymoe[:, i, :], ymoe[:, i, :], g2bc, ALU.mult, eng="pool")
        P.tt(xt[:], xt[:], ymoe[:, i, :], ALU.add)
        P.dma(dst[i * 128:(i + 1) * 128, :], xt[:], semkey=("xt", i % 2))


def final_norm(k, b):
    P, B, I, S = k.P, k.B, k.I, k.S
    fw = B.AF[:, 0:1024]
    P.dma(fw, I.fnw_bc)
    for i in range(TX // 128):
        xt = B.xt[i % 2]
        P.dma(xt[:], S.xcur[b][i * 128:(i + 1) * 128, :], semkey=("xt", i % 2))
        st = B.st[:, (i % 2) * 4:(i % 2) * 4 + 4]
        P.act(B.junk[:], xt[:], AF.Square, accum_out=st[:, 0:1])
        rstd_col(k, st[:, 2:3], st[:, 0:1], D, st[:, 1:2])
        P.stt(xt[:], xt[:], st[:, 2:3], fw, ALU.mult, ALU.mult)
        k.fin.append(P.dma(k.out[b][i * 128:(i + 1) * 128, :], xt[:], semkey=("xt", i % 2)))


def dump_dram(k, name, src, T):
    if name not in k.cfg.get("taps", ()):
        return
    o = k.dout("tap_" + name, [T, D])
    for i in range(T // 128):
        xt = k.B.xt[i % 2]
        k.P.dma(xt[:], src[i * 128:(i + 1) * 128, :], semkey=("xt", i % 2))
        k.fin.append(k.P.dma(o[i * 128:(i + 1) * 128, :], xt[:], semkey=("xt", i % 2)))


def col_layout(v, kk):
    return np.ascontiguousarray(np.asarray(v, np.float32).reshape(kk, 128).T)


def bc_layout(v):
    v = np.asarray(v, np.float32).reshape(1, -1)
    return np.ascontiguousarray(np.broadcast_to(v, (128, v.shape[1])))


def shared_inputs(inp):
    f = lambda a: np.ascontiguousarray(np.asarray(a, np.float32))
    sh = {}
    sh["w_ada"] = f(inp["w_ada"])
    sh["b_ada_col"] = np.stack([col_layout(inp["b_ada"][l], 48) for l in range(NL)])
    sh["n1col"] = np.stack([col_layout(inp["norm1_w"][l], KC) for l in range(NL)])
    sh["n2col"] = np.stack([col_layout(inp["norm2_w"][l], KC) for l in range(NL)])
    sh["fnw_bc"] = bc_layout(inp["final_norm_w"])
    sh["w_in"] = f(inp["w_in"])
    cw = np.asarray(inp["qkv_conv_w"], np.float32)
    sh["convw"] = np.ascontiguousarray(cw.reshape(NL, 5, 24, 128).transpose(0, 3, 2, 1))
    sh["alog_bc"] = np.stack([bc_layout(inp["gdn_a_log"][l].reshape(-1)) for l in range(NL)])
    sh["dtb_bc"] = np.stack([bc_layout(inp["gdn_dt_bias"][l].reshape(-1)) for l in range(NL)])
    sh["gnw_bc"] = np.stack([bc_layout(inp["gdn_norm_w"][l]) for l in range(NL)])
    sh["lnw_bc"] = np.stack([bc_layout(inp["cmlp_ln_w"][l]) for l in range(NL)])
    sh["lnb_bc"] = np.stack([bc_layout(inp["cmlp_ln_b"][l]) for l in range(NL)])
    sh["wsT"] = np.ascontiguousarray(np.asarray(inp["cmlp_w_s"], np.float32).transpose(0, 1, 3, 2))
    sh["bs_col"] = np.ascontiguousarray(np.asarray(inp["cmlp_b_s"], np.float32).transpose(0, 2, 1))
    sh["pool_w"] = f(inp["pool_w"])
    sh["psc_col"] = np.stack([col_layout(inp["pool_scale"][l], 4) for l in range(NL)])
    for nm in ("w_br_a", "w_br_b", "w_br_c", "w_out", "w_router", "w_gate", "w_up", "w_down"):
        sh[nm] = f(inp[nm])
    sh["cst"] = make_consts()
    return sh


def core_inputs(inp, sh, core):
    m = dict(sh)
    b0 = 2 * core
    m["x"] = np.ascontiguousarray(np.asarray(inp["x"][b0:b0 + 2], np.float32))
    m["ctx"] = np.ascontiguousarray(np.asarray(inp["ctx"][b0:b0 + 2], np.float32))
    cc = np.stack([np.asarray(inp["c"][b0], np.float32), np.asarray(inp["c"][b0 + 1], np.float32),
                   np.asarray(inp["c_ctx"], np.float32)], axis=-1)
    m["ccol"] = np.ascontiguousarray(cc.reshape(KC, 128, 3).transpose(1, 0, 2))
    return m


_CACHE = {}


def kernel(**inputs):
    n = 8
    if "k" not in _CACHE:
        _CACHE["k"] = build({"batches": [0, 1], "layers": [0, 1], "streams": ["ctx", "x"],
                             "moe": True, "final": True, "taps": []})
    k = _CACHE["k"]
    sh = shared_inputs(inputs)
    in_maps = [core_inputs(inputs, sh, c) for c in range(n)]
    res = run_bass_kernel_spmd(k.nc, in_maps, core_ids=list(range(n)))
    out = np.concatenate([np.asarray(r["out"], np.float32) for r in res.results], axis=0)
    return out
```

```python
import numpy as np
import concourse.bass as bass
import concourse.mybir as mybir
from concourse.bass_utils import run_bass_kernel_spmd

F32 = mybir.dt.float32
BF16 = mybir.dt.bfloat16
AF = mybir.ActivationFunctionType
ALU = mybir.AluOpType
AX = mybir.AxisListType

ENGS = ("pe", "act", "dve", "pool", "sp")


def _box(ap):
    t = ap.tensor
    name = t.name
    space = str(ap.space)
    dims = ap.ap
    off = ap.offset
    if space == "DRAM":
        lo = off
        hi = off + sum((c - 1) * abs(s) for s, c in dims) + 1
        return (name, 0, 1, lo, hi)
    pstride = dims[0][0]
    if pstride == 0:
        pstride = 1 << 40
    tshape = t.shape
    fsz = 1
    for s in list(tshape)[1:]:
        fsz *= s
    p0 = off // fsz
    f0 = off % fsz
    npart = dims[0][1]
    f1 = f0 + sum((c - 1) * abs(s) for s, c in dims[1:]) + 1
    return (name, p0, p0 + npart, f0, f1)


def _overlap(a, b):
    return a[1] < b[2] and b[1] < a[2] and a[3] < b[4] and b[3] < a[4]


def _contains(a, b):
    return a[1] <= b[1] and b[2] <= a[2] and a[3] <= b[3] and b[4] <= a[4]


class Op:
    __slots__ = ("eng", "fn", "idx", "deps", "is_dma", "semkey", "has_dep", "tick",
                 "group", "pos")

    def __init__(self, eng, fn, is_dma=False, semkey=None):
        self.eng = eng
        self.fn = fn
        self.is_dma = is_dma
        self.semkey = semkey
        self.deps = set()
        self.has_dep = False
        self.tick = None
        self.group = None


class Prog:
    def __init__(self, nc, n_dma_sems=80):
        self.nc = nc
        self.ops = []
        self.acc = {}
        self.n_dma_sems = n_dma_sems
        self.same_engine_sync = True

    def _track(self, op, reads, writes):
        ekey = ("dma", op.semkey) if op.is_dma else op.eng
        preads = [ap for ap in reads if str(ap.space) == "PSUM"]
        reads = [ap for ap in reads if str(ap.space) != "PSUM"]
        writes = list(writes) + preads
        for ap in reads:
            b = _box(ap)
            ent = self.acc.setdefault(b[0], {"w": [], "r": {}})
            for (ob, oop) in ent["w"]:
                if _overlap(ob, b):
                    op.deps.add(oop)
            ent["r"][(b, ekey)] = op
        for ap in writes:
            b = _box(ap)
            if str(ap.space) == "PSUM":
                b = (b[0], 0, 128, 0, 1 << 30)
            ent = self.acc.setdefault(b[0], {"w": [], "r": {}})
            keep = []
            for (ob, oop) in ent["w"]:
                if oop is op:
                    keep.append((ob, oop))
                    continue
                if _overlap(ob, b):
                    op.deps.add(oop)
                    if _contains(b, ob):
                        continue
                keep.append((ob, oop))
            keep.append((b, op))
            ent["w"] = keep
            rk = {}
            for (ob, ek), oop in ent["r"].items():
                if oop is op:
                    rk[(ob, ek)] = oop
                    continue
                if _overlap(ob, b):
                    op.deps.add(oop)
                    if _contains(b, ob):
                        continue
                rk[(ob, ek)] = oop
            ent["r"] = rk
        op.deps.discard(op)

    def add(self, eng, fn, reads=(), writes=()):
        op = Op(eng, fn)
        op.idx = len(self.ops)
        self.ops.append(op)
        self._track(op, reads, writes)
        return op

    def dma(self, out, in_, eng="sp", semkey=None, **kw):
        sb = out if str(out.space) != "DRAM" else in_
        if semkey is None:
            semkey = sb.tensor.name
        op = Op(eng, lambda e, out=out, in_=in_, kw=kw: e.dma_start(out=out, in_=in_, **kw),
                is_dma=True, semkey=semkey)
        op.idx = len(self.ops)
        self.ops.append(op)
        self._track(op, [in_], [out])
        return op

    def mm(self, out, lhsT, rhs, start=True, stop=True, **kw):
        reads = [lhsT, rhs] + ([] if start else [out])
        return self.add("pe", lambda e: e.matmul(out, lhsT, rhs, start=start, stop=stop, **kw),
                        reads, [out])

    def transpose(self, out, in_, ident):
        return self.add("pe", lambda e: e.transpose(out, in_, ident), [in_, ident], [out])

    def act(self, out, in_, func, bias=None, scale=1.0, accum_out=None, eng="act"):
        reads = [in_]
        if bias is not None and not isinstance(bias, (int, float)):
            reads.append(bias)
        if not isinstance(scale, (int, float)):
            reads.append(scale)
        writes = [out] + ([accum_out] if accum_out is not None else [])
        kw = {}
        if accum_out is not None:
            kw["accum_out"] = accum_out
        if bias is not None:
            kw["bias"] = bias
        return self.add(eng, lambda e: e.activation(out=out, in_=in_, func=func, scale=scale, **kw),
                        reads, writes)

    def tt(self, out, in0, in1, op, eng="dve"):
        return self.add(eng, lambda e: e.tensor_tensor(out=out, in0=in0, in1=in1, op=op),
                        [in0, in1], [out])

    def ts(self, out, in0, s1, s2, op0, op1=None, eng="dve", accum_out=None):
        reads = [in0] + [s for s in (s1, s2) if s is not None and not isinstance(s, (int, float))]
        writes = [out] + ([accum_out] if accum_out is not None else [])
        kw = {}
        if op1 is not None:
            kw["op1"] = op1
        if accum_out is not None:
            kw["accum_out"] = accum_out
        return self.add(eng, lambda e: e.tensor_scalar(out=out, in0=in0, scalar1=s1, scalar2=s2,
                                                       op0=op0, **kw), reads, writes)

    def stt(self, out, in0, scalar, in1, op0, op1, eng="dve", accum_out=None):
        reads = [in0, in1] + ([scalar] if not isinstance(scalar, (int, float)) else [])
        writes = [out] + ([accum_out] if accum_out is not None else [])
        kw = {}
        if eng == "pool":
            eng = "dve"
        if accum_out is not None:
            kw["accum_out"] = accum_out
        return self.add(eng, lambda e: e.scalar_tensor_tensor(out=out, in0=in0, scalar=scalar, in1=in1,
                                                              op0=op0, op1=op1, **kw), reads, writes)

    def copy(self, out, in_, eng="dve"):
        if eng == "act":
            return self.add("act", lambda e: e.copy(out=out, in_=in_), [in_], [out])
        return self.add(eng, lambda e: e.tensor_copy(out=out, in_=in_), [in_], [out])

    def memset(self, ap, val, eng="pool"):
        return self.add(eng, lambda e: e.memset(ap, val), [], [ap])

    def reduce(self, out, in_, op, axis=None, eng="dve"):
        axis = axis or AX.X
        return self.add(eng, lambda e: e.tensor_reduce(out=out, in_=in_, axis=axis, op=op), [in_], [out])

    def emit(self, final_wait_ops=()):
        nc = self.nc
        ops = self.ops
        streams = {e: [] for e in ENGS}
        for op in ops:
            op.pos = len(streams[op.eng])
            streams[op.eng].append(op)
        for op in ops:
            red = {}
            dm = []
            for d in op.deps:
                if d.is_dma:
                    dm.append(d)
                else:
                    if d.eng == op.eng and (d.eng == "pe" or not self.same_engine_sync) and not op.is_dma:
                        continue
                    if d.eng not in red or red[d.eng].idx < d.idx:
                        red[d.eng] = d
            op.deps = list(red.values()) + dm
            for d in op.deps:
                d.has_dep = True
        for op in final_wait_ops:
            op.has_dep = True
        LIM = 30000
        cnt = {e: 0 for e in ENGS}
        for op in ops:
            if not op.is_dma and op.has_dep:
                cnt[op.eng] += 1
                op.tick = ((cnt[op.eng] - 1) // LIM, (cnt[op.eng] - 1) % LIM + 1)
        keys = []
        for op in ops:
            if op.is_dma and op.semkey not in keys:
                keys.append(op.semkey)
        nsem = min(len(keys), self.n_dma_sems)
        key2sem = {k: i % max(nsem, 1) for i, k in enumerate(keys)}
        semstate = {}
        dma_wait_prev = {}
        consumers = {}
        for op in ops:
            for d in op.deps:
                if d.is_dma:
                    consumers.setdefault(d, []).append(op)
        first_consumer_idx = {}
        for d, cl in consumers.items():
            first_consumer_idx[d] = min(c.idx for c in cl)
        for op in final_wait_ops:
            if op.is_dma:
                first_consumer_idx.setdefault(op, len(ops))
        groups = []
        for op in ops:
            if not op.is_dma:
                continue
            s = key2sem[op.semkey]
            st = semstate.setdefault(s, {"total": 0, "open": None, "close_at": None, "prev_final": 0})
            g = st["open"]
            if g is not None and st["close_at"] is not None and st["close_at"] <= op.idx:
                st["prev_final"] = g["final"]
                g = None
            if g is None:
                g = {"sem": s, "members": [], "final": st["total"]}
                groups.append(g)
                st["open"] = g
                st["close_at"] = None
                if st["prev_final"] > 0:
                    dma_wait_prev[op] = (s, st["prev_final"])
            st["total"] += 16
            g["members"].append(op)
            g["final"] = st["total"]
            op.group = g
            fc = first_consumer_idx.get(op)
            if fc is not None:
                st["close_at"] = fc if st["close_at"] is None else min(st["close_at"], fc)
        self.stats = {"n_ops": len(ops), "n_dma_sems": nsem, "incs": dict(cnt)}
        from contextlib import ExitStack
        es = ExitStack()
        esem = {}
        for e in ENGS:
            if e == "sp":
                continue
            for gen in range((cnt[e] + LIM - 1) // LIM + 1):
                esem[(e, gen)] = es.enter_context(nc.semaphore("c_%s%d" % (e, gen)))
        dsem = [es.enter_context(nc.semaphore("d_%d" % i)) for i in range(nsem)]
        known = {e: {} for e in ENGS}
        nwaits = 0

        def emit_stream(ename, eobj):
            nonlocal nwaits
            kn = known[ename]
            for op in streams[ename]:
                waits = {}
                for d in op.deps:
                    if d.is_dma:
                        g = d.group
                        key = ("d", g["sem"])
                        val = g["final"]
                    else:
                        key = ("e", d.eng, d.tick[0])
                        val = d.tick[1]
                    if waits.get(key, 0) < val:
                        waits[key] = val
                if op in dma_wait_prev:
                    s, v = dma_wait_prev[op]
                    key = ("d", s)
                    if waits.get(key, 0) < v:
                        waits[key] = v
                for key, val in waits.items():
                    if kn.get(key, 0) >= val:
                        continue
                    kn[key] = val
                    sem = dsem[key[1]] if key[0] == "d" else esem[(key[1], key[2])]
                    eobj.wait_ge(sem, val)
                    nwaits += 1
                ins = op.fn(eobj)
                if op.is_dma:
                    ins.then_inc(dsem[op.group["sem"]], 16)
                elif op.has_dep:
                    ins.then_inc(esem[(ename, op.tick[0])], 1)
            if ename == "sp":
                for op in final_wait_ops:
                    g = op.group
                    eobj.wait_ge(dsem[g["sem"]], g["final"])

        with nc.Block() as block:
            @block.tensor
            def _(e):
                emit_stream("pe", e)

            @block.scalar
            def _(e):
                emit_stream("act", e)

            @block.vector
            def _(e):
                emit_stream("dve", e)

            @block.gpsimd
            def _(e):
                emit_stream("pool", e)

            @block.sync
            def _(e):
                emit_stream("sp", e)
        self.stats["nwaits"] = nwaits
        es.close()


from contextlib import ExitStack

D = 1024
KC = 8
H = 8
NL = 2
OFF_Q, OFF_K, OFF_V, OFF_Z, OFF_B, OFF_A, OFF_U, OFF_VG, OFF_P, OFF_G = (
    0, 1024, 2048, 3072, 4096, 4112, 4128, 4640, 5152, 5664)
IN_COLS = 8736
EPS = 1e-6
NEG = -30000.0
TX = 2048
TC = 256
NE = 16

C_ID, C_U, C_L, C_NMF, C_NMB, C_SL, C_SU, C_ONE = [i * 128 for i in range(8)]
C_IOTA = 1024
C_RCX = 1280
C_RCC = 1536
NCST = 2560


def make_consts():
    c = np.zeros((128, NCST), np.float32)
    i = np.arange(128)[:, None]
    j = np.arange(128)[None, :]
    c[:, C_ID:C_ID + 128] = (i == j)
    c[:, C_U:C_U + 128] = (i <= j)
    c[:, C_L:C_L + 128] = (i >= j)
    c[:, C_NMF:C_NMF + 128] = np.where(i >= j, 0.0, NEG)
    c[:, C_NMB:C_NMB + 128] = np.where(i <= j, 0.0, NEG)
    c[:, C_SL:C_SL + 128] = (i > j)
    c[:, C_SU:C_SU + 128] = (i < j)
    c[:, C_ONE:C_ONE + 128] = 1.0
    c[:, C_IOTA:C_IOTA + 256] = np.arange(1, 257)[None, :]
    for seg, off in ((64, C_RCX), (256, C_RCC)):
        t = np.arange(seg)
        for gi, w in enumerate((2, 4, 8, 16)):
            lo = np.clip(t - w // 2, 0, seg)
            hi = np.clip(t + w // 2, 0, seg)
            c[:, off + gi * seg: off + (gi + 1) * seg] = (1.0 / (hi - lo).astype(np.float32))[None, :]
    return c


def tokblocks(T):
    bs = min(512, T)
    return [(s, bs) for s in range(0, T, bs)]


class K:
    pass


def build(cfg):
    nc = bass.Bass("TRN2", target_bir_lowering=False)
    k = K()
    k.nc = nc
    k.cfg = cfg
    P = Prog(nc)
    k.P = P
    es = ExitStack()
    k.fin = []
    k.dbg = {}

    def din(name, shape, dt=F32):
        return nc.dram_tensor(name, list(shape), dt, kind="ExternalInput").ap()

    def dscr(name, shape, dt=F32):
        return nc.dram_tensor(name, list(shape), dt).ap()

    def dout(name, shape, dt=F32):
        return nc.dram_tensor(name, list(shape), dt, kind="ExternalOutput").ap()

    def sb(name, shape, dt=F32):
        return es.enter_context(nc.sbuf_tensor(name, list(shape), dt))

    def ps(name, shape, dt=F32):
        return es.enter_context(nc.psum_tensor(name, list(shape), dt))

    k.dout = dout
    I = K()
    k.I = I
    I.x = din("x", [2, TX, D])
    I.ctx = din("ctx", [2, TC, D])
    I.ccol = din("ccol", [128, KC, 3])
    I.w_ada = din("w_ada", [NL, D, 6 * D])
    I.b_ada_col = din("b_ada_col", [NL, 128, 48])
    I.n1col = din("n1col", [NL, 128, KC])
    I.n2col = din("n2col", [NL, 128, KC])
    I.fnw_bc = din("fnw_bc", [128, D])
    I.w_in = din("w_in", [NL, D, IN_COLS])
    I.convw = din("convw", [NL, 128, 24, 5])
    I.alog_bc = din("alog_bc", [NL, 128, 16])
    I.dtb_bc = din("dtb_bc", [NL, 128, 16])
    I.gnw_bc = din("gnw_bc", [NL, 128, 128])
    I.lnw_bc = din("lnw_bc", [NL, 128, 512])
    I.lnb_bc = din("lnb_bc", [NL, 128, 512])
    I.wsT = din("wsT", [NL, 4, 128, 128])
    I.bs_col = din("bs_col", [NL, 128, 4])
    I.pool_w = din("pool_w", [NL, 4, 128, 128])
    I.psc_col = din("psc_col", [NL, 128, 4])
    I.w_br_a = din("w_br_a", [NL, 1024, D])
    I.w_br_b = din("w_br_b", [NL, 512, D])
    I.w_br_c = din("w_br_c", [NL, 512, D])
    I.w_out = din("w_out", [NL, D, D])
    I.w_router = din("w_router", [NL, D, NE])
    I.w_gate = din("w_gate", [NL, NE, D, D])
    I.w_up = din("w_up", [NL, NE, D, D])
    I.w_down = din("w_down", [NL, NE, D, D])
    I.cst = din("cst", [128, NCST])
    k.out = dout("out", [2, TX, D])
    S = K()
    k.S = S
    S.xcur = dscr("xcur", [2, TX, D])
    S.ccur = dscr("ccur", [2, TC, D])
    S.gates = dscr("gatesD", [24, 128, TX], BF16)
    B = K()
    k.B = B
    B.cst = sb("cst_sb", [128, 1280])
    B.cstb = sb("cstb", [128, 1024 + 256], BF16)
    B.AB = sb("AB", [128, 49152], BF16)
    B.AF = sb("AF", [128, 16384], F32)
    B.xt = [sb("xt%d" % i, [128, D]) for i in range(2)]
    B.xn = sb("xn", [128, D], BF16)
    B.st = sb("st", [128, 64])
    B.wst = [sb("wst%d" % i, [128, KC, 256], BF16) for i in range(2)]
    B.wsm = [sb("wsm%d" % i, [128, KC, 128], BF16) for i in range(4)]
    B.mod = sb("mod", [128, NL, 3, 48])
    B.modA = sb("modA", [128, NL, 3, 2, KC])
    B.sc3 = sb("sc3", [128, KC, 3])
    B.small = sb("small", [128, 2048])
    B.states = B.AF[:, 10240:12288].rearrange("p (a c) -> p a c", c=128)
    B.gsc = B.AF[:, 12288:14592].rearrange("p (a b c) -> p a b c", a=9, b=16)
    B.junk = sb("junk", [128, D], BF16)
    k.pf = [ps("pf%d" % i, [128, 512]) for i in range(6)]
    k.pb = [ps("pb%d" % i, [128, 1024], BF16) for i in range(2)]

    def cst(off, n=128):
        return B.cst[:, off:off + n]

    def cstb(off, n=128):
        return B.cstb[:, off:off + n]

    k.c = cst
    k.cb = cstb
    P.dma(B.cst[:], I.cst[:, 0:1280])
    P.copy(B.cstb[:, 0:1024], B.cst[:, 0:1024])
    P.copy(B.cstb[:, 1024:1280], B.cst[:, C_IOTA:C_IOTA + 256])
    k.rr = [0]

    prologue(k)
    for b in cfg["batches"]:
        for l in cfg["layers"]:
            last = l == NL - 1
            src_c = I.ctx[b] if l == 0 else S.ccur[b]
            src_x = I.x[b] if l == 0 else S.xcur[b]
            if "ctx" in cfg["streams"]:
                mixer(k, b, l, src_c, S.ccur[b], TC, True, last)
                if not last and cfg.get("moe", True):
                    moe(k, b, l, S.ccur[b], S.ccur[b], TC, True)
                if not last:
                    dump_dram(k, "ccur", S.ccur[b], TC)
            if "x" in cfg["streams"]:
                mixer(k, b, l, src_x, S.xcur[b], TX, False, False)
                if cfg.get("moe", True):
                    moe(k, b, l, S.xcur[b], S.xcur[b], TX, False)
                dump_dram(k, "xcur%d" % l, S.xcur[b], TX)
        if cfg.get("final", True):
            final_norm(k, b)
    P.emit(final_wait_ops=k.fin)
    es.close()
    k.stats = P.stats
    return k


def tap(k, name, ap_sb, shape=None, dt=F32):
    if name not in k.cfg.get("taps", ()):
        return
    shp = list(ap_sb.shape)
    o = k.dout("tap_" + name, shp, ap_sb.dtype)
    k.fin.append(k.P.dma(o, ap_sb, semkey="tap"))


def evac_eng(k):
    k.rr[0] += 1
    return "act" if k.rr[0] % 2 else "dve"


def pcopy(k, out, in_, eng=None):
    eng = eng or evac_eng(k)
    k.P.copy(out, in_, eng=eng)


def prologue(k):
    P, B, I = k.P, k.B, k.I
    raw = B.small[:, 0:24].rearrange("p (a b) -> p a b", a=KC)
    P.dma(raw, I.ccol)
    P.act(B.sc3[:], raw, AF.Silu)
    for l in range(NL):
        wv = I.w_ada[l].rearrange("(kk p) c -> p kk c", p=128)
        acc = k.pf[0][:, 0:144].rearrange("p (j v) -> p j v", v=3)
        for cb in range(12):
            wblk = B.AF[:, (cb % 2) * 4096:(cb % 2) * 4096 + 4096].rearrange("p (a b) -> p a b", a=KC)
            P.dma(wblk, wv[:, :, cb * 512:(cb + 1) * 512], semkey=("wada", cb % 2))
            for jj in range(4):
                j = cb * 4 + jj
                for kk in range(KC):
                    P.mm(acc[:, j, :], wblk[:, kk, jj * 128:(jj + 1) * 128], B.sc3[:, kk, :],
                         start=(kk == 0), stop=(kk == KC - 1))
        bcol = B.small[:, 32:80]
        P.dma(bcol, I.b_ada_col[l])
        for v in range(3):
            P.tt(B.mod[:, l, v, :], acc[:, :, v], bcol, ALU.add)
        n1 = B.small[:, 80:88]
        n2 = B.small[:, 88:96]
        P.dma(n1, I.n1col[l])
        P.dma(n2, I.n2col[l])
        for v in range(3):
            P.stt(B.modA[:, l, v, 0, :], B.mod[:, l, v, 8:16], 1.0, n1, ALU.add, ALU.mult)
            P.stt(B.modA[:, l, v, 1, :], B.mod[:, l, v, 32:40], 1.0, n2, ALU.add, ALU.mult)
    tap(k, "mod", B.mod[:].rearrange("p l v j -> p (l v j)"))


def rstd_col(k, out_col, ss_col, n, tmp_col):
    P = k.P
    P.ts(tmp_col, ss_col, 1.0 / n, EPS, ALU.mult, ALU.add)
    P.act(tmp_col, tmp_col, AF.Sqrt)
    P.add("dve", lambda e: e.reciprocal(out=out_col, in_=tmp_col), [tmp_col], [out_col])


def norm_to_T(k, src, T, Acol, Shcol, hT, xn_tok=None):
    P, B = k.P, k.B
    NT = T // 128
    for i in range(NT):
        xt = B.xt[i % 2]
        P.dma(xt[:], src[i * 128:(i + 1) * 128, :], semkey=("xt", i % 2))
        st = B.st[:, (i % 2) * 4:(i % 2) * 4 + 4]
        P.act(B.junk[:], xt[:], AF.Square, accum_out=st[:, 0:1])
        rstd_col(k, st[:, 2:3], st[:, 0:1], D, st[:, 1:2])
        xn = xn_tok[:, i, :] if xn_tok is not None else B.xn[:]
        P.ts(xn, xt[:], st[:, 2:3], None, ALU.mult)
        if hT is None:
            continue
        pb = k.pb[i % 2]
        for kk in range(KC):
            P.transpose(pb[:, kk * 128:(kk + 1) * 128], xn[:, kk * 128:(kk + 1) * 128], k.cb(C_ID))
        pv = pb[:].rearrange("p (a b) -> p a b", a=KC)
        dst = hT[:, :, i * 128:(i + 1) * 128]
        P.tt(dst, pv, Acol.unsqueeze(2).broadcast_to([128, KC, 128]), ALU.mult)
        P.tt(dst, dst, Shcol.unsqueeze(2).broadcast_to([128, KC, 128]), ALU.add, eng="pool")


def load_w_cols(k, dst, l, c0, ncols, key):
    wv = k.I.w_in[l].rearrange("(kk p) c -> p kk c", p=128)
    k.P.dma(dst, wv[:, :, c0:c0 + ncols], eng="pool", semkey=key)


def proj_fm(k, hT, T, w, ncol, consume):
    P = k.P
    for j in range(ncol // 128):
        for bi, (t0, n) in enumerate(tokblocks(T)):
            pf = k.pf[(j * 4 + bi) % 4]
            for kk in range(KC):
                P.mm(pf[:, 0:n], w[:, kk, j * 128:(j + 1) * 128], hT[:, kk, t0:t0 + n],
                     start=(kk == 0), stop=(kk == KC - 1))
            consume(j, t0, n, pf[:, 0:n])


def mixer(k, b, l, src, dst, T, is_ctx, states_only):
    P, B, I, S = k.P, k.B, k.I, k.S
    NT = T // 128
    v = 2 if is_ctx else b
    AB = B.AB
    h1T = AB[:, 0:KC * T].rearrange("p (a t) -> p a t", a=KC)
    yaT = AB[:, 16384:16384 + 8 * T].rearrange("p (a t) -> p a t", a=8)
    ybT = AB[:, 32768:32768 + 4 * T].rearrange("p (a t) -> p a t", a=4)
    ycT = AB[:, 40960:40960 + 4 * T].rearrange("p (a t) -> p a t", a=4)
    norm_to_T(k, src, T, B.modA[:, l, v, 0, :], B.mod[:, l, v, 0:8], h1T)
    tap(k, "h1T", h1T)
    if not states_only:
        gate_phase(k, l, h1T, T)
    gdn(k, b, l, h1T, yaT, T, is_ctx, states_only)
    if states_only:
        return
    tap(k, "yaT", yaT)
    cmlp(k, l, h1T, ybT, T)
    tap(k, "ybT", ybT)
    pool(k, l, h1T, ycT, T, is_ctx)
    tap(k, "ycT", ycT)
    merge_out(k, b, l, v, src, dst, yaT, ybT, ycT, T)


def gate_phase(k, l, h1T, T):
    P, B, S = k.P, k.B, k.S
    for blk in range(12):
        w = B.wst[blk % 2]
        load_w_cols(k, w[:], l, OFF_G + blk * 256, 256, ("wst", blk % 2))

        def consume(j, t0, n, pap, blk=blk):
            stg = B.AB[:, 49152 - 1024 + (j % 2) * 512:49152 - 1024 + (j % 2) * 512 + n]
            P.act(stg, pap, AF.Sigmoid)
            P.dma(S.gates[blk * 2 + j, :, t0:t0 + n], stg, eng="sp", semkey=("gst", j % 2))
        proj_fm(k, h1T, T, w, 256, consume)


GS_BETA, GS_NBETA, GS_G, GS_GC, GS_EGC, GS_EDEC, GS_ETOT, GS_BEXP, GS_TOT = range(9)


def gdn(k, b, l, h1T, yaT, T, is_ctx, states_only):
    P, B, I = k.P, k.B, k.I
    NT = T // 128
    AB, AFa = B.AB, B.AF
    GB = 32768
    QT = AB[:, GB:GB + T]
    KT = AB[:, GB + 2048:GB + 2048 + T]
    VT = AB[:, GB + 4096:GB + 4096 + T]
    Ktok = AB[:, GB + 6144:GB + 6144 + T].rearrange("p (a c) -> p a c", c=128)
    Vtok = AB[:, GB + 8192:GB + 8192 + T].rearrange("p (a c) -> p a c", c=128)
    zs = AB[:, GB + 10240:GB + 10240 + T].rearrange("p (a c) -> p a c", c=128)
    yatok = AB[:, GB + 12288:GB + 12288 + T].rearrange("p (a c) -> p a c", c=128)
    tb = GB + 14336
    attn_b = AB[:, tb:tb + 128]
    attnT = AB[:, tb + 128:tb + 256]
    wT = AB[:, tb + 256:tb + 384]
    qgT = AB[:, tb + 384:tb + 512]
    kdec = AB[:, tb + 512:tb + 640]
    vnew = AB[:, tb + 640:tb + 768]
    Sbf = AB[:, tb + 768:tb + 896]
    cbuf = AFa[:, 0:T + 4]
    cs = AFa[:, 2052:2052 + T]
    oacc = AFa[:, 4100:4100 + T].rearrange("p (a c) -> p a c", c=128)
    fb = 6148
    Rm = AFa[:, fb:fb + 128]
    dec = AFa[:, fb + 128:fb + 256]
    egr = AFa[:, fb + 256:fb + 384]
    nk = [AFa[:, fb + 384 + i * 128:fb + 512 + i * 128] for i in range(2)]
    Pk = [AFa[:, fb + 640 + i * 128:fb + 768 + i * 128] for i in range(2)]
    Xk = [AFa[:, fb + 896 + i * 128:fb + 1024 + i * 128] for i in range(2)]
    rhsu = AFa[:, fb + 1152:fb + 1280]
    rhsw = AFa[:, fb + 1280:fb + 1408]
    usb = AFa[:, fb + 1408:fb + 1536]
    Sst = AFa[:, fb + 1536:fb + 1664]
    sq = AFa[:, fb:fb + 512]
    rinv = AFa[:, fb + 512:fb + 1024]
    tmpf = AFa[:, fb + 2688:fb + 2688 + 128]
    ident, identb = k.c(C_ID), k.cb(C_ID)
    ones = k.c(C_ONE)
    gs = B.gsc

    wba = B.wsm[0]
    load_w_cols(k, wba[:, :, 0:32], l, OFF_B, 32, ("wsm", 0))
    sm = B.small
    alog = sm[:, 128:144]
    dtb = sm[:, 144:160]
    negA = sm[:, 160:176]
    gnw = sm[:, 256:384]
    P.dma(alog, I.alog_bc[l])
    P.dma(dtb, I.dtb_bc[l])
    P.dma(gnw, I.gnw_bc[l])
    cw = sm[:, 384:504].rearrange("p (a t) -> p a t", t=5)
    P.dma(cw, I.convw[l])
    P.act(negA, alog, AF.Exp)
    P.ts(negA, negA, -1.0, None, ALU.mult)
    for i in range(NT):
        pf = k.pf[4]
        for kk in range(KC):
            P.mm(pf[:, 0:32], h1T[:, kk, i * 128:(i + 1) * 128], wba[:, kk, 0:32],
                 start=(kk == 0), stop=(kk == KC - 1))
        P.act(gs[:, GS_BETA, i, :], pf[:, 0:16], AF.Sigmoid)
        P.tt(gs[:, GS_G, i, :], pf[:, 16:32], dtb, ALU.add)
    nt = slice(0, NT)
    P.ts(gs[:, GS_NBETA, nt, :], gs[:, GS_BETA, nt, :], -1.0, None, ALU.mult)
    P.act(gs[:, GS_G, nt, :], gs[:, GS_G, nt, :], AF.Exp)
    P.act(gs[:, GS_G, nt, :], gs[:, GS_G, nt, :], AF.Ln, bias=1.0)
    P.tt(gs[:, GS_G, nt, :], gs[:, GS_G, nt, :], negA.unsqueeze(1).broadcast_to([128, NT, 16]), ALU.mult)
    for i in range(NT):
        pf = k.pf[4]
        P.mm(pf[:, 0:8], k.c(C_U), gs[:, GS_G, i, 0:8])
        P.mm(pf[:, 8:16], k.c(C_L), gs[:, GS_G, i, 8:16])
        P.mm(pf[:, 16:32], ones, gs[:, GS_G, i, :])
        pcopy(k, gs[:, GS_GC, i, :], pf[:, 0:16], "dve")
        pcopy(k, gs[:, GS_TOT, i, :], pf[:, 16:32], "act")
    P.act(gs[:, GS_EGC, nt, :], gs[:, GS_GC, nt, :], AF.Exp)
    P.act(gs[:, GS_ETOT, nt, :], gs[:, GS_TOT, nt, :], AF.Exp)
    P.tt(gs[:, GS_EDEC, nt, :], gs[:, GS_TOT, nt, :], gs[:, GS_GC, nt, :], ALU.subtract)
    P.act(gs[:, GS_EDEC, nt, :], gs[:, GS_EDEC, nt, :], AF.Exp)
    P.tt(gs[:, GS_BEXP, nt, :], gs[:, GS_BETA, nt, :], gs[:, GS_EGC, nt, :], ALU.mult)
    tap(k, "gsc", gs)

    P.memset(cbuf[:, 0:2], 0.0)
    P.memset(cbuf[:, T + 2:T + 4], 0.0)
    for h in range(H):
        for ci, (off, dstT) in enumerate(((OFF_Q, QT), (OFF_K, KT), (OFF_V, VT))):
            w = B.wsm[1 + ci]
            load_w_cols(k, w[:], l, off + h * 128, 128, ("wsm", 1 + ci))

            def consume(j, t0, n, pap):
                pcopy(k, cbuf[:, 2 + t0:2 + t0 + n], pap)
            proj_fm(k, h1T, T, w, 128, consume)
            ce = "dve" if ci != 1 else "pool"
            cwc = cw[:, ci * 8 + h, :]
            P.ts(cs, cbuf[:, 0:T], cwc[:, 0:1], None, ALU.mult, eng=ce)
            for tp in range(1, 5):
                P.stt(cs, cbuf[:, tp:tp + T], cwc[:, tp:tp + 1], cs, ALU.mult, ALU.add, eng=ce)
            if ci == 2:
                P.act(dstT, cs, AF.Silu)
                continue
            P.act(cs, cs, AF.Silu)
            for (t0, n) in tokblocks(T):
                P.act(sq[:, 0:n], cs[:, t0:t0 + n], AF.Square)
                pf = k.pf[5]
                P.mm(pf[:, 0:n], ones, sq[:, 0:n])
                P.ts(rinv[:, 0:n], pf[:, 0:n], EPS, None, ALU.add)
                P.act(rinv[:, 0:n], rinv[:, 0:n], AF.Sqrt)
                P.add("dve", lambda e, n=n: e.reciprocal(out=rinv[:, 0:n], in_=rinv[:, 0:n]),
                      [rinv[:, 0:n]], [rinv[:, 0:n]])
                if ci == 0:
                    P.stt(dstT[:, t0:t0 + n], cs[:, t0:t0 + n], 128.0 ** -0.5, rinv[:, 0:n], ALU.mult, ALU.mult)
                else:
                    P.tt(dstT[:, t0:t0 + n], cs[:, t0:t0 + n], rinv[:, 0:n], ALU.mult)
        for (srcT, dtok) in ((KT, Ktok), (VT, Vtok)):
            for i0 in range(0, NT, 8):
                pb = k.pb[(i0 // 8) % 2]
                ni = min(8, NT - i0)
                for i in range(ni):
                    P.transpose(pb[:, i * 128:(i + 1) * 128], srcT[:, (i0 + i) * 128:(i0 + i + 1) * 128], identb)
                pcopy(k, dtok[:, i0:i0 + ni, :], pb[:, 0:ni * 128].rearrange("p (a c) -> p a c", c=128))
        if h == 0:
            tap(k, "QT0", QT)
            tap(k, "KT0", KT)
            tap(k, "Vtok0", Vtok)
        if not states_only:
            wz = B.wsm[0]
            load_w_cols(k, wz[:], l, OFF_Z + h * 128, 128, ("wsm", 0))
            for i in range(NT):
                pf = k.pf[4]
                for kk in range(KC):
                    P.mm(pf[:, 0:128], h1T[:, kk, i * 128:(i + 1) * 128], wz[:, kk, :],
                         start=(kk == 0), stop=(kk == KC - 1))
                P.act(zs[:, i, :], pf[:, 0:128], AF.Silu)
        P.memset(oacc, 0.0)

        def chain(dr):
            col = dr * 8 + h
            msk = k.c(C_U) if dr == 0 else k.c(C_L)
            nm = k.c(C_NMF) if dr == 0 else k.c(C_NMB)
            sm_ = k.c(C_SL) if dr == 0 else k.c(C_SU)
            fo = fb + dr * 1664
            Rm = AFa[:, fo:fo + 128]
            dec = AFa[:, fo + 128:fo + 256]
            egr = AFa[:, fo + 256:fo + 384]
            nk = [AFa[:, fo + 384 + i * 128:fo + 512 + i * 128] for i in range(2)]
            Pk = [AFa[:, fo + 640 + i * 128:fo + 768 + i * 128] for i in range(2)]
            Xk = [AFa[:, fo + 896 + i * 128:fo + 1024 + i * 128] for i in range(2)]
            rhsu = AFa[:, fo + 1152:fo + 1280]
            rhsw = AFa[:, fo + 1280:fo + 1408]
            usb = AFa[:, fo + 1408:fo + 1536]
            Sst = AFa[:, fo + 1536:fo + 1664]
            bo = tb + dr * 896
            attn_b = AB[:, bo:bo + 128]
            attnT = AB[:, bo + 128:bo + 256]
            wT = AB[:, bo + 256:bo + 384]
            qgT = AB[:, bo + 384:bo + 512]
            kdec = AB[:, bo + 512:bo + 640]
            vnew = AB[:, bo + 640:bo + 768]
            Sbf = AB[:, bo + 768:bo + 896]
            b0, b1, b2 = k.pf[dr * 3], k.pf[dr * 3 + 1], k.pf[dr * 3 + 2]
            pbx = k.pb[dr]
            if is_ctx:
                P.memset(Sst, 0.0)
            else:
                P.copy(Sst, B.states[:, col, :], eng="pool")
            P.copy(Sbf, Sst, eng="pool")
            yield
            order = range(NT) if dr == 0 else range(NT - 1, -1, -1)
            for c in order:
                tsl = slice(c * 128, (c + 1) * 128)
                sc = lambda kind: gs[:, kind, c, col:col + 1]
                P.ts(Rm, msk, sc(GS_G), -1.0, ALU.mult, ALU.mult, eng="pool")
                P.mm(b1[:, 0:128], KT[:, tsl], KT[:, tsl])
                P.mm(b1[:, 128:256], QT[:, tsl], KT[:, tsl])
                P.mm(b0[:, 0:128], ones, Rm)
                yield
                P.act(egr, b0[:, 0:128], AF.Exp, scale=-1.0)
                P.tt(dec, b0[:, 0:128], nm, ALU.add)
                yield
                P.act(dec, dec, AF.Exp, bias=sc(GS_GC))
                P.ts(rhsu, Vtok[:, c, :], sc(GS_BETA), None, ALU.mult, eng="pool")
                P.ts(rhsw, Ktok[:, c, :], sc(GS_BEXP), None, ALU.mult, eng="pool")
                yield
                P.stt(nk[0], b1[:, 0:128], sc(GS_NBETA), dec, ALU.mult, ALU.mult)
                P.tt(attn_b, b1[:, 128:256], dec, ALU.mult)
                yield
                P.tt(nk[0], nk[0], sm_, ALU.mult, eng="pool")
                P.transpose(pbx[:, 0:128], attn_b, identb)
                yield
                P.transpose(b2[:, 0:128], nk[0], ident)
                pcopy(k, attnT, pbx[:, 0:128], "act")
                yield
                pcopy(k, Pk[0], b2[:, 0:128], "act")
                P.tt(Xk[0], b2[:, 0:128], ident, ALU.add)
                P.tt(qgT, QT[:, tsl], egr, ALU.mult, eng="pool")
                P.ts(kdec, Ktok[:, c, :], sc(GS_EDEC), None, ALU.mult, eng="pool")
                yield
                cur = 0
                for lev in range(1, 7):
                    nxt = 1 - cur
                    P.mm(b2[:, 0:128], Pk[cur], nk[cur])
                    if lev < 6:
                        P.mm(b2[:, 128:256], nk[cur], Pk[cur])
                    yield
                    pcopy(k, nk[nxt], b2[:, 0:128], "act")
                    if lev < 6:
                        pcopy(k, Pk[nxt], b2[:, 128:256], "dve")
                    yield
                    P.mm(b0[:, 0:128], nk[nxt], Xk[cur])
                    yield
                    P.tt(Xk[nxt], Xk[cur], b0[:, 0:128], ALU.add)
                    yield
                    cur = nxt
                X = Xk[cur]
                P.mm(b1[:, 128:256], X, rhsu)
                P.mm(b1[:, 256:384], rhsw, X)
                yield
                pcopy(k, usb, b1[:, 128:256], "act")
                pcopy(k, wT, b1[:, 256:384], "dve")
                yield
                P.mm(b0[:, 256:384], wT, Sbf)
                yield
                P.tt(vnew, usb, b0[:, 256:384], ALU.subtract)
                yield
                P.mm(b1[:, 0:128], qgT, Sbf, start=True, stop=False)
                P.mm(b1[:, 0:128], attnT, vnew, start=False, stop=True)
                P.mm(b2[:, 0:128], kdec, vnew)
                yield
                P.tt(oacc[:, c, :], oacc[:, c, :], b1[:, 0:128], ALU.add)
                P.stt(Sst, Sst, sc(GS_ETOT), b2[:, 0:128], ALU.mult, ALU.add)
                yield
                P.copy(Sbf, Sst, eng="act")
                yield
            if is_ctx:
                P.copy(B.states[:, col, :], Sst, eng="pool")

        gens = [chain(0), chain(1)]
        while gens:
            for g_ in list(gens):
                try:
                    next(g_)
                except StopIteration:
                    gens.remove(g_)
        if states_only:
            continue
        if h == 0:
            tap(k, "oacc0", oacc)
        P.act(cs[:, 0:T].rearrange("p (a c) -> p a c", c=128), oacc, AF.Square)
        ssv = B.st[:, 16:16 + NT]
        P.reduce(ssv, cs[:, 0:T].rearrange("p (a c) -> p a c", c=128), ALU.add)
        P.ts(ssv, ssv, 1.0 / 128, EPS, ALU.mult, ALU.add)
        P.act(ssv, ssv, AF.Sqrt)
        P.add("dve", lambda e, ssv=ssv: e.reciprocal(out=ssv, in_=ssv), [ssv], [ssv])
        P.tt(oacc, oacc, ssv.unsqueeze(2).broadcast_to([128, NT, 128]), ALU.mult)
        P.tt(oacc, oacc, gnw.unsqueeze(1).broadcast_to([128, NT, 128]), ALU.mult, eng="pool")
        P.tt(yatok, oacc, zs, ALU.mult)
        for i0 in range(0, NT, 8):
            pb = k.pb[(i0 // 8) % 2]
            ni = min(8, NT - i0)
            for i in range(ni):
                P.transpose(pb[:, i * 128:(i + 1) * 128], yatok[:, i0 + i, :], identb)
            pcopy(k, yaT[:, h, i0 * 128:(i0 + ni) * 128], pb[:, 0:ni * 128])
    if is_ctx:
        tap(k, "states", B.states)


def gelu_tanh(k, out, in_, tmp, eng="dve"):
    P = k.P
    P.act(tmp, in_, AF.Square)
    P.ts(tmp, tmp, 0.044715, 1.0, ALU.mult, ALU.add, eng=eng)
    P.tt(tmp, tmp, in_, ALU.mult, eng=eng)
    P.act(tmp, tmp, AF.Sigmoid, scale=1.5957691216057308)
    P.tt(out, tmp, in_, ALU.mult, eng=eng)


def cmlp(k, l, h1T, ybT, T):
    P, B, I = k.P, k.B, k.I
    NT = T // 128
    AF_ = B.AF
    for q in range(2):
        load_w_cols(k, B.wst[q][:], l, OFF_U + q * 256, 256, ("wst", q))
    for q in range(4):
        load_w_cols(k, B.wsm[q][:], l, OFF_VG + q * 128, 128, ("wsm", q))
    sm = B.small
    lnw = sm[:, 512:1024]
    lnb = sm[:, 1024:1536]
    P.dma(lnw, I.lnw_bc[l])
    P.dma(lnb, I.lnb_bc[l])
    bs = sm[:, 176:180]
    P.dma(bs, I.bs_col[l])
    wsT = AF_[:, 8192:8192 + 512].rearrange("p (g c) -> p g c", g=4)
    P.dma(wsT, I.wsT[l].rearrange("g q p -> q g p"))
    ub = AF_[:, 0:512]
    vb = AF_[:, 512:1024]
    t1 = AF_[:, 1024:1536]
    t2 = AF_[:, 1536:2048]
    ybb = B.junk[:, 0:512]
    for i in range(NT):
        pu = k.pf[0]
        pv = k.pf[1]
        for q in range(2):
            for kk in range(KC):
                P.mm(pu[:, q * 256:(q + 1) * 256], h1T[:, kk, i * 128:(i + 1) * 128], B.wst[q][:, kk, :],
                     start=(kk == 0), stop=(kk == KC - 1))
        for q in range(4):
            for kk in range(KC):
                P.mm(pv[:, q * 128:(q + 1) * 128], h1T[:, kk, i * 128:(i + 1) * 128], B.wsm[q][:, kk, :],
                     start=(kk == 0), stop=(kk == KC - 1))
        pcopy(k, ub, pu[:, 0:512], "act")
        pcopy(k, vb, pv[:, 0:512], "dve")
        gelu_tanh(k, ub, ub, t1, eng="pool")
        gelu_tanh(k, vb, vb, t2, eng="dve")
        st = B.st[:, 32:40]
        P.reduce(st[:, 0:1], vb, ALU.add)
        P.ts(st[:, 1:2], st[:, 0:1], -1.0 / 512, None, ALU.mult)
        P.ts(vb, vb, st[:, 1:2], None, ALU.add)
        P.act(t2, vb, AF.Square, accum_out=st[:, 2:3])
        rstd_col(k, st[:, 4:5], st[:, 2:3], 512, st[:, 3:4])
        P.ts(vb, vb, st[:, 4:5], None, ALU.mult)
        P.tt(vb, vb, lnw, ALU.mult, eng="pool")
        P.tt(vb, vb, lnb, ALU.add, eng="pool")
        pm = k.pf[2]
        for g in range(4):
            P.mm(pm[:, g * 128:(g + 1) * 128], wsT[:, g, :], vb[:, g * 128:(g + 1) * 128])
        for g in range(4):
            P.stt(ybb[:, g * 128:(g + 1) * 128], pm[:, g * 128:(g + 1) * 128], bs[:, g:g + 1],
                  ub[:, g * 128:(g + 1) * 128], ALU.add, ALU.mult)
        pb = k.pb[i % 2]
        for g in range(4):
            P.transpose(pb[:, g * 128:(g + 1) * 128], ybb[:, g * 128:(g + 1) * 128], k.cb(C_ID))
        pcopy(k, ybT[:, :, i * 128:(i + 1) * 128], pb[:, 0:512].rearrange("p (g c) -> p g c", g=4))


def pool(k, l, h1T, ycT, T, is_ctx):
    P, B, I = k.P, k.B, k.I
    seg = 256 if is_ctx else 64
    nseg = T // seg
    W = seg + 32
    AF_ = B.AF
    sm = B.small
    psc = sm[:, 180:184]
    P.dma(psc, I.psc_col[l])
    pw = AF_[:, 8192:8192 + 512].rearrange("p (g c) -> p g c", g=4)
    P.dma(pw, I.pool_w[l].rearrange("g c d -> c g d"))
    pwb = B.junk[:, 0:512].rearrange("p (g c) -> p g c", g=4)
    P.copy(pwb, pw, eng="pool")
    rc0 = C_RCC if is_ctx else C_RCX
    rcs = sm[:, 512:512 + 4 * seg]
    P.dma(rcs, I.cst[:, rc0:rc0 + 4 * seg])
    bufs = [AF_[:, i * 3072:(i + 1) * 3072][:, 0:nseg * W].rearrange("p (s w) -> p s w", w=W) for i in range(2)]
    pin = AF_[:, 6144:6144 + T].rearrange("p (s w) -> p s w", w=seg)
    pooled = AF_[:, 8704:8704 + T]
    pooledb = B.AB[:, 49152 - 2048:49152 - 2048 + T]
    for g in range(4):
        def consume(j, t0, n, pap):
            pcopy(k, AF_[:, 6144 + t0:6144 + t0 + n], pap)
        w = B.wsm[g]
        load_w_cols(k, w[:], l, OFF_P + g * 128, 128, ("wsm", g))
        proj_fm(k, h1T, T, w, 128, consume)
        a, bb = bufs
        P.memset(a, 0.0)
        P.memset(bb, 0.0, eng="dve")
        P.copy(a[:, :, 16:16 + seg], pin, eng="pool")
        lo, hi = 8, seg + 24
        P.tt(bb[:, :, lo:hi], a[:, :, lo:hi], a[:, :, lo - 1:hi - 1], ALU.add)
        cur, oth = bb, a
        for lev in range(g):
            sh = 1 << lev
            P.tt(oth[:, :, lo:hi], cur[:, :, lo - sh:hi - sh], cur[:, :, lo + sh:hi + sh], ALU.add, eng="pool")
            cur, oth = oth, cur
        rc = rcs[:, g * seg:(g + 1) * seg]
        pv = pooled.rearrange("p (s w) -> p s w", w=seg)
        P.tt(pv, cur[:, :, 16:16 + seg], rc.unsqueeze(1).broadcast_to([128, nseg, seg]), ALU.mult)
        P.tt(pooledb.rearrange("p (s w) -> p s w", w=seg), pv, pin, ALU.subtract)
        for bi, (t0, n) in enumerate(tokblocks(T)):
            pf = k.pf[4 + bi % 2]
            P.mm(pf[:, 0:n], pwb[:, g, :], pooledb[:, t0:t0 + n])
            P.act(ycT[:, g, t0:t0 + n], pf[:, 0:n], AF.Identity, scale=psc[:, g:g + 1])


def bcast_row(k, out_bc, col8, dg=None):
    P = k.P
    for half in range(2):
        pf = k.pf[half]
        for kk in range(4):
            c = half * 4 + kk
            if dg is None:
                dg = k.B.AF[:, 16384 - 128:16384]
            P.ts(dg, k.c(C_ID), col8[:, c:c + 1], None, ALU.mult)
            P.mm(pf[:, kk * 128:(kk + 1) * 128], k.c(C_ONE), dg)
        pcopy(k, out_bc[:, half * 512:(half + 1) * 512], pf[:, 0:512])


def merge_out(k, b, l, v, src, dst, yaT, ybT, ycT, T):
    P, B, I, S = k.P, k.B, k.I, k.S
    NT = T // 128
    AF_ = B.AF
    mT = B.AB[:, 0:KC * T].rearrange("p (a t) -> p a t", a=KC)
    g1bc = AF_[:, 0:1024]
    bcast_row(k, g1bc, B.mod[:, l, v, 16:24])
    for dc in range(KC):
        wbr = B.wsm[dc % 2]
        wbr2 = B.wsm[2 + dc % 2]
        P.dma(wbr[:], I.w_br_a[l].rearrange("(kk p) c -> p kk c", p=128)[:, :, dc * 128:(dc + 1) * 128],
              eng="pool", semkey=("wsm", dc % 2))
        P.dma(wbr2[:, 0:4, :], I.w_br_b[l].rearrange("(kk p) c -> p kk c", p=128)[:, :, dc * 128:(dc + 1) * 128],
              eng="pool", semkey=("wsm", 2 + dc % 2))
        P.dma(wbr2[:, 4:8, :], I.w_br_c[l].rearrange("(kk p) c -> p kk c", p=128)[:, :, dc * 128:(dc + 1) * 128],
              eng="pool", semkey=("wsm", 2 + dc % 2))
        for bi, (t0, n) in enumerate(tokblocks(T)):
            gts = [AF_[:, 12288 + a * 512:12288 + a * 512 + n] for a in range(3)]
            gtb = [B.junk[:, 0:512], B.junk[:, 512:1024], B.xn[:, 0:512]]
            for a in range(3):
                P.dma(gtb[a][:, 0:n], S.gates[a * 8 + dc, :, t0:t0 + n], semkey=("gld", a))
            acc = AF_[:, 14336:14336 + n]
            for a, (yT, nk_, wsrc, koff) in enumerate(((yaT, 8, wbr, 0), (ybT, 4, wbr2, 0), (ycT, 4, wbr2, 4))):
                pf = k.pf[a]
                for kk in range(nk_):
                    P.mm(pf[:, 0:n], wsrc[:, koff + kk, :], yT[:, kk, t0:t0 + n], start=(kk == 0), stop=(kk == nk_ - 1))
                if a == 0:
                    P.tt(acc, pf[:, 0:n], gtb[a][:, 0:n], ALU.mult)
                else:
                    P.tt(gts[a], pf[:, 0:n], gtb[a][:, 0:n], ALU.mult)
                    P.tt(acc, acc, gts[a], ALU.add, eng="pool")
            P.copy(mT[:, dc, t0:t0 + n], acc, eng="act")
    wv = I.w_out[l].rearrange("(kk p) c -> p kk c", p=128)
    for q in range(4):
        wdst = B.wst[q % 2]
        cs_ = slice(q * 256, (q + 1) * 256)
        for kk in range(KC):
            stg = AF_[:, 2048 + (kk % 2) * 256:2048 + (kk % 2) * 256 + 256]
            P.dma(stg, wv[:, kk, cs_], semkey=("wo", kk % 2))
            P.tt(wdst[:, kk, :], stg, g1bc[:, cs_], ALU.mult)
        for i in range(NT):
            xq = B.xt[i % 2][:, cs_]
            P.dma(xq, src[i * 128:(i + 1) * 128, cs_], semkey=("xt", i % 2))
            pf = k.pf[4 + i % 2]
            for kk in range(KC):
                P.mm(pf[:, 0:256], mT[:, kk, i * 128:(i + 1) * 128], wdst[:, kk, :],
                     start=(kk == 0), stop=(kk == KC - 1))
            P.tt(xq, xq, pf[:, 0:256], ALU.add)
            P.dma(dst[i * 128:(i + 1) * 128, cs_], xq, semkey=("xt", i % 2))


def moe(k, b, l, src, dst, T, is_ctx):
    P, B, I = k.P, k.B, k.I
    NT = T // 128
    cap = 2 * T // NE
    v = 2 if is_ctx else b
    AB, AF_ = B.AB, B.AF
    nsh = (cap + 127) // 128
    sp_ = min(cap, 128)
    xn_tok = AB[:, 0:NT * 1024].rearrange("p (a d) -> p a d", d=1024)
    Sel = AB[:, 16384:16384 + NT * cap].rearrange("p (a s) -> p a s", s=cap)
    SelT = AB[:, 20480:20480 + nsh * T].rearrange("p (a t) -> p a t", a=nsh)
    xeT = AB[:, 24576:24576 + KC * cap].rearrange("p (a s) -> p a s", a=KC)
    hT = AB[:, 26624:26624 + KC * cap].rearrange("p (a s) -> p a s", a=KC)
    ye = AB[:, 28672:28672 + nsh * 1024].rearrange("p (a d) -> p a d", a=nsh)
    wring = [AB[:, 30720 + i * 2048:30720 + (i + 1) * 2048] for i in range(8)]
    ymoe = AF_[:, 0:NT * 1024].rearrange("p (a d) -> p a d", d=1024)
    sm = B.small
    A2 = B.modA[:, l, v, 1, :]
    sh2 = B.mod[:, l, v, 24:32]
    ident, identb = k.c(C_ID), k.cb(C_ID)
    wr = AF_[:, 7168:7296].rearrange("p (a e) -> p a e", a=KC)
    P.dma(wr, I.w_router[l].rearrange("(kk p) e -> p kk e", p=128))
    wrs = AF_[:, 7296:7424].rearrange("p (a e) -> p a e", a=KC)
    P.tt(wrs, wr, A2.unsqueeze(2).broadcast_to([128, KC, NE]), ALU.mult)
    pbias = k.pf[5]
    rep = AF_[:, 7424:7552]
    for kk in range(KC):
        P.ts(rep, k.c(C_ONE), sh2[:, kk:kk + 1], None, ALU.mult)
        P.mm(pbias[:, 0:NE], rep, wr[:, kk, :], start=(kk == 0), stop=(kk == KC - 1))
    rbias = AF_[:, 7552:7568]
    P.copy(rbias, pbias[:, 0:NE])
    afft = sm[:, 1024:1024 + NT * NE].rearrange("p (a e) -> p a e", e=NE)
    maskf = sm[:, 1280:1280 + NT * NE].rearrange("p (a e) -> p a e", e=NE)
    pref = sm[:, 1536:1536 + NT * NE].rearrange("p (a e) -> p a e", e=NE)
    twc = sm[:, 1792:1800]
    xT = AF_[:, 2048:3072].rearrange("p (a c) -> p a c", a=KC)
    xnf = B.xt[0]
    for i in range(NT):
        xt = B.xt[1]
        P.dma(xt[:], src[i * 128:(i + 1) * 128, :], semkey=("xt", 1))
        st = B.st[:, 40:48]
        P.act(B.junk[:], xt[:], AF.Square, accum_out=st[:, 0:1])
        rstd_col(k, st[:, 2:3], st[:, 0:1], D, st[:, 1:2])
        P.ts(xnf[:], xt[:], st[:, 2:3], None, ALU.mult)
        P.copy(xn_tok[:, i, :], xnf[:], eng="pool")
        for half in range(2):
            pt = k.pf[half]
            for kk in range(4):
                c = half * 4 + kk
                P.transpose(pt[:, kk * 128:(kk + 1) * 128], xnf[:, c * 128:(c + 1) * 128], ident)
            pcopy(k, xT[:, half * 4:half * 4 + 4, :], pt[:, 0:512].rearrange("p (a c) -> p a c", a=4))
        plog = k.pf[4]
        for kk in range(KC):
            P.mm(plog[:, 0:NE], xT[:, kk, :], wrs[:, kk, :], start=(kk == 0), stop=(kk == KC - 1))
        lg = B.st[:, 48:64]
        P.tt(lg, plog[:, 0:NE], rbias, ALU.add)
        P.reduce(st[:, 3:4], lg, ALU.max)
        P.ts(st[:, 4:5], st[:, 3:4], -1.0, None, ALU.mult)
        P.act(lg, lg, AF.Exp, bias=st[:, 4:5], accum_out=st[:, 5:6])
        P.add("dve", lambda e, st=st: e.reciprocal(out=st[:, 6:7], in_=st[:, 5:6]), [st[:, 5:6]], [st[:, 6:7]])
        P.ts(afft[:, i, :], lg, st[:, 6:7], None, ALU.mult)
    tap(k, "aff", afft)
    affE = AF_[0:NE, 3072:3072 + T]
    wk = AF_[0:NE, 5120:5120 + T]
    for i0 in range(0, NT, 4):
        pt = k.pf[0]
        ni = min(4, NT - i0)
        for i in range(ni):
            P.transpose(pt[0:NE, i * 128:(i + 1) * 128], afft[:, i0 + i, :], ident)
        pcopy(k, affE[:, i0 * 128:(i0 + ni) * 128], pt[0:NE, 0:ni * 128])
    m8 = B.st[0:NE, 8:16]
    for r in range(cap // 8):
        srcv = affE if r == 0 else wk
        P.add("dve", lambda e, srcv=srcv: e.max(out=m8, in_=srcv), [srcv], [m8])
        P.add("dve", lambda e, srcv=srcv: e.match_replace(out=wk, in_to_replace=m8, in_values=srcv, imm_value=-1.0),
              [m8, srcv], [wk])
    maskE = B.wst[0][:].rearrange("p a c -> p (a c)")[0:NE, 0:T]
    P.tt(maskE, affE, wk, ALU.not_equal)
    pm = k.pb[0]
    for i in range(NT):
        P.transpose(pm[:, i * NE:(i + 1) * NE], maskE[:, i * 128:(i + 1) * 128], identb[0:NE, 0:NE])
    maskb = B.junk[:, 0:NT * NE].rearrange("p (a e) -> p a e", e=NE)
    pcopy(k, maskb, pm[:, 0:NT * NE].rearrange("p (a e) -> p a e", e=NE), "act")
    pcopy(k, maskf, pm[:, 0:NT * NE].rearrange("p (a e) -> p a e", e=NE), "dve")
    tap(k, "mask", maskf)
    hl = B.junk[:, 256:256 + NT * NE * 2].rearrange("p (a e h) -> p a e h", e=NE, h=2)
    P.copy(hl[:, :, :, 0], afft)
    hif = B.st[:, 48:64]
    for i in range(NT):
        P.copy(hif, hl[:, i, :, 0], eng="pool")
        P.tt(hl[:, i, :, 1], afft[:, i, :], hif, ALU.subtract, eng="pool")
    for i in range(NT):
        pp = k.pf[1]
        for i2 in range(i + 1):
            P.mm(pp[:, 0:NE], k.cb(C_U) if i2 == i else k.cb(C_ONE), maskb[:, i2, :],
                 start=(i2 == 0), stop=(i2 == i))
        pcopy(k, pref[:, i, :], pp[:, 0:NE], "dve")
    g2bc = None
    for e in range(NE):
        for i in range(NT):
            P.ts(Sel[:, i, :], k.c(C_IOTA, cap), pref[:, i, e:e + 1], maskf[:, i, e:e + 1],
                 ALU.is_equal, ALU.mult)
        for hh in range(nsh):
            for i0 in range(0, NT, 8):
                pb = k.pb[(i0 // 8) % 2]
                ni = min(8, NT - i0)
                for i in range(ni):
                    P.transpose(pb[0:sp_, i * 128:(i + 1) * 128], Sel[:, i0 + i, hh * 128:hh * 128 + sp_], identb)
                pcopy(k, SelT[0:sp_, hh, i0 * 128:(i0 + ni) * 128], pb[0:sp_, 0:ni * 128])
        for kk in range(KC):
            pg = k.pf[kk % 2]
            for i in range(NT):
                P.mm(pg[:, 0:cap], xn_tok[:, i, kk * 128:(kk + 1) * 128], Sel[:, i, :],
                     start=(i == 0), stop=(i == NT - 1))
            P.act(xeT[:, kk, :], pg[:, 0:cap], AF.Identity, scale=A2[:, kk:kk + 1], bias=sh2[:, kk:kk + 1])
        for hh in range(nsh):
            pw_ = k.pf[2]
            for i in range(NT):
                P.mm(pw_[0:sp_, hh * 2:hh * 2 + 2], Sel[:, i, hh * 128:hh * 128 + sp_], hl[:, i, e, :],
                     start=(i == 0), stop=(i == NT - 1))
            P.reduce(twc[0:sp_, hh:hh + 1], pw_[0:sp_, hh * 2:hh * 2 + 2], ALU.add)
        wg_v = I.w_gate[l, e].rearrange("(kk p) f -> p kk f", p=128)
        wu_v = I.w_up[l, e].rearrange("(kk p) f -> p kk f", p=128)
        wd_v = I.w_down[l, e].rearrange("(m p) d -> p m d", p=128)
        for m2 in range(4):
            gp = wring[(m2 % 2) * 2].rearrange("p (a c) -> p a c", a=KC)
            up = wring[(m2 % 2) * 2 + 1].rearrange("p (a c) -> p a c", a=KC)
            P.dma(gp, wg_v[:, :, m2 * 256:(m2 + 1) * 256], eng="pool", semkey=("wr", (m2 % 2) * 2))
            P.dma(up, wu_v[:, :, m2 * 256:(m2 + 1) * 256], eng="pool", semkey=("wr", (m2 % 2) * 2 + 1))
            for mm_ in range(2):
                m = m2 * 2 + mm_
                pg = k.pf[0]
                pu = k.pf[1]
                for kk in range(KC):
                    P.mm(pg[:, 0:cap], gp[:, kk, mm_ * 128:(mm_ + 1) * 128], xeT[:, kk, :],
                         start=(kk == 0), stop=(kk == KC - 1))
                for kk in range(KC):
                    P.mm(pu[:, 0:cap], up[:, kk, mm_ * 128:(mm_ + 1) * 128], xeT[:, kk, :],
                         start=(kk == 0), stop=(kk == KC - 1))
                sg = sm[:, (m % 2) * 256:(m % 2) * 256 + cap]
                P.act(sg, pg[:, 0:cap], AF.Silu)
                P.tt(hT[:, m, :], sg, pu[:, 0:cap], ALU.mult)
        for m2 in range(4):
            dp = wring[4 + m2].rearrange("p (a d) -> p a d", a=2)
            P.dma(dp, wd_v[:, m2 * 2:m2 * 2 + 2, :], eng="pool", semkey=("wr", 4 + m2))
            for mm_ in range(2):
                m = m2 * 2 + mm_
                for hh in range(nsh):
                    for dh in range(2):
                        pd = k.pf[2 + hh * 2 + dh]
                        P.mm(pd[0:sp_, 0:512], hT[:, m, hh * 128:hh * 128 + sp_], dp[:, mm_, dh * 512:(dh + 1) * 512],
                             start=(m == 0), stop=(m == KC - 1))
        for hh in range(nsh):
            for dh in range(2):
                pd = k.pf[2 + hh * 2 + dh]
                P.act(ye[0:sp_, hh, dh * 512:(dh + 1) * 512], pd[0:sp_, 0:512], AF.Identity,
                      scale=twc[0:sp_, hh:hh + 1])
        for i in range(NT):
            for dh in range(2):
                pc = k.pf[(i * 2 + dh) % 2]
                for hh in range(nsh):
                    P.mm(pc[:, 0:512], SelT[0:sp_, hh, i * 128:(i + 1) * 128], ye[0:sp_, hh, dh * 512:(dh + 1) * 512],
                         start=(hh == 0), stop=(hh == nsh - 1))
                dsty = ymoe[:, i, dh * 512:(dh + 1) * 512]
                if e == 0:
                    pcopy(k, dsty, pc[:, 0:512])
                else:
                    P.tt(dsty, dsty, pc[:, 0:512], ALU.add)
    g2bc = sm[:, 0:1024]
    bcast_row(k, g2bc, B.mod[:, l, v, 40:48], dg=sm[:, 1920:2048])
    for i in range(NT):
        xt = B.xt[i % 2]
        P.dma(xt[:], src[i * 128:(i + 1) * 128, :], semkey=("xt", i % 2))
        P.tt(ymoe[:, i, :], ymoe[:, i, :], g2bc, ALU.mult, eng="pool")
        P.tt(xt[:], xt[:], ymoe[:, i, :], ALU.add)
        P.dma(dst[i * 128:(i + 1) * 128, :], xt[:], semkey=("xt", i % 2))


def final_norm(k, b):
    P, B, I, S = k.P, k.B, k.I, k.S
    fw = B.AF[:, 0:1024]
    P.dma(fw, I.fnw_bc)
    for i in range(TX // 128):
        xt = B.xt[i % 2]
        P.dma(xt[:], S.xcur[b][i * 128:(i + 1) * 128, :], semkey=("xt", i % 2))
        st = B.st[:, (i % 2) * 4:(i % 2) * 4 + 4]
        P.act(B.junk[:], xt[:], AF.Square, accum_out=st[:, 0:1])
        rstd_col(k, st[:, 2:3], st[:, 0:1], D, st[:, 1:2])
        P.stt(xt[:], xt[:], st[:, 2:3], fw, ALU.mult, ALU.mult)
        k.fin.append(P.dma(k.out[b][i * 128:(i + 1) * 128, :], xt[:], semkey=("xt", i % 2)))


def dump_dram(k, name, src, T):
    if name not in k.cfg.get("taps", ()):
        return
    o = k.dout("tap_" + name, [T, D])
    for i in range(T // 128):
        xt = k.B.xt[i % 2]
        k.P.dma(xt[:], src[i * 128:(i + 1) * 128, :], semkey=("xt", i % 2))
        k.fin.append(k.P.dma(o[i * 128:(i + 1) * 128, :], xt[:], semkey=("xt", i % 2)))


def col_layout(v, kk):
    return np.ascontiguousarray(np.asarray(v, np.float32).reshape(kk, 128).T)


def bc_layout(v):
    v = np.asarray(v, np.float32).reshape(1, -1)
    return np.ascontiguousarray(np.broadcast_to(v, (128, v.shape[1])))


def shared_inputs(inp):
    f = lambda a: np.ascontiguousarray(np.asarray(a, np.float32))
    sh = {}
    sh["w_ada"] = f(inp["w_ada"])
    sh["b_ada_col"] = np.stack([col_layout(inp["b_ada"][l], 48) for l in range(NL)])
    sh["n1col"] = np.stack([col_layout(inp["norm1_w"][l], KC) for l in range(NL)])
    sh["n2col"] = np.stack([col_layout(inp["norm2_w"][l], KC) for l in range(NL)])
    sh["fnw_bc"] = bc_layout(inp["final_norm_w"])
    sh["w_in"] = f(inp["w_in"])
    cw = np.asarray(inp["qkv_conv_w"], np.float32)
    sh["convw"] = np.ascontiguousarray(cw.reshape(NL, 5, 24, 128).transpose(0, 3, 2, 1))
    sh["alog_bc"] = np.stack([bc_layout(inp["gdn_a_log"][l].reshape(-1)) for l in range(NL)])
    sh["dtb_bc"] = np.stack([bc_layout(inp["gdn_dt_bias"][l].reshape(-1)) for l in range(NL)])
    sh["gnw_bc"] = np.stack([bc_layout(inp["gdn_norm_w"][l]) for l in range(NL)])
    sh["lnw_bc"] = np.stack([bc_layout(inp["cmlp_ln_w"][l]) for l in range(NL)])
    sh["lnb_bc"] = np.stack([bc_layout(inp["cmlp_ln_b"][l]) for l in range(NL)])
    sh["wsT"] = np.ascontiguousarray(np.asarray(inp["cmlp_w_s"], np.float32).transpose(0, 1, 3, 2))
    sh["bs_col"] = np.ascontiguousarray(np.asarray(inp["cmlp_b_s"], np.float32).transpose(0, 2, 1))
    sh["pool_w"] = f(inp["pool_w"])
    sh["psc_col"] = np.stack([col_layout(inp["pool_scale"][l], 4) for l in range(NL)])
    for nm in ("w_br_a", "w_br_b", "w_br_c", "w_out", "w_router", "w_gate", "w_up", "w_down"):
        sh[nm] = f(inp[nm])
    sh["cst"] = make_consts()
    return sh


def core_inputs(inp, sh, core):
    m = dict(sh)
    b0 = 2 * core
    m["x"] = np.ascontiguousarray(np.asarray(inp["x"][b0:b0 + 2], np.float32))
    m["ctx"] = np.ascontiguousarray(np.asarray(inp["ctx"][b0:b0 + 2], np.float32))
    cc = np.stack([np.asarray(inp["c"][b0], np.float32), np.asarray(inp["c"][b0 + 1], np.float32),
                   np.asarray(inp["c_ctx"], np.float32)], axis=-1)
    m["ccol"] = np.ascontiguousarray(cc.reshape(KC, 128, 3).transpose(1, 0, 2))
    return m


_CACHE = {}


def kernel(**inputs):
    n = 8
    if "k" not in _CACHE:
        _CACHE["k"] = build({"batches": [0, 1], "layers": [0, 1], "streams": ["ctx", "x"],
                             "moe": True, "final": True, "taps": []})
    k = _CACHE["k"]
    sh = shared_inputs(inputs)
    in_maps = [core_inputs(inputs, sh, c) for c in range(n)]
    res = run_bass_kernel_spmd(k.nc, in_maps, core_ids=list(range(n)))
    out = np.concatenate([np.asarray(r["out"], np.float32) for r in res.results], axis=0)
    return out
```

```python
import numpy as np
import concourse.bass as bass
import concourse.mybir as mybir
from concourse.bass_utils import run_bass_kernel_spmd

F32 = mybir.dt.float32
BF16 = mybir.dt.bfloat16
AF = mybir.ActivationFunctionType
ALU = mybir.AluOpType
AX = mybir.AxisListType

ENGS = ("pe", "act", "dve", "pool", "sp")


def _box(ap):
    t = ap.tensor
    name = t.name
    space = str(ap.space)
    dims = ap.ap
    off = ap.offset
    if space == "DRAM":
        lo = off
        hi = off + sum((c - 1) * abs(s) for s, c in dims) + 1
        return (name, 0, 1, lo, hi)
    pstride = dims[0][0]
    if pstride == 0:
        pstride = 1 << 40
    tshape = t.shape
    fsz = 1
    for s in list(tshape)[1:]:
        fsz *= s
    p0 = off // fsz
    f0 = off % fsz
    npart = dims[0][1]
    f1 = f0 + sum((c - 1) * abs(s) for s, c in dims[1:]) + 1
    return (name, p0, p0 + npart, f0, f1)


def _overlap(a, b):
    return a[1] < b[2] and b[1] < a[2] and a[3] < b[4] and b[3] < a[4]


def _contains(a, b):
    return a[1] <= b[1] and b[2] <= a[2] and a[3] <= b[3] and b[4] <= a[4]


class Op:
    __slots__ = ("eng", "fn", "idx", "deps", "is_dma", "semkey", "has_dep", "tick",
                 "group", "pos")

    def __init__(self, eng, fn, is_dma=False, semkey=None):
        self.eng = eng
        self.fn = fn
        self.is_dma = is_dma
        self.semkey = semkey
        self.deps = set()
        self.has_dep = False
        self.tick = None
        self.group = None


class Prog:
    def __init__(self, nc, n_dma_sems=80):
        self.nc = nc
        self.ops = []
        self.acc = {}
        self.n_dma_sems = n_dma_sems
        self.same_engine_sync = True

    def _track(self, op, reads, writes):
        ekey = ("dma", op.semkey) if op.is_dma else op.eng
        preads = [ap for ap in reads if str(ap.space) == "PSUM"]
        reads = [ap for ap in reads if str(ap.space) != "PSUM"]
        writes = list(writes) + preads
        for ap in reads:
            b = _box(ap)
            ent = self.acc.setdefault(b[0], {"w": [], "r": {}})
            for (ob, oop) in ent["w"]:
                if _overlap(ob, b):
                    op.deps.add(oop)
            ent["r"][(b, ekey)] = op
        for ap in writes:
            b = _box(ap)
            if str(ap.space) == "PSUM":
                b = (b[0], 0, 128, 0, 1 << 30)
            ent = self.acc.setdefault(b[0], {"w": [], "r": {}})
            keep = []
            for (ob, oop) in ent["w"]:
                if oop is op:
                    keep.append((ob, oop))
                    continue
                if _overlap(ob, b):
                    op.deps.add(oop)
                    if _contains(b, ob):
                        continue
                keep.append((ob, oop))
            keep.append((b, op))
            ent["w"] = keep
            rk = {}
            for (ob, ek), oop in ent["r"].items():
                if oop is op:
                    rk[(ob, ek)] = oop
                    continue
                if _overlap(ob, b):
                    op.deps.add(oop)
                    if _contains(b, ob):
                        continue
                rk[(ob, ek)] = oop
            ent["r"] = rk
        op.deps.discard(op)

    def add(self, eng, fn, reads=(), writes=()):
        op = Op(eng, fn)
        op.idx = len(self.ops)
        self.ops.append(op)
        self._track(op, reads, writes)
        return op

    def dma(self, out, in_, eng="sp", semkey=None, **kw):
        sb = out if str(out.space) != "DRAM" else in_
        if semkey is None:
            semkey = sb.tensor.name
        op = Op(eng, lambda e, out=out, in_=in_, kw=kw: e.dma_start(out=out, in_=in_, **kw),
                is_dma=True, semkey=semkey)
        op.idx = len(self.ops)
        self.ops.append(op)
        self._track(op, [in_], [out])
        return op

    def mm(self, out, lhsT, rhs, start=True, stop=True, **kw):
        reads = [lhsT, rhs] + ([] if start else [out])
        return self.add("pe", lambda e: e.matmul(out, lhsT, rhs, start=start, stop=stop, **kw),
                        reads, [out])

    def transpose(self, out, in_, ident):
        return self.add("pe", lambda e: e.transpose(out, in_, ident), [in_, ident], [out])

    def act(self, out, in_, func, bias=None, scale=1.0, accum_out=None, eng="act"):
        reads = [in_]
        if bias is not None and not isinstance(bias, (int, float)):
            reads.append(bias)
        if not isinstance(scale, (int, float)):
            reads.append(scale)
        writes = [out] + ([accum_out] if accum_out is not None else [])
        kw = {}
        if accum_out is not None:
            kw["accum_out"] = accum_out
        if bias is not None:
            kw["bias"] = bias
        return self.add(eng, lambda e: e.activation(out=out, in_=in_, func=func, scale=scale, **kw),
                        reads, writes)

    def tt(self, out, in0, in1, op, eng="dve"):
        return self.add(eng, lambda e: e.tensor_tensor(out=out, in0=in0, in1=in1, op=op),
                        [in0, in1], [out])

    def ts(self, out, in0, s1, s2, op0, op1=None, eng="dve", accum_out=None):
        reads = [in0] + [s for s in (s1, s2) if s is not None and not isinstance(s, (int, float))]
        writes = [out] + ([accum_out] if accum_out is not None else [])
        kw = {}
        if op1 is not None:
            kw["op1"] = op1
        if accum_out is not None:
            kw["accum_out"] = accum_out
        return self.add(eng, lambda e: e.tensor_scalar(out=out, in0=in0, scalar1=s1, scalar2=s2,
                                                       op0=op0, **kw), reads, writes)

    def stt(self, out, in0, scalar, in1, op0, op1, eng="dve", accum_out=None):
        reads = [in0, in1] + ([scalar] if not isinstance(scalar, (int, float)) else [])
        writes = [out] + ([accum_out] if accum_out is not None else [])
        kw = {}
        if eng == "pool":
            eng = "dve"
        if accum_out is not None:
            kw["accum_out"] = accum_out
        return self.add(eng, lambda e: e.scalar_tensor_tensor(out=out, in0=in0, scalar=scalar, in1=in1,
                                                              op0=op0, op1=op1, **kw), reads, writes)

    def copy(self, out, in_, eng="dve"):
        if eng == "act":
            return self.add("act", lambda e: e.copy(out=out, in_=in_), [in_], [out])
        return self.add(eng, lambda e: e.tensor_copy(out=out, in_=in_), [in_], [out])

    def memset(self, ap, val, eng="pool"):
        return self.add(eng, lambda e: e.memset(ap, val), [], [ap])

    def reduce(self, out, in_, op, axis=None, eng="dve"):
        axis = axis or AX.X
        return self.add(eng, lambda e: e.tensor_reduce(out=out, in_=in_, axis=axis, op=op), [in_], [out])

    def emit(self, final_wait_ops=()):
        nc = self.nc
        ops = self.ops
        streams = {e: [] for e in ENGS}
        for op in ops:
            op.pos = len(streams[op.eng])
            streams[op.eng].append(op)
        for op in ops:
            red = {}
            dm = []
            for d in op.deps:
                if d.is_dma:
                    dm.append(d)
                else:
                    if d.eng == op.eng and (d.eng == "pe" or not self.same_engine_sync) and not op.is_dma:
                        continue
                    if d.eng not in red or red[d.eng].idx < d.idx:
                        red[d.eng] = d
            op.deps = list(red.values()) + dm
            for d in op.deps:
                d.has_dep = True
        for op in final_wait_ops:
            op.has_dep = True
        LIM = 30000
        cnt = {e: 0 for e in ENGS}
        for op in ops:
            if not op.is_dma and op.has_dep:
                cnt[op.eng] += 1
                op.tick = ((cnt[op.eng] - 1) // LIM, (cnt[op.eng] - 1) % LIM + 1)
        keys = []
        for op in ops:
            if op.is_dma and op.semkey not in keys:
                keys.append(op.semkey)
        nsem = min(len(keys), self.n_dma_sems)
        key2sem = {k: i % max(nsem, 1) for i, k in enumerate(keys)}
        semstate = {}
        dma_wait_prev = {}
        consumers = {}
        for op in ops:
            for d in op.deps:
                if d.is_dma:
                    consumers.setdefault(d, []).append(op)
        first_consumer_idx = {}
        for d, cl in consumers.items():
            first_consumer_idx[d] = min(c.idx for c in cl)
        for op in final_wait_ops:
            if op.is_dma:
                first_consumer_idx.setdefault(op, len(ops))
        groups = []
        for op in ops:
            if not op.is_dma:
                continue
            s = key2sem[op.semkey]
            st = semstate.setdefault(s, {"total": 0, "open": None, "close_at": None, "prev_final": 0})
            g = st["open"]
            if g is not None and st["close_at"] is not None and st["close_at"] <= op.idx:
                st["prev_final"] = g["final"]
                g = None
            if g is None:
                g = {"sem": s, "members": [], "final": st["total"]}
                groups.append(g)
                st["open"] = g
                st["close_at"] = None
                if st["prev_final"] > 0:
                    dma_wait_prev[op] = (s, st["prev_final"])
            st["total"] += 16
            g["members"].append(op)
            g["final"] = st["total"]
            op.group = g
            fc = first_consumer_idx.get(op)
            if fc is not None:
                st["close_at"] = fc if st["close_at"] is None else min(st["close_at"], fc)
        self.stats = {"n_ops": len(ops), "n_dma_sems": nsem, "incs": dict(cnt)}
        from contextlib import ExitStack
        es = ExitStack()
        esem = {}
        for e in ENGS:
            if e == "sp":
                continue
            for gen in range((cnt[e] + LIM - 1) // LIM + 1):
                esem[(e, gen)] = es.enter_context(nc.semaphore("c_%s%d" % (e, gen)))
        dsem = [es.enter_context(nc.semaphore("d_%d" % i)) for i in range(nsem)]
        known = {e: {} for e in ENGS}
        nwaits = 0

        def emit_stream(ename, eobj):
            nonlocal nwaits
            kn = known[ename]
            for op in streams[ename]:
                waits = {}
                for d in op.deps:
                    if d.is_dma:
                        g = d.group
                        key = ("d", g["sem"])
                        val = g["final"]
                    else:
                        key = ("e", d.eng, d.tick[0])
                        val = d.tick[1]
                    if waits.get(key, 0) < val:
                        waits[key] = val
                if op in dma_wait_prev:
                    s, v = dma_wait_prev[op]
                    key = ("d", s)
                    if waits.get(key, 0) < v:
                        waits[key] = v
                for key, val in waits.items():
                    if kn.get(key, 0) >= val:
                        continue
                    kn[key] = val
                    sem = dsem[key[1]] if key[0] == "d" else esem[(key[1], key[2])]
                    eobj.wait_ge(sem, val)
                    nwaits += 1
                ins = op.fn(eobj)
                if op.is_dma:
                    ins.then_inc(dsem[op.group["sem"]], 16)
                elif op.has_dep:
                    ins.then_inc(esem[(ename, op.tick[0])], 1)
            if ename == "sp":
                for op in final_wait_ops:
                    g = op.group
                    eobj.wait_ge(dsem[g["sem"]], g["final"])

        with nc.Block() as block:
            @block.tensor
            def _(e):
                emit_stream("pe", e)

            @block.scalar
            def _(e):
                emit_stream("act", e)

            @block.vector
            def _(e):
                emit_stream("dve", e)

            @block.gpsimd
            def _(e):
                emit_stream("pool", e)

            @block.sync
            def _(e):
                emit_stream("sp", e)
        self.stats["nwaits"] = nwaits
        es.close()


from contextlib import ExitStack

D = 1024
KC = 8
H = 8
NL = 2
OFF_Q, OFF_K, OFF_V, OFF_Z, OFF_B, OFF_A, OFF_U, OFF_VG, OFF_P, OFF_G = (
    0, 1024, 2048, 3072, 4096, 4112, 4128, 4640, 5152, 5664)
IN_COLS = 8736
EPS = 1e-6
NEG = -30000.0
TX = 2048
TC = 256
NE = 16

C_ID, C_U, C_L, C_NMF, C_NMB, C_SL, C_SU, C_ONE = [i * 128 for i in range(8)]
C_IOTA = 1024
C_RCX = 1280
C_RCC = 1536
NCST = 2560


def make_consts():
    c = np.zeros((128, NCST), np.float32)
    i = np.arange(128)[:, None]
    j = np.arange(128)[None, :]
    c[:, C_ID:C_ID + 128] = (i == j)
    c[:, C_U:C_U + 128] = (i <= j)
    c[:, C_L:C_L + 128] = (i >= j)
    c[:, C_NMF:C_NMF + 128] = np.where(i >= j, 0.0, NEG)
    c[:, C_NMB:C_NMB + 128] = np.where(i <= j, 0.0, NEG)
    c[:, C_SL:C_SL + 128] = (i > j)
    c[:, C_SU:C_SU + 128] = (i < j)
    c[:, C_ONE:C_ONE + 128] = 1.0
    c[:, C_IOTA:C_IOTA + 256] = np.arange(1, 257)[None, :]
    for seg, off in ((64, C_RCX), (256, C_RCC)):
        t = np.arange(seg)
        for gi, w in enumerate((2, 4, 8, 16)):
            lo = np.clip(t - w // 2, 0, seg)
            hi = np.clip(t + w // 2, 0, seg)
            c[:, off + gi * seg: off + (gi + 1) * seg] = (1.0 / (hi - lo).astype(np.float32))[None, :]
    return c


def tokblocks(T):
    bs = min(512, T)
    return [(s, bs) for s in range(0, T, bs)]


class K:
    pass


def build(cfg):
    nc = bass.Bass("TRN2", target_bir_lowering=False)
    k = K()
    k.nc = nc
    k.cfg = cfg
    P = Prog(nc)
    k.P = P
    es = ExitStack()
    k.fin = []
    k.dbg = {}

    def din(name, shape, dt=F32):
        return nc.dram_tensor(name, list(shape), dt, kind="ExternalInput").ap()

    def dscr(name, shape, dt=F32):
        return nc.dram_tensor(name, list(shape), dt).ap()

    def dout(name, shape, dt=F32):
        return nc.dram_tensor(name, list(shape), dt, kind="ExternalOutput").ap()

    def sb(name, shape, dt=F32):
        return es.enter_context(nc.sbuf_tensor(name, list(shape), dt))

    def ps(name, shape, dt=F32):
        return es.enter_context(nc.psum_tensor(name, list(shape), dt))

    k.dout = dout
    I = K()
    k.I = I
    I.x = din("x", [2, TX, D])
    I.ctx = din("ctx", [2, TC, D])
    I.ccol = din("ccol", [128, KC, 3])
    I.w_ada = din("w_ada", [NL, D, 6 * D])
    I.b_ada_col = din("b_ada_col", [NL, 128, 48])
    I.n1col = din("n1col", [NL, 128, KC])
    I.n2col = din("n2col", [NL, 128, KC])
    I.fnw_bc = din("fnw_bc", [128, D])
    I.w_in = din("w_in", [NL, D, IN_COLS])
    I.convw = din("convw", [NL, 128, 24, 5])
    I.alog_bc = din("alog_bc", [NL, 128, 16])
    I.dtb_bc = din("dtb_bc", [NL, 128, 16])
    I.gnw_bc = din("gnw_bc", [NL, 128, 128])
    I.lnw_bc = din("lnw_bc", [NL, 128, 512])
    I.lnb_bc = din("lnb_bc", [NL, 128, 512])
    I.wsT = din("wsT", [NL, 4, 128, 128])
    I.bs_col = din("bs_col", [NL, 128, 4])
    I.pool_w = din("pool_w", [NL, 4, 128, 128])
    I.psc_col = din("psc_col", [NL, 128, 4])
    I.w_br_a = din("w_br_a", [NL, 1024, D])
    I.w_br_b = din("w_br_b", [NL, 512, D])
    I.w_br_c = din("w_br_c", [NL, 512, D])
    I.w_out = din("w_out", [NL, D, D])
    I.w_router = din("w_router", [NL, D, NE])
    I.w_gate = din("w_gate", [NL, NE, D, D])
    I.w_up = din("w_up", [NL, NE, D, D])
    I.w_down = din("w_down", [NL, NE, D, D])
    I.cst = din("cst", [128, NCST])
    k.out = dout("out", [2, TX, D])
    S = K()
    k.S = S
    S.xcur = dscr("xcur", [2, TX, D])
    S.ccur = dscr("ccur", [2, TC, D])
    S.gates = dscr("gatesD", [24, 128, TX], BF16)
    B = K()
    k.B = B
    B.cst = sb("cst_sb", [128, 1280])
    B.cstb = sb("cstb", [128, 1024 + 256], BF16)
    B.AB = sb("AB", [128, 49152], BF16)
    B.AF = sb("AF", [128, 16384], F32)
    B.xt = [sb("xt%d" % i, [128, D]) for i in range(2)]
    B.xn = sb("xn", [128, D], BF16)
    B.st = sb("st", [128, 64])
    B.wst = [sb("wst%d" % i, [128, KC, 256], BF16) for i in range(2)]
    B.wsm = [sb("wsm%d" % i, [128, KC, 128], BF16) for i in range(4)]
    B.mod = sb("mod", [128, NL, 3, 48])
    B.modA = sb("modA", [128, NL, 3, 2, KC])
    B.sc3 = sb("sc3", [128, KC, 3])
    B.small = sb("small", [128, 2048])
    B.states = B.AF[:, 10240:12288].rearrange("p (a c) -> p a c", c=128)
    B.gsc = B.AF[:, 12288:14592].rearrange("p (a b c) -> p a b c", a=9, b=16)
    B.junk = sb("junk", [128, D], BF16)
    k.pf = [ps("pf%d" % i, [128, 512]) for i in range(6)]
    k.pb = [ps("pb%d" % i, [128, 1024], BF16) for i in range(2)]

    def cst(off, n=128):
        return B.cst[:, off:off + n]

    def cstb(off, n=128):
        return B.cstb[:, off:off + n]

    k.c = cst
    k.cb = cstb
    P.dma(B.cst[:], I.cst[:, 0:1280])
    P.copy(B.cstb[:, 0:1024], B.cst[:, 0:1024])
    P.copy(B.cstb[:, 1024:1280], B.cst[:, C_IOTA:C_IOTA + 256])
    k.rr = [0]

    prologue(k)
    for b in cfg["batches"]:
        for l in cfg["layers"]:
            last = l == NL - 1
            src_c = I.ctx[b] if l == 0 else S.ccur[b]
            src_x = I.x[b] if l == 0 else S.xcur[b]
            if "ctx" in cfg["streams"]:
                mixer(k, b, l, src_c, S.ccur[b], TC, True, last)
                if not last and cfg.get("moe", True):
                    moe(k, b, l, S.ccur[b], S.ccur[b], TC, True)
                if not last:
                    dump_dram(k, "ccur", S.ccur[b], TC)
            if "x" in cfg["streams"]:
                mixer(k, b, l, src_x, S.xcur[b], TX, False, False)
                if cfg.get("moe", True):
                    moe(k, b, l, S.xcur[b], S.xcur[b], TX, False)
                dump_dram(k, "xcur%d" % l, S.xcur[b], TX)
        if cfg.get("final", True):
            final_norm(k, b)
    P.emit(final_wait_ops=k.fin)
    es.close()
    k.stats = P.stats
    return k


def tap(k, name, ap_sb, shape=None, dt=F32):
    if name not in k.cfg.get("taps", ()):
        return
    shp = list(ap_sb.shape)
    o = k.dout("tap_" + name, shp, ap_sb.dtype)
    k.fin.append(k.P.dma(o, ap_sb, semkey="tap"))


def evac_eng(k):
    k.rr[0] += 1
    return "act" if k.rr[0] % 2 else "dve"


def pcopy(k, out, in_, eng=None):
    eng = eng or evac_eng(k)
    k.P.copy(out, in_, eng=eng)


def prologue(k):
    P, B, I = k.P, k.B, k.I
    raw = B.small[:, 0:24].rearrange("p (a b) -> p a b", a=KC)
    P.dma(raw, I.ccol)
    P.act(B.sc3[:], raw, AF.Silu)
    for l in range(NL):
        wv = I.w_ada[l].rearrange("(kk p) c -> p kk c", p=128)
        acc = k.pf[0][:, 0:144].rearrange("p (j v) -> p j v", v=3)
        for cb in range(12):
            wblk = B.AF[:, (cb % 2) * 4096:(cb % 2) * 4096 + 4096].rearrange("p (a b) -> p a b", a=KC)
            P.dma(wblk, wv[:, :, cb * 512:(cb + 1) * 512], semkey=("wada", cb % 2))
            for jj in range(4):
                j = cb * 4 + jj
                for kk in range(KC):
                    P.mm(acc[:, j, :], wblk[:, kk, jj * 128:(jj + 1) * 128], B.sc3[:, kk, :],
                         start=(kk == 0), stop=(kk == KC - 1))
        bcol = B.small[:, 32:80]
        P.dma(bcol, I.b_ada_col[l])
        for v in range(3):
            P.tt(B.mod[:, l, v, :], acc[:, :, v], bcol, ALU.add)
        n1 = B.small[:, 80:88]
        n2 = B.small[:, 88:96]
        P.dma(n1, I.n1col[l])
        P.dma(n2, I.n2col[l])
        for v in range(3):
            P.stt(B.modA[:, l, v, 0, :], B.mod[:, l, v, 8:16], 1.0, n1, ALU.add, ALU.mult)
            P.stt(B.modA[:, l, v, 1, :], B.mod[:, l, v, 32:40], 1.0, n2, ALU.add, ALU.mult)
    tap(k, "mod", B.mod[:].rearrange("p l v j -> p (l v j)"))


def rstd_col(k, out_col, ss_col, n, tmp_col):
    P = k.P
    P.ts(tmp_col, ss_col, 1.0 / n, EPS, ALU.mult, ALU.add)
    P.act(tmp_col, tmp_col, AF.Sqrt)
    P.add("dve", lambda e: e.reciprocal(out=out_col, in_=tmp_col), [tmp_col], [out_col])


def norm_to_T(k, src, T, Acol, Shcol, hT, xn_tok=None):
    P, B = k.P, k.B
    NT = T // 128
    for i in range(NT):
        xt = B.xt[i % 2]
        P.dma(xt[:], src[i * 128:(i + 1) * 128, :], semkey=("xt", i % 2))
        st = B.st[:, (i % 2) * 4:(i % 2) * 4 + 4]
        P.act(B.junk[:], xt[:], AF.Square, accum_out=st[:, 0:1])
        rstd_col(k, st[:, 2:3], st[:, 0:1], D, st[:, 1:2])
        xn = xn_tok[:, i, :] if xn_tok is not None else B.xn[:]
        P.ts(xn, xt[:], st[:, 2:3], None, ALU.mult)
        if hT is None:
            continue
        pb = k.pb[i % 2]
        for kk in range(KC):
            P.transpose(pb[:, kk * 128:(kk + 1) * 128], xn[:, kk * 128:(kk + 1) * 128], k.cb(C_ID))
        pv = pb[:].rearrange("p (a b) -> p a b", a=KC)
        dst = hT[:, :, i * 128:(i + 1) * 128]
        P.tt(dst, pv, Acol.unsqueeze(2).broadcast_to([128, KC, 128]), ALU.mult)
        P.tt(dst, dst, Shcol.unsqueeze(2).broadcast_to([128, KC, 128]), ALU.add, eng="pool")


def load_w_cols(k, dst, l, c0, ncols, key):
    wv = k.I.w_in[l].rearrange("(kk p) c -> p kk c", p=128)
    k.P.dma(dst, wv[:, :, c0:c0 + ncols], eng="pool", semkey=key)


def proj_fm(k, hT, T, w, ncol, consume):
    P = k.P
    for j in range(ncol // 128):
        for bi, (t0, n) in enumerate(tokblocks(T)):
            pf = k.pf[(j * 4 + bi) % 4]
            for kk in range(KC):
                P.mm(pf[:, 0:n], w[:, kk, j * 128:(j + 1) * 128], hT[:, kk, t0:t0 + n],
                     start=(kk == 0), stop=(kk == KC - 1))
            consume(j, t0, n, pf[:, 0:n])


def mixer(k, b, l, src, dst, T, is_ctx, states_only):
    P, B, I, S = k.P, k.B, k.I, k.S
    NT = T // 128
    v = 2 if is_ctx else b
    AB = B.AB
    h1T = AB[:, 0:KC * T].rearrange("p (a t) -> p a t", a=KC)
    yaT = AB[:, 16384:16384 + 8 * T].rearrange("p (a t) -> p a t", a=8)
    ybT = AB[:, 32768:32768 + 4 * T].rearrange("p (a t) -> p a t", a=4)
    ycT = AB[:, 40960:40960 + 4 * T].rearrange("p (a t) -> p a t", a=4)
    norm_to_T(k, src, T, B.modA[:, l, v, 0, :], B.mod[:, l, v, 0:8], h1T)
    tap(k, "h1T", h1T)
    if not states_only:
        gate_phase(k, l, h1T, T)
    gdn(k, b, l, h1T, yaT, T, is_ctx, states_only)
    if states_only:
        return
    tap(k, "yaT", yaT)
    cmlp(k, l, h1T, ybT, T)
    tap(k, "ybT", ybT)
    pool(k, l, h1T, ycT, T, is_ctx)
    tap(k, "ycT", ycT)
    merge_out(k, b, l, v, src, dst, yaT, ybT, ycT, T)


def gate_phase(k, l, h1T, T):
    P, B, S = k.P, k.B, k.S
    for blk in range(12):
        w = B.wst[blk % 2]
        load_w_cols(k, w[:], l, OFF_G + blk * 256, 256, ("wst", blk % 2))

        def consume(j, t0, n, pap, blk=blk):
            stg = B.AB[:, 49152 - 1024 + (j % 2) * 512:49152 - 1024 + (j % 2) * 512 + n]
            P.act(stg, pap, AF.Sigmoid)
            P.dma(S.gates[blk * 2 + j, :, t0:t0 + n], stg, eng="sp", semkey=("gst", j % 2))
        proj_fm(k, h1T, T, w, 256, consume)


GS_BETA, GS_NBETA, GS_G, GS_GC, GS_EGC, GS_EDEC, GS_ETOT, GS_BEXP, GS_TOT = range(9)


def gdn(k, b, l, h1T, yaT, T, is_ctx, states_only):
    P, B, I = k.P, k.B, k.I
    NT = T // 128
    AB, AFa = B.AB, B.AF
    GB = 32768
    QT = AB[:, GB:GB + T]
    KT = AB[:, GB + 2048:GB + 2048 + T]
    VT = AB[:, GB + 4096:GB + 4096 + T]
    Ktok = AB[:, GB + 6144:GB + 6144 + T].rearrange("p (a c) -> p a c", c=128)
    Vtok = AB[:, GB + 8192:GB + 8192 + T].rearrange("p (a c) -> p a c", c=128)
    zs = AB[:, GB + 10240:GB + 10240 + T].rearrange("p (a c) -> p a c", c=128)
    yatok = AB[:, GB + 12288:GB + 12288 + T].rearrange("p (a c) -> p a c", c=128)
    tb = GB + 14336
    attn_b = AB[:, tb:tb + 128]
    attnT = AB[:, tb + 128:tb + 256]
    wT = AB[:, tb + 256:tb + 384]
    qgT = AB[:, tb + 384:tb + 512]
    kdec = AB[:, tb + 512:tb + 640]
    vnew = AB[:, tb + 640:tb + 768]
    Sbf = AB[:, tb + 768:tb + 896]
    cbuf = AFa[:, 0:T + 4]
    cs = AFa[:, 2052:2052 + T]
    oacc = AFa[:, 4100:4100 + T].rearrange("p (a c) -> p a c", c=128)
    fb = 6148
    Rm = AFa[:, fb:fb + 128]
    dec = AFa[:, fb + 128:fb + 256]
    egr = AFa[:, fb + 256:fb + 384]
    nk = [AFa[:, fb + 384 + i * 128:fb + 512 + i * 128] for i in range(2)]
    Pk = [AFa[:, fb + 640 + i * 128:fb + 768 + i * 128] for i in range(2)]
    Xk = [AFa[:, fb + 896 + i * 128:fb + 1024 + i * 128] for i in range(2)]
    rhsu = AFa[:, fb + 1152:fb + 1280]
    rhsw = AFa[:, fb + 1280:fb + 1408]
    usb = AFa[:, fb + 1408:fb + 1536]
    Sst = AFa[:, fb + 1536:fb + 1664]
    sq = AFa[:, fb:fb + 512]
    rinv = AFa[:, fb + 512:fb + 1024]
    tmpf = AFa[:, fb + 2688:fb + 2688 + 128]
    ident, identb = k.c(C_ID), k.cb(C_ID)
    ones = k.c(C_ONE)
    gs = B.gsc

    wba = B.wsm[0]
    load_w_cols(k, wba[:, :, 0:32], l, OFF_B, 32, ("wsm", 0))
    sm = B.small
    alog = sm[:, 128:144]
    dtb = sm[:, 144:160]
    negA = sm[:, 160:176]
    gnw = sm[:, 256:384]
    P.dma(alog, I.alog_bc[l])
    P.dma(dtb, I.dtb_bc[l])
    P.dma(gnw, I.gnw_bc[l])
    cw = sm[:, 384:504].rearrange("p (a t) -> p a t", t=5)
    P.dma(cw, I.convw[l])
    P.act(negA, alog, AF.Exp)
    P.ts(negA, negA, -1.0, None, ALU.mult)
    for i in range(NT):
        pf = k.pf[4]
        for kk in range(KC):
            P.mm(pf[:, 0:32], h1T[:, kk, i * 128:(i + 1) * 128], wba[:, kk, 0:32],
                 start=(kk == 0), stop=(kk == KC - 1))
        P.act(gs[:, GS_BETA, i, :], pf[:, 0:16], AF.Sigmoid)
        P.tt(gs[:, GS_G, i, :], pf[:, 16:32], dtb, ALU.add)
    nt = slice(0, NT)
    P.ts(gs[:, GS_NBETA, nt, :], gs[:, GS_BETA, nt, :], -1.0, None, ALU.mult)
    P.act(gs[:, GS_G, nt, :], gs[:, GS_G, nt, :], AF.Exp)
    P.act(gs[:, GS_G, nt, :], gs[:, GS_G, nt, :], AF.Ln, bias=1.0)
    P.tt(gs[:, GS_G, nt, :], gs[:, GS_G, nt, :], negA.unsqueeze(1).broadcast_to([128, NT, 16]), ALU.mult)
    for i in range(NT):
        pf = k.pf[4]
        P.mm(pf[:, 0:8], k.c(C_U), gs[:, GS_G, i, 0:8])
        P.mm(pf[:, 8:16], k.c(C_L), gs[:, GS_G, i, 8:16])
        P.mm(pf[:, 16:32], ones, gs[:, GS_G, i, :])
        pcopy(k, gs[:, GS_GC, i, :], pf[:, 0:16], "dve")
        pcopy(k, gs[:, GS_TOT, i, :], pf[:, 16:32], "act")
    P.act(gs[:, GS_EGC, nt, :], gs[:, GS_GC, nt, :], AF.Exp)
    P.act(gs[:, GS_ETOT, nt, :], gs[:, GS_TOT, nt, :], AF.Exp)
    P.tt(gs[:, GS_EDEC, nt, :], gs[:, GS_TOT, nt, :], gs[:, GS_GC, nt, :], ALU.subtract)
    P.act(gs[:, GS_EDEC, nt, :], gs[:, GS_EDEC, nt, :], AF.Exp)
    P.tt(gs[:, GS_BEXP, nt, :], gs[:, GS_BETA, nt, :], gs[:, GS_EGC, nt, :], ALU.mult)
    tap(k, "gsc", gs)

    P.memset(cbuf[:, 0:2], 0.0)
    P.memset(cbuf[:, T + 2:T + 4], 0.0)
    for h in range(H):
        for ci, (off, dstT) in enumerate(((OFF_Q, QT), (OFF_K, KT), (OFF_V, VT))):
            w = B.wsm[1 + ci]
            load_w_cols(k, w[:], l, off + h * 128, 128, ("wsm", 1 + ci))

            def consume(j, t0, n, pap):
                pcopy(k, cbuf[:, 2 + t0:2 + t0 + n], pap)
            proj_fm(k, h1T, T, w, 128, consume)
            ce = "dve" if ci != 1 else "pool"
            cwc = cw[:, ci * 8 + h, :]
            P.ts(cs, cbuf[:, 0:T], cwc[:, 0:1], None, ALU.mult, eng=ce)
            for tp in range(1, 5):
                P.stt(cs, cbuf[:, tp:tp + T], cwc[:, tp:tp + 1], cs, ALU.mult, ALU.add, eng=ce)
            if ci == 2:
                P.act(dstT, cs, AF.Silu)
                continue
            P.act(cs, cs, AF.Silu)
            for (t0, n) in tokblocks(T):
                P.act(sq[:, 0:n], cs[:, t0:t0 + n], AF.Square)
                pf = k.pf[5]
                P.mm(pf[:, 0:n], ones, sq[:, 0:n])
                P.ts(rinv[:, 0:n], pf[:, 0:n], EPS, None, ALU.add)
                P.act(rinv[:, 0:n], rinv[:, 0:n], AF.Sqrt)
                P.add("dve", lambda e, n=n: e.reciprocal(out=rinv[:, 0:n], in_=rinv[:, 0:n]),
                      [rinv[:, 0:n]], [rinv[:, 0:n]])
                if ci == 0:
                    P.stt(dstT[:, t0:t0 + n], cs[:, t0:t0 + n], 128.0 ** -0.5, rinv[:, 0:n], ALU.mult, ALU.mult)
                else:
                    P.tt(dstT[:, t0:t0 + n], cs[:, t0:t0 + n], rinv[:, 0:n], ALU.mult)
        for (srcT, dtok) in ((KT, Ktok), (VT, Vtok)):
            for i0 in range(0, NT, 8):
                pb = k.pb[(i0 // 8) % 2]
                ni = min(8, NT - i0)
                for i in range(ni):
                    P.transpose(pb[:, i * 128:(i + 1) * 128], srcT[:, (i0 + i) * 128:(i0 + i + 1) * 128], identb)
                pcopy(k, dtok[:, i0:i0 + ni, :], pb[:, 0:ni * 128].rearrange("p (a c) -> p a c", c=128))
        if h == 0:
            tap(k, "QT0", QT)
            tap(k, "KT0", KT)
            tap(k, "Vtok0", Vtok)
        if not states_only:
            wz = B.wsm[0]
            load_w_cols(k, wz[:], l, OFF_Z + h * 128, 128, ("wsm", 0))
            for i in range(NT):
                pf = k.pf[4]
                for kk in range(KC):
                    P.mm(pf[:, 0:128], h1T[:, kk, i * 128:(i + 1) * 128], wz[:, kk, :],
                         start=(kk == 0), stop=(kk == KC - 1))
                P.act(zs[:, i, :], pf[:, 0:128], AF.Silu)
        P.memset(oacc, 0.0)
        negU = AFa[:, fb + 3328:fb + 3456]
        negL = AFa[:, fb + 3456:fb + 3584]
        if h == 0:
            P.ts(negU, k.c(C_U), -1.0, None, ALU.mult)
            P.ts(negL, k.c(C_L), -1.0, None, ALU.mult)

        def chain(dr):
            col = dr * 8 + h
            nmsk = negU if dr == 0 else negL
            nm = k.c(C_NMF) if dr == 0 else k.c(C_NMB)
            sm_ = k.c(C_SL) if dr == 0 else k.c(C_SU)
            fo = fb + dr * 1664
            Rm = AFa[:, fo:fo + 128]
            dec = AFa[:, fo + 128:fo + 256]
            egr = AFa[:, fo + 256:fo + 384]
            nk = [AFa[:, fo + 384 + i * 128:fo + 512 + i * 128] for i in range(2)]
            Pk = [AFa[:, fo + 640 + i * 128:fo + 768 + i * 128] for i in range(2)]
            Xk = [AFa[:, fo + 896 + i * 128:fo + 1024 + i * 128] for i in range(2)]
            rhsu = AFa[:, fo + 1152:fo + 1280]
            rhsw = AFa[:, fo + 1280:fo + 1408]
            usb = AFa[:, fo + 1408:fo + 1536]
            Sst = AFa[:, fo + 1536:fo + 1664]
            bo = tb + dr * 896
            attn_b = AB[:, bo:bo + 128]
            attnT = AB[:, bo + 128:bo + 256]
            wT = AB[:, bo + 256:bo + 384]
            qgT = AB[:, bo + 384:bo + 512]
            kdec = AB[:, bo + 512:bo + 640]
            vnew = AB[:, bo + 640:bo + 768]
            Sbf = AB[:, bo + 768:bo + 896]
            b0, b1, b2 = k.pf[dr * 3], k.pf[dr * 3 + 1], k.pf[dr * 3 + 2]
            pbx = k.pb[dr]
            if is_ctx:
                P.memset(Sst, 0.0)
            else:
                P.copy(Sst, B.states[:, col, :], eng="pool")
            P.copy(Sbf, Sst, eng="pool")
            yield
            order = range(NT) if dr == 0 else range(NT - 1, -1, -1)
            for c in order:
                tsl = slice(c * 128, (c + 1) * 128)
                sc = lambda kind: gs[:, kind, c, col:col + 1]
                P.act(Rm, nmsk, AF.Identity, scale=sc(GS_G))
                P.mm(b1[:, 0:128], KT[:, tsl], KT[:, tsl])
                P.mm(b1[:, 128:256], QT[:, tsl], KT[:, tsl])
                P.mm(b0[:, 0:128], ones, Rm)
                yield
                P.act(egr, b0[:, 0:128], AF.Exp, scale=-1.0)
                P.tt(dec, b0[:, 0:128], nm, ALU.add)
                yield
                P.act(dec, dec, AF.Exp, bias=sc(GS_GC))
                P.act(rhsu, Vtok[:, c, :], AF.Identity, scale=sc(GS_BETA))
                P.act(rhsw, Ktok[:, c, :], AF.Identity, scale=sc(GS_BEXP))
                yield
                P.stt(nk[0], b1[:, 0:128], sc(GS_NBETA), dec, ALU.mult, ALU.mult)
                P.tt(attn_b, b1[:, 128:256], dec, ALU.mult)
                yield
                P.tt(nk[0], nk[0], sm_, ALU.mult)
                P.transpose(pbx[:, 0:128], attn_b, identb)
                yield
                P.transpose(b2[:, 0:128], nk[0], ident)
                pcopy(k, attnT, pbx[:, 0:128], "act")
                yield
                pcopy(k, Pk[0], b2[:, 0:128], "act")
                P.tt(Xk[0], b2[:, 0:128], ident, ALU.add)
                P.tt(qgT, QT[:, tsl], egr, ALU.mult)
                P.act(kdec, Ktok[:, c, :], AF.Identity, scale=sc(GS_EDEC))
                yield
                cur = 0
                for lev in range(1, 7):
                    nxt = 1 - cur
                    P.mm(b2[:, 0:128], Pk[cur], nk[cur])
                    if lev < 6:
                        P.mm(b2[:, 128:256], nk[cur], Pk[cur])
                    yield
                    pcopy(k, nk[nxt], b2[:, 0:128], "act")
                    if lev < 6:
                        pcopy(k, Pk[nxt], b2[:, 128:256], "dve")
                    yield
                    P.mm(b0[:, 0:128], nk[nxt], Xk[cur])
                    yield
                    P.tt(Xk[nxt], Xk[cur], b0[:, 0:128], ALU.add)
                    yield
                    cur = nxt
                X = Xk[cur]
                P.mm(b1[:, 128:256], X, rhsu)
                P.mm(b1[:, 256:384], rhsw, X)
                yield
                pcopy(k, usb, b1[:, 128:256], "act")
                pcopy(k, wT, b1[:, 256:384], "dve")
                yield
                P.mm(b0[:, 256:384], wT, Sbf)
                yield
                P.tt(vnew, usb, b0[:, 256:384], ALU.subtract)
                yield
                P.mm(b1[:, 0:128], qgT, Sbf, start=True, stop=False)
                P.mm(b1[:, 0:128], attnT, vnew, start=False, stop=True)
                P.mm(b2[:, 0:128], kdec, vnew)
                yield
                P.tt(oacc[:, c, :], oacc[:, c, :], b1[:, 0:128], ALU.add)
                P.stt(Sst, Sst, sc(GS_ETOT), b2[:, 0:128], ALU.mult, ALU.add)
                yield
                P.copy(Sbf, Sst, eng="act")
                yield
            if is_ctx:
                P.copy(B.states[:, col, :], Sst, eng="pool")

        gens = [chain(0), chain(1)]
        while gens:
            for g_ in list(gens):
                try:
                    next(g_)
                except StopIteration:
                    gens.remove(g_)
        if states_only:
            continue
        if h == 0:
            tap(k, "oacc0", oacc)
        P.act(cs[:, 0:T].rearrange("p (a c) -> p a c", c=128), oacc, AF.Square)
        ssv = B.st[:, 16:16 + NT]
        P.reduce(ssv, cs[:, 0:T].rearrange("p (a c) -> p a c", c=128), ALU.add)
        P.ts(ssv, ssv, 1.0 / 128, EPS, ALU.mult, ALU.add)
        P.act(ssv, ssv, AF.Sqrt)
        P.add("dve", lambda e, ssv=ssv: e.reciprocal(out=ssv, in_=ssv), [ssv], [ssv])
        P.tt(oacc, oacc, ssv.unsqueeze(2).broadcast_to([128, NT, 128]), ALU.mult)
        P.tt(oacc, oacc, gnw.unsqueeze(1).broadcast_to([128, NT, 128]), ALU.mult, eng="pool")
        P.tt(yatok, oacc, zs, ALU.mult)
        for i0 in range(0, NT, 8):
            pb = k.pb[(i0 // 8) % 2]
            ni = min(8, NT - i0)
            for i in range(ni):
                P.transpose(pb[:, i * 128:(i + 1) * 128], yatok[:, i0 + i, :], identb)
            pcopy(k, yaT[:, h, i0 * 128:(i0 + ni) * 128], pb[:, 0:ni * 128])
    if is_ctx:
        tap(k, "states", B.states)


def gelu_tanh(k, out, in_, tmp, eng="dve"):
    P = k.P
    P.act(tmp, in_, AF.Square)
    P.ts(tmp, tmp, 0.044715, 1.0, ALU.mult, ALU.add, eng=eng)
    P.tt(tmp, tmp, in_, ALU.mult, eng=eng)
    P.act(tmp, tmp, AF.Sigmoid, scale=1.5957691216057308)
    P.tt(out, tmp, in_, ALU.mult, eng=eng)


def cmlp(k, l, h1T, ybT, T):
    P, B, I = k.P, k.B, k.I
    NT = T // 128
    AF_ = B.AF
    for q in range(2):
        load_w_cols(k, B.wst[q][:], l, OFF_U + q * 256, 256, ("wst", q))
    for q in range(4):
        load_w_cols(k, B.wsm[q][:], l, OFF_VG + q * 128, 128, ("wsm", q))
    sm = B.small
    lnw = sm[:, 512:1024]
    lnb = sm[:, 1024:1536]
    P.dma(lnw, I.lnw_bc[l])
    P.dma(lnb, I.lnb_bc[l])
    bs = sm[:, 176:180]
    P.dma(bs, I.bs_col[l])
    wsT = AF_[:, 8192:8192 + 512].rearrange("p (g c) -> p g c", g=4)
    P.dma(wsT, I.wsT[l].rearrange("g q p -> q g p"))
    ub = AF_[:, 0:512]
    vb = AF_[:, 512:1024]
    t1 = AF_[:, 1024:1536]
    t2 = AF_[:, 1536:2048]
    ybb = B.junk[:, 0:512]
    for i in range(NT):
        pu = k.pf[0]
        pv = k.pf[1]
        for q in range(2):
            for kk in range(KC):
                P.mm(pu[:, q * 256:(q + 1) * 256], h1T[:, kk, i * 128:(i + 1) * 128], B.wst[q][:, kk, :],
                     start=(kk == 0), stop=(kk == KC - 1))
        for q in range(4):
            for kk in range(KC):
                P.mm(pv[:, q * 128:(q + 1) * 128], h1T[:, kk, i * 128:(i + 1) * 128], B.wsm[q][:, kk, :],
                     start=(kk == 0), stop=(kk == KC - 1))
        pcopy(k, ub, pu[:, 0:512], "act")
        pcopy(k, vb, pv[:, 0:512], "dve")
        gelu_tanh(k, ub, ub, t1, eng="dve")
        gelu_tanh(k, vb, vb, t2, eng="dve")
        st = B.st[:, 32:40]
        P.reduce(st[:, 0:1], vb, ALU.add)
        P.ts(st[:, 1:2], st[:, 0:1], -1.0 / 512, None, ALU.mult)
        P.ts(vb, vb, st[:, 1:2], None, ALU.add)
        P.act(t2, vb, AF.Square, accum_out=st[:, 2:3])
        rstd_col(k, st[:, 4:5], st[:, 2:3], 512, st[:, 3:4])
        P.ts(vb, vb, st[:, 4:5], None, ALU.mult)
        P.tt(vb, vb, lnw, ALU.mult, eng="pool")
        P.tt(vb, vb, lnb, ALU.add, eng="pool")
        pm = k.pf[2]
        for g in range(4):
            P.mm(pm[:, g * 128:(g + 1) * 128], wsT[:, g, :], vb[:, g * 128:(g + 1) * 128])
        for g in range(4):
            P.stt(ybb[:, g * 128:(g + 1) * 128], pm[:, g * 128:(g + 1) * 128], bs[:, g:g + 1],
                  ub[:, g * 128:(g + 1) * 128], ALU.add, ALU.mult)
        pb = k.pb[i % 2]
        for g in range(4):
            P.transpose(pb[:, g * 128:(g + 1) * 128], ybb[:, g * 128:(g + 1) * 128], k.cb(C_ID))
        pcopy(k, ybT[:, :, i * 128:(i + 1) * 128], pb[:, 0:512].rearrange("p (g c) -> p g c", g=4))


def pool(k, l, h1T, ycT, T, is_ctx):
    P, B, I = k.P, k.B, k.I
    seg = 256 if is_ctx else 64
    nseg = T // seg
    W = seg + 32
    AF_ = B.AF
    sm = B.small
    psc = sm[:, 180:184]
    P.dma(psc, I.psc_col[l])
    pw = AF_[:, 8192:8192 + 512].rearrange("p (g c) -> p g c", g=4)
    P.dma(pw, I.pool_w[l].rearrange("g c d -> c g d"))
    pwb = B.junk[:, 0:512].rearrange("p (g c) -> p g c", g=4)
    P.copy(pwb, pw, eng="pool")
    rc0 = C_RCC if is_ctx else C_RCX
    rcs = sm[:, 512:512 + 4 * seg]
    P.dma(rcs, I.cst[:, rc0:rc0 + 4 * seg])
    bufs = [AF_[:, i * 3072:(i + 1) * 3072][:, 0:nseg * W].rearrange("p (s w) -> p s w", w=W) for i in range(2)]
    pin = AF_[:, 6144:6144 + T].rearrange("p (s w) -> p s w", w=seg)
    pooled = AF_[:, 8704:8704 + T]
    pooledb = B.AB[:, 49152 - 2048:49152 - 2048 + T]
    for g in range(4):
        def consume(j, t0, n, pap):
            pcopy(k, AF_[:, 6144 + t0:6144 + t0 + n], pap)
        w = B.wsm[g]
        load_w_cols(k, w[:], l, OFF_P + g * 128, 128, ("wsm", g))
        proj_fm(k, h1T, T, w, 128, consume)
        a, bb = bufs
        P.memset(a, 0.0)
        P.memset(bb, 0.0, eng="dve")
        P.copy(a[:, :, 16:16 + seg], pin, eng="pool")
        lo, hi = 8, seg + 24
        P.tt(bb[:, :, lo:hi], a[:, :, lo:hi], a[:, :, lo - 1:hi - 1], ALU.add)
        cur, oth = bb, a
        for lev in range(g):
            sh = 1 << lev
            P.tt(oth[:, :, lo:hi], cur[:, :, lo - sh:hi - sh], cur[:, :, lo + sh:hi + sh], ALU.add, eng="pool")
            cur, oth = oth, cur
        rc = rcs[:, g * seg:(g + 1) * seg]
        pv = pooled.rearrange("p (s w) -> p s w", w=seg)
        P.tt(pv, cur[:, :, 16:16 + seg], rc.unsqueeze(1).broadcast_to([128, nseg, seg]), ALU.mult)
        P.tt(pooledb.rearrange("p (s w) -> p s w", w=seg), pv, pin, ALU.subtract)
        for bi, (t0, n) in enumerate(tokblocks(T)):
            pf = k.pf[4 + bi % 2]
            P.mm(pf[:, 0:n], pwb[:, g, :], pooledb[:, t0:t0 + n])
            P.act(ycT[:, g, t0:t0 + n], pf[:, 0:n], AF.Identity, scale=psc[:, g:g + 1])


def bcast_row(k, out_bc, col8, dg=None):
    P = k.P
    for half in range(2):
        pf = k.pf[half]
        for kk in range(4):
            c = half * 4 + kk
            if dg is None:
                dg = k.B.AF[:, 16384 - 128:16384]
            P.ts(dg, k.c(C_ID), col8[:, c:c + 1], None, ALU.mult)
            P.mm(pf[:, kk * 128:(kk + 1) * 128], k.c(C_ONE), dg)
        pcopy(k, out_bc[:, half * 512:(half + 1) * 512], pf[:, 0:512])


def merge_out(k, b, l, v, src, dst, yaT, ybT, ycT, T):
    P, B, I, S = k.P, k.B, k.I, k.S
    NT = T // 128
    AF_ = B.AF
    mT = B.AB[:, 0:KC * T].rearrange("p (a t) -> p a t", a=KC)
    g1bc = AF_[:, 0:1024]
    bcast_row(k, g1bc, B.mod[:, l, v, 16:24])
    for dc in range(KC):
        wbr = B.wsm[dc % 2]
        wbr2 = B.wsm[2 + dc % 2]
        P.dma(wbr[:], I.w_br_a[l].rearrange("(kk p) c -> p kk c", p=128)[:, :, dc * 128:(dc + 1) * 128],
              eng="pool", semkey=("wsm", dc % 2))
        P.dma(wbr2[:, 0:4, :], I.w_br_b[l].rearrange("(kk p) c -> p kk c", p=128)[:, :, dc * 128:(dc + 1) * 128],
              eng="pool", semkey=("wsm", 2 + dc % 2))
        P.dma(wbr2[:, 4:8, :], I.w_br_c[l].rearrange("(kk p) c -> p kk c", p=128)[:, :, dc * 128:(dc + 1) * 128],
              eng="pool", semkey=("wsm", 2 + dc % 2))
        for bi, (t0, n) in enumerate(tokblocks(T)):
            gts = [AF_[:, 12288 + a * 512:12288 + a * 512 + n] for a in range(3)]
            gtb = [B.junk[:, 0:512], B.junk[:, 512:1024], B.xn[:, 0:512]]
            for a in range(3):
                P.dma(gtb[a][:, 0:n], S.gates[a * 8 + dc, :, t0:t0 + n], semkey=("gld", a))
            acc = AF_[:, 14336:14336 + n]
            for a, (yT, nk_, wsrc, koff) in enumerate(((yaT, 8, wbr, 0), (ybT, 4, wbr2, 0), (ycT, 4, wbr2, 4))):
                pf = k.pf[a]
                for kk in range(nk_):
                    P.mm(pf[:, 0:n], wsrc[:, koff + kk, :], yT[:, kk, t0:t0 + n], start=(kk == 0), stop=(kk == nk_ - 1))
                if a == 0:
                    P.tt(acc, pf[:, 0:n], gtb[a][:, 0:n], ALU.mult)
                else:
                    P.tt(gts[a], pf[:, 0:n], gtb[a][:, 0:n], ALU.mult)
                    P.tt(acc, acc, gts[a], ALU.add)
            P.copy(mT[:, dc, t0:t0 + n], acc, eng="act")
    wv = I.w_out[l].rearrange("(kk p) c -> p kk c", p=128)
    for q in range(4):
        wdst = B.wst[q % 2]
        cs_ = slice(q * 256, (q + 1) * 256)
        for kk in range(KC):
            stg = AF_[:, 2048 + (kk % 2) * 256:2048 + (kk % 2) * 256 + 256]
            P.dma(stg, wv[:, kk, cs_], semkey=("wo", kk % 2))
            P.tt(wdst[:, kk, :], stg, g1bc[:, cs_], ALU.mult)
        for i in range(NT):
            xq = B.xt[i % 2][:, cs_]
            P.dma(xq, src[i * 128:(i + 1) * 128, cs_], semkey=("xt", i % 2))
            pf = k.pf[4 + i % 2]
            for kk in range(KC):
                P.mm(pf[:, 0:256], mT[:, kk, i * 128:(i + 1) * 128], wdst[:, kk, :],
                     start=(kk == 0), stop=(kk == KC - 1))
            P.tt(xq, xq, pf[:, 0:256], ALU.add)
            P.dma(dst[i * 128:(i + 1) * 128, cs_], xq, semkey=("xt", i % 2))


def moe(k, b, l, src, dst, T, is_ctx):
    P, B, I = k.P, k.B, k.I
    NT = T // 128
    cap = 2 * T // NE
    v = 2 if is_ctx else b
    AB, AF_ = B.AB, B.AF
    nsh = (cap + 127) // 128
    sp_ = min(cap, 128)
    xn_tok = AB[:, 0:NT * 1024].rearrange("p (a d) -> p a d", d=1024)
    Sel = AB[:, 16384:16384 + NT * cap].rearrange("p (a s) -> p a s", s=cap)
    SelT = AB[:, 20480:20480 + nsh * T].rearrange("p (a t) -> p a t", a=nsh)
    xeT = AB[:, 24576:24576 + KC * cap].rearrange("p (a s) -> p a s", a=KC)
    hT = AB[:, 26624:26624 + KC * cap].rearrange("p (a s) -> p a s", a=KC)
    ye = AB[:, 28672:28672 + nsh * 1024].rearrange("p (a d) -> p a d", a=nsh)
    wring = [AB[:, 30720 + i * 2048:30720 + (i + 1) * 2048] for i in range(8)]
    ymoe = AF_[:, 0:NT * 1024].rearrange("p (a d) -> p a d", d=1024)
    sm = B.small
    A2 = B.modA[:, l, v, 1, :]
    sh2 = B.mod[:, l, v, 24:32]
    ident, identb = k.c(C_ID), k.cb(C_ID)
    wr = AF_[:, 7168:7296].rearrange("p (a e) -> p a e", a=KC)
    P.dma(wr, I.w_router[l].rearrange("(kk p) e -> p kk e", p=128))
    wrs = AF_[:, 7296:7424].rearrange("p (a e) -> p a e", a=KC)
    P.tt(wrs, wr, A2.unsqueeze(2).broadcast_to([128, KC, NE]), ALU.mult)
    pbias = k.pf[5]
    rep = AF_[:, 7424:7552]
    for kk in range(KC):
        P.ts(rep, k.c(C_ONE), sh2[:, kk:kk + 1], None, ALU.mult)
        P.mm(pbias[:, 0:NE], rep, wr[:, kk, :], start=(kk == 0), stop=(kk == KC - 1))
    rbias = AF_[:, 7552:7568]
    P.copy(rbias, pbias[:, 0:NE])
    afft = sm[:, 1024:1024 + NT * NE].rearrange("p (a e) -> p a e", e=NE)
    maskf = sm[:, 1280:1280 + NT * NE].rearrange("p (a e) -> p a e", e=NE)
    pref = sm[:, 1536:1536 + NT * NE].rearrange("p (a e) -> p a e", e=NE)
    twc = sm[:, 1792:1800]
    xT = AF_[:, 2048:3072].rearrange("p (a c) -> p a c", a=KC)
    xnf = B.xt[0]
    for i in range(NT):
        xt = B.xt[1]
        P.dma(xt[:], src[i * 128:(i + 1) * 128, :], semkey=("xt", 1))
        st = B.st[:, 40:48]
        P.act(B.junk[:], xt[:], AF.Square, accum_out=st[:, 0:1])
        rstd_col(k, st[:, 2:3], st[:, 0:1], D, st[:, 1:2])
        P.ts(xnf[:], xt[:], st[:, 2:3], None, ALU.mult)
        P.copy(xn_tok[:, i, :], xnf[:], eng="act")
        for half in range(2):
            pt = k.pf[half]
            for kk in range(4):
                c = half * 4 + kk
                P.transpose(pt[:, kk * 128:(kk + 1) * 128], xnf[:, c * 128:(c + 1) * 128], ident)
            pcopy(k, xT[:, half * 4:half * 4 + 4, :], pt[:, 0:512].rearrange("p (a c) -> p a c", a=4))
        plog = k.pf[4]
        for kk in range(KC):
            P.mm(plog[:, 0:NE], xT[:, kk, :], wrs[:, kk, :], start=(kk == 0), stop=(kk == KC - 1))
        lg = B.st[:, 48:64]
        P.tt(lg, plog[:, 0:NE], rbias, ALU.add)
        P.reduce(st[:, 3:4], lg, ALU.max)
        P.ts(st[:, 4:5], st[:, 3:4], -1.0, None, ALU.mult)
        P.act(lg, lg, AF.Exp, bias=st[:, 4:5], accum_out=st[:, 5:6])
        P.add("dve", lambda e, st=st: e.reciprocal(out=st[:, 6:7], in_=st[:, 5:6]), [st[:, 5:6]], [st[:, 6:7]])
        P.ts(afft[:, i, :], lg, st[:, 6:7], None, ALU.mult)
    tap(k, "aff", afft)
    affE = AF_[0:NE, 3072:3072 + T]
    wk = AF_[0:NE, 5120:5120 + T]
    for i0 in range(0, NT, 4):
        pt = k.pf[0]
        ni = min(4, NT - i0)
        for i in range(ni):
            P.transpose(pt[0:NE, i * 128:(i + 1) * 128], afft[:, i0 + i, :], ident)
        pcopy(k, affE[:, i0 * 128:(i0 + ni) * 128], pt[0:NE, 0:ni * 128])
    m8 = B.st[0:NE, 8:16]
    for r in range(cap // 8):
        srcv = affE if r == 0 else wk
        P.add("dve", lambda e, srcv=srcv: e.max(out=m8, in_=srcv), [srcv], [m8])
        P.add("dve", lambda e, srcv=srcv: e.match_replace(out=wk, in_to_replace=m8, in_values=srcv, imm_value=-1.0),
              [m8, srcv], [wk])
    maskE = B.wst[0][:].rearrange("p a c -> p (a c)")[0:NE, 0:T]
    P.tt(maskE, affE, wk, ALU.not_equal)
    pm = k.pb[0]
    for i in range(NT):
        P.transpose(pm[:, i * NE:(i + 1) * NE], maskE[:, i * 128:(i + 1) * 128], identb[0:NE, 0:NE])
    maskb = B.junk[:, 0:NT * NE].rearrange("p (a e) -> p a e", e=NE)
    pcopy(k, maskb, pm[:, 0:NT * NE].rearrange("p (a e) -> p a e", e=NE), "act")
    pcopy(k, maskf, pm[:, 0:NT * NE].rearrange("p (a e) -> p a e", e=NE), "dve")
    tap(k, "mask", maskf)
    hl = B.junk[:, 256:256 + NT * NE * 2].rearrange("p (a e h) -> p a e h", e=NE, h=2)
    P.copy(hl[:, :, :, 0], afft)
    hif = B.st[:, 48:64]
    for i in range(NT):
        P.copy(hif, hl[:, i, :, 0], eng="pool")
        P.tt(hl[:, i, :, 1], afft[:, i, :], hif, ALU.subtract, eng="pool")
    for i in range(NT):
        pp = k.pf[1]
        for i2 in range(i + 1):
            P.mm(pp[:, 0:NE], k.cb(C_U) if i2 == i else k.cb(C_ONE), maskb[:, i2, :],
                 start=(i2 == 0), stop=(i2 == i))
        pcopy(k, pref[:, i, :], pp[:, 0:NE], "dve")
    g2bc = None
    for e in range(NE):
        for i in range(NT):
            P.ts(Sel[:, i, :], k.c(C_IOTA, cap), pref[:, i, e:e + 1], maskf[:, i, e:e + 1],
                 ALU.is_equal, ALU.mult)
        for hh in range(nsh):
            for i0 in range(0, NT, 8):
                pb = k.pb[(i0 // 8) % 2]
                ni = min(8, NT - i0)
                for i in range(ni):
                    P.transpose(pb[0:sp_, i * 128:(i + 1) * 128], Sel[:, i0 + i, hh * 128:hh * 128 + sp_], identb)
                pcopy(k, SelT[0:sp_, hh, i0 * 128:(i0 + ni) * 128], pb[0:sp_, 0:ni * 128])
        for kk in range(KC):
            pg = k.pf[kk % 2]
            for i in range(NT):
                P.mm(pg[:, 0:cap], xn_tok[:, i, kk * 128:(kk + 1) * 128], Sel[:, i, :],
                     start=(i == 0), stop=(i == NT - 1))
            P.act(xeT[:, kk, :], pg[:, 0:cap], AF.Identity, scale=A2[:, kk:kk + 1], bias=sh2[:, kk:kk + 1])
        for hh in range(nsh):
            pw_ = k.pf[2]
            for i in range(NT):
                P.mm(pw_[0:sp_, hh * 2:hh * 2 + 2], Sel[:, i, hh * 128:hh * 128 + sp_], hl[:, i, e, :],
                     start=(i == 0), stop=(i == NT - 1))
            P.reduce(twc[0:sp_, hh:hh + 1], pw_[0:sp_, hh * 2:hh * 2 + 2], ALU.add)
        wg_v = I.w_gate[l, e].rearrange("(kk p) f -> p kk f", p=128)
        wu_v = I.w_up[l, e].rearrange("(kk p) f -> p kk f", p=128)
        wd_v = I.w_down[l, e].rearrange("(m p) d -> p m d", p=128)
        for m2 in range(4):
            gp = wring[(m2 % 2) * 2].rearrange("p (a c) -> p a c", a=KC)
            up = wring[(m2 % 2) * 2 + 1].rearrange("p (a c) -> p a c", a=KC)
            P.dma(gp, wg_v[:, :, m2 * 256:(m2 + 1) * 256], eng="pool", semkey=("wr", (m2 % 2) * 2))
            P.dma(up, wu_v[:, :, m2 * 256:(m2 + 1) * 256], eng="pool", semkey=("wr", (m2 % 2) * 2 + 1))
            for mm_ in range(2):
                m = m2 * 2 + mm_
                pg = k.pf[0]
                pu = k.pf[1]
                for kk in range(KC):
                    P.mm(pg[:, 0:cap], gp[:, kk, mm_ * 128:(mm_ + 1) * 128], xeT[:, kk, :],
                         start=(kk == 0), stop=(kk == KC - 1))
                for kk in range(KC):
                    P.mm(pu[:, 0:cap], up[:, kk, mm_ * 128:(mm_ + 1) * 128], xeT[:, kk, :],
                         start=(kk == 0), stop=(kk == KC - 1))
                sg = sm[:, (m % 2) * 256:(m % 2) * 256 + cap]
                P.act(sg, pg[:, 0:cap], AF.Silu)
                P.tt(hT[:, m, :], sg, pu[:, 0:cap], ALU.mult)
        for m2 in range(4):
            dp = wring[4 + m2].rearrange("p (a d) -> p a d", a=2)
            P.dma(dp, wd_v[:, m2 * 2:m2 * 2 + 2, :], eng="pool", semkey=("wr", 4 + m2))
            for mm_ in range(2):
                m = m2 * 2 + mm_
                for hh in range(nsh):
                    for dh in range(2):
                        pd = k.pf[2 + hh * 2 + dh]
                        P.mm(pd[0:sp_, 0:512], hT[:, m, hh * 128:hh * 128 + sp_], dp[:, mm_, dh * 512:(dh + 1) * 512],
                             start=(m == 0), stop=(m == KC - 1))
        for hh in range(nsh):
            for dh in range(2):
                pd = k.pf[2 + hh * 2 + dh]
                P.act(ye[0:sp_, hh, dh * 512:(dh + 1) * 512], pd[0:sp_, 0:512], AF.Identity,
                      scale=twc[0:sp_, hh:hh + 1])
        for i in range(NT):
            for dh in range(2):
                pc = k.pf[(i * 2 + dh) % 2]
                for hh in range(nsh):
                    P.mm(pc[:, 0:512], SelT[0:sp_, hh, i * 128:(i + 1) * 128], ye[0:sp_, hh, dh * 512:(dh + 1) * 512],
                         start=(hh == 0), stop=(hh == nsh - 1))
                dsty = ymoe[:, i, dh * 512:(dh + 1) * 512]
                if e == 0:
                    pcopy(k, dsty, pc[:, 0:512])
                else:
                    P.tt(dsty, dsty, pc[:, 0:512], ALU.add)
    g2bc = sm[:, 0:1024]
    bcast_row(k, g2bc, B.mod[:, l, v, 40:48], dg=sm[:, 1920:2048])
    for i in range(NT):
        xt = B.xt[i % 2]
        P.dma(xt[:], src[i * 128:(i + 1) * 128, :], semkey=("xt", i % 2))
        P.tt(ymoe[:, i, :], ymoe[:, i, :], g2bc, ALU.mult)
        P.tt(xt[:], xt[:], ymoe[:, i, :], ALU.add)
        P.dma(dst[i * 128:(i + 1) * 128, :], xt[:], semkey=("xt", i % 2))


def final_norm(k, b):
    P, B, I, S = k.P, k.B, k.I, k.S
    fw = B.AF[:, 0:1024]
    P.dma(fw, I.fnw_bc)
    for i in range(TX // 128):
        xt = B.xt[i % 2]
        P.dma(xt[:], S.xcur[b][i * 128:(i + 1) * 128, :], semkey=("xt", i % 2))
        st = B.st[:, (i % 2) * 4:(i % 2) * 4 + 4]
        P.act(B.junk[:], xt[:], AF.Square, accum_out=st[:, 0:1])
        rstd_col(k, st[:, 2:3], st[:, 0:1], D, st[:, 1:2])
        P.stt(xt[:], xt[:], st[:, 2:3], fw, ALU.mult, ALU.mult)
        k.fin.append(P.dma(k.out[b][i * 128:(i + 1) * 128, :], xt[:], semkey=("xt", i % 2)))


def dump_dram(k, name, src, T):
    if name not in k.cfg.get("taps", ()):
        return
    o = k.dout("tap_" + name, [T, D])
    for i in range(T // 128):
        xt = k.B.xt[i % 2]
        k.P.dma(xt[:], src[i * 128:(i + 1) * 128, :], semkey=("xt", i % 2))
        k.fin.append(k.P.dma(o[i * 128:(i + 1) * 128, :], xt[:], semkey=("xt", i % 2)))


def col_layout(v, kk):
    return np.ascontiguousarray(np.asarray(v, np.float32).reshape(kk, 128).T)


def bc_layout(v):
    v = np.asarray(v, np.float32).reshape(1, -1)
    return np.ascontiguousarray(np.broadcast_to(v, (128, v.shape[1])))


def shared_inputs(inp):
    f = lambda a: np.ascontiguousarray(np.asarray(a, np.float32))
    sh = {}
    sh["w_ada"] = f(inp["w_ada"])
    sh["b_ada_col"] = np.stack([col_layout(inp["b_ada"][l], 48) for l in range(NL)])
    sh["n1col"] = np.stack([col_layout(inp["norm1_w"][l], KC) for l in range(NL)])
    sh["n2col"] = np.stack([col_layout(inp["norm2_w"][l], KC) for l in range(NL)])
    sh["fnw_bc"] = bc_layout(inp["final_norm_w"])
    sh["w_in"] = f(inp["w_in"])
    cw = np.asarray(inp["qkv_conv_w"], np.float32)
    sh["convw"] = np.ascontiguousarray(cw.reshape(NL, 5, 24, 128).transpose(0, 3, 2, 1))
    sh["alog_bc"] = np.stack([bc_layout(inp["gdn_a_log"][l].reshape(-1)) for l in range(NL)])
    sh["dtb_bc"] = np.stack([bc_layout(inp["gdn_dt_bias"][l].reshape(-1)) for l in range(NL)])
    sh["gnw_bc"] = np.stack([bc_layout(inp["gdn_norm_w"][l]) for l in range(NL)])
    sh["lnw_bc"] = np.stack([bc_layout(inp["cmlp_ln_w"][l]) for l in range(NL)])
    sh["lnb_bc"] = np.stack([bc_layout(inp["cmlp_ln_b"][l]) for l in range(NL)])
    sh["wsT"] = np.ascontiguousarray(np.asarray(inp["cmlp_w_s"], np.float32).transpose(0, 1, 3, 2))
    sh["bs_col"] = np.ascontiguousarray(np.asarray(inp["cmlp_b_s"], np.float32).transpose(0, 2, 1))
    sh["pool_w"] = f(inp["pool_w"])
    sh["psc_col"] = np.stack([col_layout(inp["pool_scale"][l], 4) for l in range(NL)])
    for nm in ("w_br_a", "w_br_b", "w_br_c", "w_out", "w_router", "w_gate", "w_up", "w_down"):
        sh[nm] = f(inp[nm])
    sh["cst"] = make_consts()
    return sh


def core_inputs(inp, sh, core):
    m = dict(sh)
    b0 = 2 * core
    m["x"] = np.ascontiguousarray(np.asarray(inp["x"][b0:b0 + 2], np.float32))
    m["ctx"] = np.ascontiguousarray(np.asarray(inp["ctx"][b0:b0 + 2], np.float32))
    cc = np.stack([np.asarray(inp["c"][b0], np.float32), np.asarray(inp["c"][b0 + 1], np.float32),
                   np.asarray(inp["c_ctx"], np.float32)], axis=-1)
    m["ccol"] = np.ascontiguousarray(cc.reshape(KC, 128, 3).transpose(1, 0, 2))
    return m


_CACHE = {}


def kernel(**inputs):
    n = 8
    if "k" not in _CACHE:
        _CACHE["k"] = build({"batches": [0, 1], "layers": [0, 1], "streams": ["ctx", "x"],
                             "moe": True, "final": True, "taps": []})
    k = _CACHE["k"]
    sh = shared_inputs(inputs)
    in_maps = [core_inputs(inputs, sh, c) for c in range(n)]
    res = run_bass_kernel_spmd(k.nc, in_maps, core_ids=list(range(n)))
    out = np.concatenate([np.asarray(r["out"], np.float32) for r in res.results], axis=0)
    return out
```

```python
import numpy as np
import concourse.bass as bass
import concourse.mybir as mybir
from concourse.bass_utils import run_bass_kernel_spmd

F32 = mybir.dt.float32
BF16 = mybir.dt.bfloat16
AF = mybir.ActivationFunctionType
ALU = mybir.AluOpType
AX = mybir.AxisListType

ENGS = ("pe", "act", "dve", "pool", "sp")


def _box(ap):
    t = ap.tensor
    name = t.name
    space = str(ap.space)
    dims = ap.ap
    off = ap.offset
    if space == "DRAM":
        lo = off
        hi = off + sum((c - 1) * abs(s) for s, c in dims) + 1
        return (name, 0, 1, lo, hi)
    pstride = dims[0][0]
    if pstride == 0:
        pstride = 1 << 40
    tshape = t.shape
    fsz = 1
    for s in list(tshape)[1:]:
        fsz *= s
    p0 = off // fsz
    f0 = off % fsz
    npart = dims[0][1]
    f1 = f0 + sum((c - 1) * abs(s) for s, c in dims[1:]) + 1
    return (name, p0, p0 + npart, f0, f1)


def _overlap(a, b):
    return a[1] < b[2] and b[1] < a[2] and a[3] < b[4] and b[3] < a[4]


def _contains(a, b):
    return a[1] <= b[1] and b[2] <= a[2] and a[3] <= b[3] and b[4] <= a[4]


class Op:
    __slots__ = ("eng", "fn", "idx", "deps", "is_dma", "semkey", "has_dep", "tick",
                 "group", "pos")

    def __init__(self, eng, fn, is_dma=False, semkey=None):
        self.eng = eng
        self.fn = fn
        self.is_dma = is_dma
        self.semkey = semkey
        self.deps = set()
        self.has_dep = False
        self.tick = None
        self.group = None


class Prog:
    def __init__(self, nc, n_dma_sems=80):
        self.nc = nc
        self.ops = []
        self.acc = {}
        self.n_dma_sems = n_dma_sems
        self.same_engine_sync = True

    def _track(self, op, reads, writes):
        ekey = ("dma", op.semkey) if op.is_dma else op.eng
        preads = [ap for ap in reads if str(ap.space) == "PSUM"]
        reads = [ap for ap in reads if str(ap.space) != "PSUM"]
        writes = list(writes) + preads
        for ap in reads:
            b = _box(ap)
            ent = self.acc.setdefault(b[0], {"w": [], "r": {}})
            for (ob, oop) in ent["w"]:
                if _overlap(ob, b):
                    op.deps.add(oop)
            ent["r"][(b, ekey)] = op
        for ap in writes:
            b = _box(ap)
            if str(ap.space) == "PSUM":
                b = (b[0], 0, 128, 0, 1 << 30)
            ent = self.acc.setdefault(b[0], {"w": [], "r": {}})
            keep = []
            for (ob, oop) in ent["w"]:
                if oop is op:
                    keep.append((ob, oop))
                    continue
                if _overlap(ob, b):
                    op.deps.add(oop)
                    if _contains(b, ob):
                        continue
                keep.append((ob, oop))
            keep.append((b, op))
            ent["w"] = keep
            rk = {}
            for (ob, ek), oop in ent["r"].items():
                if oop is op:
                    rk[(ob, ek)] = oop
                    continue
                if _overlap(ob, b):
                    op.deps.add(oop)
                    if _contains(b, ob):
                        continue
                rk[(ob, ek)] = oop
            ent["r"] = rk
        op.deps.discard(op)

    def add(self, eng, fn, reads=(), writes=()):
        op = Op(eng, fn)
        op.idx = len(self.ops)
        self.ops.append(op)
        self._track(op, reads, writes)
        return op

    def dma(self, out, in_, eng="sp", semkey=None, **kw):
        sb = out if str(out.space) != "DRAM" else in_
        if semkey is None:
            semkey = sb.tensor.name
        op = Op(eng, lambda e, out=out, in_=in_, kw=kw: e.dma_start(out=out, in_=in_, **kw),
                is_dma=True, semkey=semkey)
        op.idx = len(self.ops)
        self.ops.append(op)
        self._track(op, [in_], [out])
        return op

    def mm(self, out, lhsT, rhs, start=True, stop=True, **kw):
        reads = [lhsT, rhs] + ([] if start else [out])
        return self.add("pe", lambda e: e.matmul(out, lhsT, rhs, start=start, stop=stop, **kw),
                        reads, [out])

    def transpose(self, out, in_, ident):
        return self.add("pe", lambda e: e.transpose(out, in_, ident), [in_, ident], [out])

    def act(self, out, in_, func, bias=None, scale=1.0, accum_out=None, eng="act"):
        reads = [in_]
        if bias is not None and not isinstance(bias, (int, float)):
            reads.append(bias)
        if not isinstance(scale, (int, float)):
            reads.append(scale)
        writes = [out] + ([accum_out] if accum_out is not None else [])
        kw = {}
        if accum_out is not None:
            kw["accum_out"] = accum_out
        if bias is not None:
            kw["bias"] = bias
        return self.add(eng, lambda e: e.activation(out=out, in_=in_, func=func, scale=scale, **kw),
                        reads, writes)

    def tt(self, out, in0, in1, op, eng="dve"):
        return self.add(eng, lambda e: e.tensor_tensor(out=out, in0=in0, in1=in1, op=op),
                        [in0, in1], [out])

    def ts(self, out, in0, s1, s2, op0, op1=None, eng="dve", accum_out=None):
        reads = [in0] + [s for s in (s1, s2) if s is not None and not isinstance(s, (int, float))]
        writes = [out] + ([accum_out] if accum_out is not None else [])
        kw = {}
        if op1 is not None:
            kw["op1"] = op1
        if accum_out is not None:
            kw["accum_out"] = accum_out
        return self.add(eng, lambda e: e.tensor_scalar(out=out, in0=in0, scalar1=s1, scalar2=s2,
                                                       op0=op0, **kw), reads, writes)

    def stt(self, out, in0, scalar, in1, op0, op1, eng="dve", accum_out=None):
        reads = [in0, in1] + ([scalar] if not isinstance(scalar, (int, float)) else [])
        writes = [out] + ([accum_out] if accum_out is not None else [])
        kw = {}
        if eng == "pool":
            eng = "dve"
        if accum_out is not None:
            kw["accum_out"] = accum_out
        return self.add(eng, lambda e: e.scalar_tensor_tensor(out=out, in0=in0, scalar=scalar, in1=in1,
                                                              op0=op0, op1=op1, **kw), reads, writes)

    def copy(self, out, in_, eng="dve"):
        if eng == "act":
            return self.add("act", lambda e: e.copy(out=out, in_=in_), [in_], [out])
        return self.add(eng, lambda e: e.tensor_copy(out=out, in_=in_), [in_], [out])

    def memset(self, ap, val, eng="pool"):
        return self.add(eng, lambda e: e.memset(ap, val), [], [ap])

    def reduce(self, out, in_, op, axis=None, eng="dve"):
        axis = axis or AX.X
        return self.add(eng, lambda e: e.tensor_reduce(out=out, in_=in_, axis=axis, op=op), [in_], [out])

    def emit(self, final_wait_ops=()):
        nc = self.nc
        ops = self.ops
        streams = {e: [] for e in ENGS}
        for op in ops:
            op.pos = len(streams[op.eng])
            streams[op.eng].append(op)
        for op in ops:
            red = {}
            dm = []
            for d in op.deps:
                if d.is_dma:
                    dm.append(d)
                else:
                    if d.eng == op.eng and (d.eng == "pe" or not self.same_engine_sync) and not op.is_dma:
                        continue
                    if d.eng not in red or red[d.eng].idx < d.idx:
                        red[d.eng] = d
            op.deps = list(red.values()) + dm
            for d in op.deps:
                d.has_dep = True
        for op in final_wait_ops:
            op.has_dep = True
        LIM = 30000
        cnt = {e: 0 for e in ENGS}
        for op in ops:
            if not op.is_dma and op.has_dep:
                cnt[op.eng] += 1
                op.tick = ((cnt[op.eng] - 1) // LIM, (cnt[op.eng] - 1) % LIM + 1)
        keys = []
        for op in ops:
            if op.is_dma and op.semkey not in keys:
                keys.append(op.semkey)
        nsem = min(len(keys), self.n_dma_sems)
        key2sem = {k: i % max(nsem, 1) for i, k in enumerate(keys)}
        semstate = {}
        dma_wait_prev = {}
        consumers = {}
        for op in ops:
            for d in op.deps:
                if d.is_dma:
                    consumers.setdefault(d, []).append(op)
        first_consumer_idx = {}
        for d, cl in consumers.items():
            first_consumer_idx[d] = min(c.idx for c in cl)
        for op in final_wait_ops:
            if op.is_dma:
                first_consumer_idx.setdefault(op, len(ops))
        groups = []
        for op in ops:
            if not op.is_dma:
                continue
            s = key2sem[op.semkey]
            st = semstate.setdefault(s, {"total": 0, "open": None, "close_at": None, "prev_final": 0})
            g = st["open"]
            if g is not None and st["close_at"] is not None and st["close_at"] <= op.idx:
                st["prev_final"] = g["final"]
                g = None
            if g is None:
                g = {"sem": s, "members": [], "final": st["total"]}
                groups.append(g)
                st["open"] = g
                st["close_at"] = None
                if st["prev_final"] > 0:
                    dma_wait_prev[op] = (s, st["prev_final"])
            st["total"] += 16
            g["members"].append(op)
            g["final"] = st["total"]
            op.group = g
            fc = first_consumer_idx.get(op)
            if fc is not None:
                st["close_at"] = fc if st["close_at"] is None else min(st["close_at"], fc)
        self.stats = {"n_ops": len(ops), "n_dma_sems": nsem, "incs": dict(cnt)}
        from contextlib import ExitStack
        es = ExitStack()
        esem = {}
        for e in ENGS:
            if e == "sp":
                continue
            for gen in range((cnt[e] + LIM - 1) // LIM + 1):
                esem[(e, gen)] = es.enter_context(nc.semaphore("c_%s%d" % (e, gen)))
        dsem = [es.enter_context(nc.semaphore("d_%d" % i)) for i in range(nsem)]
        known = {e: {} for e in ENGS}
        nwaits = 0

        def emit_stream(ename, eobj):
            nonlocal nwaits
            kn = known[ename]
            for op in streams[ename]:
                waits = {}
                for d in op.deps:
                    if d.is_dma:
                        g = d.group
                        key = ("d", g["sem"])
                        val = g["final"]
                    else:
                        key = ("e", d.eng, d.tick[0])
                        val = d.tick[1]
                    if waits.get(key, 0) < val:
                        waits[key] = val
                if op in dma_wait_prev:
                    s, v = dma_wait_prev[op]
                    key = ("d", s)
                    if waits.get(key, 0) < v:
                        waits[key] = v
                for key, val in waits.items():
                    if kn.get(key, 0) >= val:
                        continue
                    kn[key] = val
                    sem = dsem[key[1]] if key[0] == "d" else esem[(key[1], key[2])]
                    eobj.wait_ge(sem, val)
                    nwaits += 1
                ins = op.fn(eobj)
                if op.is_dma:
                    ins.then_inc(dsem[op.group["sem"]], 16)
                elif op.has_dep:
                    ins.then_inc(esem[(ename, op.tick[0])], 1)
            if ename == "sp":
                for op in final_wait_ops:
                    g = op.group
                    eobj.wait_ge(dsem[g["sem"]], g["final"])

        with nc.Block() as block:
            @block.tensor
            def _(e):
                emit_stream("pe", e)

            @block.scalar
            def _(e):
                emit_stream("act", e)

            @block.vector
            def _(e):
                emit_stream("dve", e)

            @block.gpsimd
            def _(e):
                emit_stream("pool", e)

            @block.sync
            def _(e):
                emit_stream("sp", e)
        self.stats["nwaits"] = nwaits
        es.close()


from contextlib import ExitStack

D = 1024
KC = 8
H = 8
NL = 2
OFF_Q, OFF_K, OFF_V, OFF_Z, OFF_B, OFF_A, OFF_U, OFF_VG, OFF_P, OFF_G = (
    0, 1024, 2048, 3072, 4096, 4112, 4128, 4640, 5152, 5664)
IN_COLS = 8736
EPS = 1e-6
NEG = -30000.0
TX = 2048
TC = 256
NE = 16

C_ID, C_U, C_L, C_NMF, C_NMB, C_SL, C_SU, C_ONE = [i * 128 for i in range(8)]
C_IOTA = 1024
C_RCX = 1280
C_RCC = 1536
NCST = 2560


def make_consts():
    c = np.zeros((128, NCST), np.float32)
    i = np.arange(128)[:, None]
    j = np.arange(128)[None, :]
    c[:, C_ID:C_ID + 128] = (i == j)
    c[:, C_U:C_U + 128] = (i <= j)
    c[:, C_L:C_L + 128] = (i >= j)
    c[:, C_NMF:C_NMF + 128] = np.where(i >= j, 0.0, NEG)
    c[:, C_NMB:C_NMB + 128] = np.where(i <= j, 0.0, NEG)
    c[:, C_SL:C_SL + 128] = (i > j)
    c[:, C_SU:C_SU + 128] = (i < j)
    c[:, C_ONE:C_ONE + 128] = 1.0
    c[:, C_IOTA:C_IOTA + 256] = np.arange(1, 257)[None, :]
    for seg, off in ((64, C_RCX), (256, C_RCC)):
        t = np.arange(seg)
        for gi, w in enumerate((2, 4, 8, 16)):
            lo = np.clip(t - w // 2, 0, seg)
            hi = np.clip(t + w // 2, 0, seg)
            c[:, off + gi * seg: off + (gi + 1) * seg] = (1.0 / (hi - lo).astype(np.float32))[None, :]
    return c


def tokblocks(T):
    bs = min(512, T)
    return [(s, bs) for s in range(0, T, bs)]


class K:
    pass


def build(cfg):
    nc = bass.Bass("TRN2", target_bir_lowering=False)
    k = K()
    k.nc = nc
    k.cfg = cfg
    P = Prog(nc)
    k.P = P
    es = ExitStack()
    k.fin = []
    k.dbg = {}

    def din(name, shape, dt=F32):
        return nc.dram_tensor(name, list(shape), dt, kind="ExternalInput").ap()

    def dscr(name, shape, dt=F32):
        return nc.dram_tensor(name, list(shape), dt).ap()

    def dout(name, shape, dt=F32):
        return nc.dram_tensor(name, list(shape), dt, kind="ExternalOutput").ap()

    def sb(name, shape, dt=F32):
        return es.enter_context(nc.sbuf_tensor(name, list(shape), dt))

    def ps(name, shape, dt=F32):
        return es.enter_context(nc.psum_tensor(name, list(shape), dt))

    k.dout = dout
    I = K()
    k.I = I
    I.x = din("x", [2, TX, D])
    I.ctx = din("ctx", [2, TC, D])
    I.ccol = din("ccol", [128, KC, 3])
    I.w_ada = din("w_ada", [NL, D, 6 * D])
    I.b_ada_col = din("b_ada_col", [NL, 128, 48])
    I.n1col = din("n1col", [NL, 128, KC])
    I.n2col = din("n2col", [NL, 128, KC])
    I.fnw_bc = din("fnw_bc", [128, D])
    I.w_in = din("w_in", [NL, D, IN_COLS])
    I.convw = din("convw", [NL, 128, 24, 5])
    I.alog_bc = din("alog_bc", [NL, 128, 16])
    I.dtb_bc = din("dtb_bc", [NL, 128, 16])
    I.gnw_bc = din("gnw_bc", [NL, 128, 128])
    I.lnw_bc = din("lnw_bc", [NL, 128, 512])
    I.lnb_bc = din("lnb_bc", [NL, 128, 512])
    I.wsT = din("wsT", [NL, 4, 128, 128])
    I.bs_col = din("bs_col", [NL, 128, 4])
    I.pool_w = din("pool_w", [NL, 4, 128, 128])
    I.psc_col = din("psc_col", [NL, 128, 4])
    I.w_br_a = din("w_br_a", [NL, 1024, D])
    I.w_br_b = din("w_br_b", [NL, 512, D])
    I.w_br_c = din("w_br_c", [NL, 512, D])
    I.w_out = din("w_out", [NL, D, D])
    I.w_router = din("w_router", [NL, D, NE])
    I.w_gate = din("w_gate", [NL, NE, D, D])
    I.w_up = din("w_up", [NL, NE, D, D])
    I.w_down = din("w_down", [NL, NE, D, D])
    I.cst = din("cst", [128, NCST])
    k.out = dout("out", [2, TX, D])
    S = K()
    k.S = S
    S.xcur = dscr("xcur", [2, TX, D])
    S.ccur = dscr("ccur", [2, TC, D])
    S.gates = dscr("gatesD", [24, 128, TX], BF16)
    B = K()
    k.B = B
    B.cst = sb("cst_sb", [128, 1280])
    B.cstb = sb("cstb", [128, 1024 + 256], BF16)
    B.AB = sb("AB", [128, 49152], BF16)
    B.AF = sb("AF", [128, 16384], F32)
    B.xt = [sb("xt%d" % i, [128, D]) for i in range(2)]
    B.xn = sb("xn", [128, D], BF16)
    B.st = sb("st", [128, 64])
    B.wst = [sb("wst%d" % i, [128, KC, 256], BF16) for i in range(2)]
    B.wsm = [sb("wsm%d" % i, [128, KC, 128], BF16) for i in range(4)]
    B.mod = sb("mod", [128, NL, 3, 48])
    B.modA = sb("modA", [128, NL, 3, 2, KC])
    B.sc3 = sb("sc3", [128, KC, 3])
    B.small = sb("small", [128, 2048])
    B.states = B.AF[:, 10240:12288].rearrange("p (a c) -> p a c", c=128)
    B.gsc = B.AF[:, 12288:14592].rearrange("p (a b c) -> p a b c", a=9, b=16)
    B.junk = sb("junk", [128, D], BF16)
    k.pf = [ps("pf%d" % i, [128, 512]) for i in range(6)]
    k.pb = [ps("pb%d" % i, [128, 1024], BF16) for i in range(2)]

    def cst(off, n=128):
        return B.cst[:, off:off + n]

    def cstb(off, n=128):
        return B.cstb[:, off:off + n]

    k.c = cst
    k.cb = cstb
    P.dma(B.cst[:], I.cst[:, 0:1280])
    P.copy(B.cstb[:, 0:1024], B.cst[:, 0:1024])
    P.copy(B.cstb[:, 1024:1280], B.cst[:, C_IOTA:C_IOTA + 256])
    k.rr = [0]

    prologue(k)
    for b in cfg["batches"]:
        for l in cfg["layers"]:
            last = l == NL - 1
            src_c = I.ctx[b] if l == 0 else S.ccur[b]
            src_x = I.x[b] if l == 0 else S.xcur[b]
            if "ctx" in cfg["streams"]:
                mixer(k, b, l, src_c, S.ccur[b], TC, True, last)
                if not last and cfg.get("moe", True):
                    moe(k, b, l, S.ccur[b], S.ccur[b], TC, True)
                if not last:
                    dump_dram(k, "ccur", S.ccur[b], TC)
            if "x" in cfg["streams"]:
                mixer(k, b, l, src_x, S.xcur[b], TX, False, False)
                if cfg.get("moe", True):
                    moe(k, b, l, S.xcur[b], S.xcur[b], TX, False)
                dump_dram(k, "xcur%d" % l, S.xcur[b], TX)
        if cfg.get("final", True):
            final_norm(k, b)
    P.emit(final_wait_ops=k.fin)
    es.close()
    k.stats = P.stats
    return k


def tap(k, name, ap_sb, shape=None, dt=F32):
    if name not in k.cfg.get("taps", ()):
        return
    shp = list(ap_sb.shape)
    o = k.dout("tap_" + name, shp, ap_sb.dtype)
    k.fin.append(k.P.dma(o, ap_sb, semkey="tap"))


def evac_eng(k):
    k.rr[0] += 1
    return "act" if k.rr[0] % 2 else "dve"


def pcopy(k, out, in_, eng=None):
    eng = eng or evac_eng(k)
    k.P.copy(out, in_, eng=eng)


def prologue(k):
    P, B, I = k.P, k.B, k.I
    raw = B.small[:, 0:24].rearrange("p (a b) -> p a b", a=KC)
    P.dma(raw, I.ccol)
    P.act(B.sc3[:], raw, AF.Silu)
    for l in range(NL):
        wv = I.w_ada[l].rearrange("(kk p) c -> p kk c", p=128)
        acc = k.pf[0][:, 0:144].rearrange("p (j v) -> p j v", v=3)
        for cb in range(12):
            wblk = B.AF[:, (cb % 2) * 4096:(cb % 2) * 4096 + 4096].rearrange("p (a b) -> p a b", a=KC)
            P.dma(wblk, wv[:, :, cb * 512:(cb + 1) * 512], semkey=("wada", cb % 2))
            for jj in range(4):
                j = cb * 4 + jj
                for kk in range(KC):
                    P.mm(acc[:, j, :], wblk[:, kk, jj * 128:(jj + 1) * 128], B.sc3[:, kk, :],
                         start=(kk == 0), stop=(kk == KC - 1))
        bcol = B.small[:, 32:80]
        P.dma(bcol, I.b_ada_col[l])
        for v in range(3):
            P.tt(B.mod[:, l, v, :], acc[:, :, v], bcol, ALU.add)
        n1 = B.small[:, 80:88]
        n2 = B.small[:, 88:96]
        P.dma(n1, I.n1col[l])
        P.dma(n2, I.n2col[l])
        for v in range(3):
            P.stt(B.modA[:, l, v, 0, :], B.mod[:, l, v, 8:16], 1.0, n1, ALU.add, ALU.mult)
            P.stt(B.modA[:, l, v, 1, :], B.mod[:, l, v, 32:40], 1.0, n2, ALU.add, ALU.mult)
    tap(k, "mod", B.mod[:].rearrange("p l v j -> p (l v j)"))


def rstd_col(k, out_col, ss_col, n, tmp_col):
    P = k.P
    P.ts(tmp_col, ss_col, 1.0 / n, EPS, ALU.mult, ALU.add)
    P.act(tmp_col, tmp_col, AF.Sqrt)
    P.add("dve", lambda e: e.reciprocal(out=out_col, in_=tmp_col), [tmp_col], [out_col])


def norm_to_T(k, src, T, Acol, Shcol, hT, xn_tok=None):
    P, B = k.P, k.B
    NT = T // 128
    for i in range(NT):
        xt = B.xt[i % 2]
        P.dma(xt[:], src[i * 128:(i + 1) * 128, :], semkey=("xt", i % 2))
        st = B.st[:, (i % 2) * 4:(i % 2) * 4 + 4]
        P.act(B.junk[:], xt[:], AF.Square, accum_out=st[:, 0:1])
        rstd_col(k, st[:, 2:3], st[:, 0:1], D, st[:, 1:2])
        xn = xn_tok[:, i, :] if xn_tok is not None else B.xn[:]
        P.ts(xn, xt[:], st[:, 2:3], None, ALU.mult)
        if hT is None:
            continue
        pb = k.pb[i % 2]
        for kk in range(KC):
            P.transpose(pb[:, kk * 128:(kk + 1) * 128], xn[:, kk * 128:(kk + 1) * 128], k.cb(C_ID))
        pv = pb[:].rearrange("p (a b) -> p a b", a=KC)
        dst = hT[:, :, i * 128:(i + 1) * 128]
        P.tt(dst, pv, Acol.unsqueeze(2).broadcast_to([128, KC, 128]), ALU.mult)
        P.tt(dst, dst, Shcol.unsqueeze(2).broadcast_to([128, KC, 128]), ALU.add, eng="pool")


def load_w_cols(k, dst, l, c0, ncols, key):
    wv = k.I.w_in[l].rearrange("(kk p) c -> p kk c", p=128)
    k.P.dma(dst, wv[:, :, c0:c0 + ncols], eng="pool", semkey=key)


def proj_fm(k, hT, T, w, ncol, consume):
    P = k.P
    for j in range(ncol // 128):
        for bi, (t0, n) in enumerate(tokblocks(T)):
            pf = k.pf[(j * 4 + bi) % 4]
            for kk in range(KC):
                P.mm(pf[:, 0:n], w[:, kk, j * 128:(j + 1) * 128], hT[:, kk, t0:t0 + n],
                     start=(kk == 0), stop=(kk == KC - 1))
            consume(j, t0, n, pf[:, 0:n])


def mixer(k, b, l, src, dst, T, is_ctx, states_only):
    P, B, I, S = k.P, k.B, k.I, k.S
    NT = T // 128
    v = 2 if is_ctx else b
    AB = B.AB
    h1T = AB[:, 0:KC * T].rearrange("p (a t) -> p a t", a=KC)
    yaT = AB[:, 16384:16384 + 8 * T].rearrange("p (a t) -> p a t", a=8)
    ybT = AB[:, 32768:32768 + 4 * T].rearrange("p (a t) -> p a t", a=4)
    ycT = AB[:, 40960:40960 + 4 * T].rearrange("p (a t) -> p a t", a=4)
    norm_to_T(k, src, T, B.modA[:, l, v, 0, :], B.mod[:, l, v, 0:8], h1T)
    tap(k, "h1T", h1T)
    if not states_only:
        gate_phase(k, l, h1T, T)
    gdn(k, b, l, h1T, yaT, T, is_ctx, states_only)
    if states_only:
        return
    tap(k, "yaT", yaT)
    cmlp(k, l, h1T, ybT, T)
    tap(k, "ybT", ybT)
    pool(k, l, h1T, ycT, T, is_ctx)
    tap(k, "ycT", ycT)
    merge_out(k, b, l, v, src, dst, yaT, ybT, ycT, T)


def gate_phase(k, l, h1T, T):
    P, B, S = k.P, k.B, k.S
    for blk in range(12):
        w = B.wst[blk % 2]
        load_w_cols(k, w[:], l, OFF_G + blk * 256, 256, ("wst", blk % 2))

        def consume(j, t0, n, pap, blk=blk):
            stg = B.AB[:, 49152 - 1024 + (j % 2) * 512:49152 - 1024 + (j % 2) * 512 + n]
            P.act(stg, pap, AF.Sigmoid)
            P.dma(S.gates[blk * 2 + j, :, t0:t0 + n], stg, eng="sp", semkey=("gst", j % 2))
        proj_fm(k, h1T, T, w, 256, consume)


GS_BETA, GS_NBETA, GS_G, GS_GC, GS_EGC, GS_EDEC, GS_ETOT, GS_BEXP, GS_TOT = range(9)


def gdn(k, b, l, h1T, yaT, T, is_ctx, states_only):
    P, B, I = k.P, k.B, k.I
    NT = T // 128
    AB, AFa = B.AB, B.AF
    GB = 32768
    QT = AB[:, GB:GB + T]
    KT = AB[:, GB + 2048:GB + 2048 + T]
    VT = AB[:, GB + 4096:GB + 4096 + T]
    Ktok = AB[:, GB + 6144:GB + 6144 + T].rearrange("p (a c) -> p a c", c=128)
    Vtok = AB[:, GB + 8192:GB + 8192 + T].rearrange("p (a c) -> p a c", c=128)
    zs = AB[:, GB + 10240:GB + 10240 + T].rearrange("p (a c) -> p a c", c=128)
    yatok = AB[:, GB + 12288:GB + 12288 + T].rearrange("p (a c) -> p a c", c=128)
    tb = GB + 14336
    attn_b = AB[:, tb:tb + 128]
    attnT = AB[:, tb + 128:tb + 256]
    wT = AB[:, tb + 256:tb + 384]
    qgT = AB[:, tb + 384:tb + 512]
    kdec = AB[:, tb + 512:tb + 640]
    vnew = AB[:, tb + 640:tb + 768]
    Sbf = AB[:, tb + 768:tb + 896]
    cbuf = AFa[:, 0:T + 4]
    cs = AFa[:, 2052:2052 + T]
    oacc = AFa[:, 4100:4100 + T].rearrange("p (a c) -> p a c", c=128)
    fb = 6148
    Rm = AFa[:, fb:fb + 128]
    dec = AFa[:, fb + 128:fb + 256]
    egr = AFa[:, fb + 256:fb + 384]
    nk = [AFa[:, fb + 384 + i * 128:fb + 512 + i * 128] for i in range(2)]
    Pk = [AFa[:, fb + 640 + i * 128:fb + 768 + i * 128] for i in range(2)]
    Xk = [AFa[:, fb + 896 + i * 128:fb + 1024 + i * 128] for i in range(2)]
    rhsu = AFa[:, fb + 1152:fb + 1280]
    rhsw = AFa[:, fb + 1280:fb + 1408]
    usb = AFa[:, fb + 1408:fb + 1536]
    Sst = AFa[:, fb + 1536:fb + 1664]
    sq = AFa[:, fb:fb + 512]
    rinv = AFa[:, fb + 512:fb + 1024]
    tmpf = AFa[:, fb + 2688:fb + 2688 + 128]
    ident, identb = k.c(C_ID), k.cb(C_ID)
    ones = k.c(C_ONE)
    gs = B.gsc

    wba = B.wsm[0]
    load_w_cols(k, wba[:, :, 0:32], l, OFF_B, 32, ("wsm", 0))
    sm = B.small
    alog = sm[:, 128:144]
    dtb = sm[:, 144:160]
    negA = sm[:, 160:176]
    gnw = sm[:, 256:384]
    P.dma(alog, I.alog_bc[l])
    P.dma(dtb, I.dtb_bc[l])
    P.dma(gnw, I.gnw_bc[l])
    cw = sm[:, 384:504].rearrange("p (a t) -> p a t", t=5)
    P.dma(cw, I.convw[l])
    P.act(negA, alog, AF.Exp)
    P.ts(negA, negA, -1.0, None, ALU.mult)
    for i in range(NT):
        pf = k.pf[4]
        for kk in range(KC):
            P.mm(pf[:, 0:32], h1T[:, kk, i * 128:(i + 1) * 128], wba[:, kk, 0:32],
                 start=(kk == 0), stop=(kk == KC - 1))
        P.act(gs[:, GS_BETA, i, :], pf[:, 0:16], AF.Sigmoid)
        P.tt(gs[:, GS_G, i, :], pf[:, 16:32], dtb, ALU.add)
    nt = slice(0, NT)
    P.ts(gs[:, GS_NBETA, nt, :], gs[:, GS_BETA, nt, :], -1.0, None, ALU.mult)
    P.act(gs[:, GS_G, nt, :], gs[:, GS_G, nt, :], AF.Exp)
    P.act(gs[:, GS_G, nt, :], gs[:, GS_G, nt, :], AF.Ln, bias=1.0)
    P.tt(gs[:, GS_G, nt, :], gs[:, GS_G, nt, :], negA.unsqueeze(1).broadcast_to([128, NT, 16]), ALU.mult)
    for i in range(NT):
        pf = k.pf[4]
        P.mm(pf[:, 0:8], k.c(C_U), gs[:, GS_G, i, 0:8])
        P.mm(pf[:, 8:16], k.c(C_L), gs[:, GS_G, i, 8:16])
        P.mm(pf[:, 16:32], ones, gs[:, GS_G, i, :])
        pcopy(k, gs[:, GS_GC, i, :], pf[:, 0:16], "dve")
        pcopy(k, gs[:, GS_TOT, i, :], pf[:, 16:32], "act")
    P.act(gs[:, GS_EGC, nt, :], gs[:, GS_GC, nt, :], AF.Exp)
    P.act(gs[:, GS_ETOT, nt, :], gs[:, GS_TOT, nt, :], AF.Exp)
    P.tt(gs[:, GS_EDEC, nt, :], gs[:, GS_TOT, nt, :], gs[:, GS_GC, nt, :], ALU.subtract)
    P.act(gs[:, GS_EDEC, nt, :], gs[:, GS_EDEC, nt, :], AF.Exp)
    P.tt(gs[:, GS_BEXP, nt, :], gs[:, GS_BETA, nt, :], gs[:, GS_EGC, nt, :], ALU.mult)
    tap(k, "gsc", gs)

    for h in range(H):
        P.memset(cbuf[:, 0:2], 0.0)
        P.memset(cbuf[:, T + 2:T + 4], 0.0)
        for ci, (off, dstT) in enumerate(((OFF_Q, QT), (OFF_K, KT), (OFF_V, VT))):
            w = B.wsm[1 + ci]
            load_w_cols(k, w[:], l, off + h * 128, 128, ("wsm", 1 + ci))

            def consume(j, t0, n, pap):
                pcopy(k, cbuf[:, 2 + t0:2 + t0 + n], pap)
            proj_fm(k, h1T, T, w, 128, consume)
            ce = "dve" if ci != 1 else "pool"
            cwc = cw[:, ci * 8 + h, :]
            P.ts(cs, cbuf[:, 0:T], cwc[:, 0:1], None, ALU.mult, eng=ce)
            for tp in range(1, 5):
                P.stt(cs, cbuf[:, tp:tp + T], cwc[:, tp:tp + 1], cs, ALU.mult, ALU.add, eng=ce)
            if ci == 2:
                P.act(dstT, cs, AF.Silu)
                continue
            P.act(cs, cs, AF.Silu)
            for (t0, n) in tokblocks(T):
                P.act(sq[:, 0:n], cs[:, t0:t0 + n], AF.Square)
                pf = k.pf[5]
                P.mm(pf[:, 0:n], ones, sq[:, 0:n])
                P.ts(rinv[:, 0:n], pf[:, 0:n], EPS, None, ALU.add)
                P.act(rinv[:, 0:n], rinv[:, 0:n], AF.Sqrt)
                P.add("dve", lambda e, n=n: e.reciprocal(out=rinv[:, 0:n], in_=rinv[:, 0:n]),
                      [rinv[:, 0:n]], [rinv[:, 0:n]])
                if ci == 0:
                    P.stt(dstT[:, t0:t0 + n], cs[:, t0:t0 + n], 128.0 ** -0.5, rinv[:, 0:n], ALU.mult, ALU.mult)
                else:
                    P.tt(dstT[:, t0:t0 + n], cs[:, t0:t0 + n], rinv[:, 0:n], ALU.mult)
        for (srcT, dtok) in ((KT, Ktok), (VT, Vtok)):
            for i0 in range(0, NT, 8):
                pb = k.pb[(i0 // 8) % 2]
                ni = min(8, NT - i0)
                for i in range(ni):
                    P.transpose(pb[:, i * 128:(i + 1) * 128], srcT[:, (i0 + i) * 128:(i0 + i + 1) * 128], identb)
                pcopy(k, dtok[:, i0:i0 + ni, :], pb[:, 0:ni * 128].rearrange("p (a c) -> p a c", c=128))
        if h == 0:
            tap(k, "QT0", QT)
            tap(k, "KT0", KT)
            tap(k, "Vtok0", Vtok)
        if not states_only:
            wz = B.wsm[0]
            load_w_cols(k, wz[:], l, OFF_Z + h * 128, 128, ("wsm", 0))
            for i in range(NT):
                pf = k.pf[4]
                for kk in range(KC):
                    P.mm(pf[:, 0:128], h1T[:, kk, i * 128:(i + 1) * 128], wz[:, kk, :],
                         start=(kk == 0), stop=(kk == KC - 1))
                P.act(zs[:, i, :], pf[:, 0:128], AF.Silu)
        P.memset(oacc, 0.0)
        negU = AFa[:, fb + 3328:fb + 3456]
        negL = AFa[:, fb + 3456:fb + 3584]
        if h == 0:
            P.ts(negU, k.c(C_U), -1.0, None, ALU.mult)
            P.ts(negL, k.c(C_L), -1.0, None, ALU.mult)

        G = min(4, NT)
        GW = G * 128

        def chain(dr):
            col = dr * 8 + h
            nmsk = negU if dr == 0 else negL
            nm = k.c(C_NMF) if dr == 0 else k.c(C_NMB)
            sm_ = k.c(C_SL) if dr == 0 else k.c(C_SU)
            fo = 0 if dr == 0 else fb
            f3 = lambda o: AFa[:, fo + o:fo + o + GW].rearrange("p (g c) -> p g c", c=128)
            Rm4 = f3(0)
            dec4 = f3(512)
            egr4 = f3(1024)
            nk4 = f3(1536)
            Pk4 = f3(2048)
            Xk4 = f3(2560)
            rhsu4, rhsw4, usb4 = Rm4, dec4, egr4
            Sst = AFa[:, fo + 3072:fo + 3200]
            bo = GB + 12288 + dr * 2048
            b3 = lambda o: AB[:, bo + o:bo + o + GW].rearrange("p (g c) -> p g c", c=128)
            attn4 = b3(0)
            attnT4 = b3(512)
            qgT4 = b3(1024)
            kdec4 = b3(1536)
            wT4 = attn4
            vnew = B.junk[:, dr * 256:dr * 256 + 128]
            Sbf = B.junk[:, dr * 256 + 128:dr * 256 + 256]
            b0, b1, b2 = k.pf[dr * 3], k.pf[dr * 3 + 1], k.pf[dr * 3 + 2]
            pbx = k.pb[dr]
            v3 = lambda bank: bank[:, 0:GW].rearrange("p (g c) -> p g c", c=128)
            bcj = lambda ap2: ap2.broadcast_to([128, G, 128])
            bcg = lambda ap2: ap2.unsqueeze(1).broadcast_to([128, G, 128])
            if is_ctx:
                P.memset(Sst, 0.0)
            else:
                P.copy(Sst, B.states[:, col, :], eng="pool")
            P.copy(Sbf, Sst, eng="pool")
            yield
            ngrp = NT // G
            grp_order = range(ngrp) if dr == 0 else range(ngrp - 1, -1, -1)
            for gi in grp_order:
                cl = gi * G
                gsl = slice(cl, cl + G)
                tsg = slice(cl * 128, (cl + G) * 128)
                scg = lambda kind: gs[:, kind, gsl, col:col + 1]
                P.tt(Rm4, bcg(nmsk), bcj(scg(GS_G)), ALU.mult)
                for g in range(G):
                    ts_ = slice((cl + g) * 128, (cl + g + 1) * 128)
                    P.mm(b1[:, g * 128:(g + 1) * 128], KT[:, ts_], KT[:, ts_])
                for g in range(G):
                    ts_ = slice((cl + g) * 128, (cl + g + 1) * 128)
                    P.mm(b2[:, g * 128:(g + 1) * 128], QT[:, ts_], KT[:, ts_])
                P.mm(b0[:, 0:GW], ones, Rm4.rearrange("p g c -> p (g c)"))
                yield
                P.act(egr4, v3(b0), AF.Exp, scale=-1.0)
                P.tt(dec4, v3(b0), bcg(nm), ALU.add)
                yield
                P.tt(dec4, dec4, bcj(scg(GS_GC)), ALU.add)
                yield
                P.act(dec4, dec4, AF.Exp)
                P.tt(qgT4, QT[:, tsg].rearrange("p (g c) -> p g c", c=128), egr4, ALU.mult)
                yield
                P.tt(nk4, v3(b1), bcj(scg(GS_NBETA)), ALU.mult)
                P.tt(attn4, v3(b2), dec4, ALU.mult)
                yield
                P.tt(nk4, nk4, dec4, ALU.mult)
                for g in range(G):
                    P.transpose(pbx[:, g * 128:(g + 1) * 128], attn4[:, g, :], identb)
                yield
                P.tt(nk4, nk4, bcg(sm_), ALU.mult)
                pcopy(k, attnT4, pbx[:, 0:GW].rearrange("p (g c) -> p g c", c=128), "act")
                yield
                for g in range(G):
                    P.transpose(b0[:, g * 128:(g + 1) * 128], nk4[:, g, :], ident)
                yield
                pcopy(k, Pk4, v3(b0), "act")
                P.tt(Xk4, v3(b0), bcg(ident), ALU.add)
                yield
                P.tt(kdec4, Ktok[:, gsl, :], bcj(scg(GS_EDEC)), ALU.mult)
                yield
                for lev in range(1, 7):
                    for g in range(G):
                        P.mm(b1[:, g * 128:(g + 1) * 128], Pk4[:, g, :], nk4[:, g, :])
                    if lev < 6:
                        for g in range(G):
                            P.mm(b2[:, g * 128:(g + 1) * 128], nk4[:, g, :], Pk4[:, g, :])
                    yield
                    pcopy(k, nk4, v3(b1), "act")
                    if lev < 6:
                        pcopy(k, Pk4, v3(b2), "dve")
                    yield
                    for g in range(G):
                        P.mm(b0[:, g * 128:(g + 1) * 128], nk4[:, g, :], Xk4[:, g, :])
                    yield
                    P.tt(Xk4, Xk4, v3(b0), ALU.add)
                    yield
                P.tt(rhsu4, Vtok[:, gsl, :], bcj(scg(GS_BETA)), ALU.mult)
                P.tt(rhsw4, Ktok[:, gsl, :], bcj(scg(GS_BEXP)), ALU.mult)
                yield
                for g in range(G):
                    P.mm(b1[:, g * 128:(g + 1) * 128], Xk4[:, g, :], rhsu4[:, g, :])
                for g in range(G):
                    P.mm(b2[:, g * 128:(g + 1) * 128], rhsw4[:, g, :], Xk4[:, g, :])
                yield
                pcopy(k, usb4, v3(b1), "act")
                pcopy(k, wT4, v3(b2), "dve")
                yield
                gorder = range(G) if dr == 0 else range(G - 1, -1, -1)
                for g in gorder:
                    c = cl + g
                    sc = lambda kind: gs[:, kind, c, col:col + 1]
                    P.mm(b0[:, 0:128], wT4[:, g, :], Sbf)
                    yield
                    P.tt(vnew, usb4[:, g, :], b0[:, 0:128], ALU.subtract)
                    yield
                    P.mm(b1[:, 0:128], qgT4[:, g, :], Sbf, start=True, stop=False)
                    P.mm(b1[:, 0:128], attnT4[:, g, :], vnew, start=False, stop=True)
                    P.mm(b2[:, 0:128], kdec4[:, g, :], vnew)
                    yield
                    P.tt(oacc[:, c, :], oacc[:, c, :], b1[:, 0:128], ALU.add)
                    P.stt(Sst, Sst, sc(GS_ETOT), b2[:, 0:128], ALU.mult, ALU.add)
                    yield
                    P.copy(Sbf, Sst, eng="act")
                    yield
            if is_ctx:
                P.copy(B.states[:, col, :], Sst, eng="pool")

        gens = [chain(0), chain(1)]
        while gens:
            for g_ in list(gens):
                try:
                    next(g_)
                except StopIteration:
                    gens.remove(g_)
        if states_only:
            continue
        if h == 0:
            tap(k, "oacc0", oacc)
        P.act(cs[:, 0:T].rearrange("p (a c) -> p a c", c=128), oacc, AF.Square)
        ssv = B.st[:, 16:16 + NT]
        P.reduce(ssv, cs[:, 0:T].rearrange("p (a c) -> p a c", c=128), ALU.add)
        P.ts(ssv, ssv, 1.0 / 128, EPS, ALU.mult, ALU.add)
        P.act(ssv, ssv, AF.Sqrt)
        P.add("dve", lambda e, ssv=ssv: e.reciprocal(out=ssv, in_=ssv), [ssv], [ssv])
        P.tt(oacc, oacc, ssv.unsqueeze(2).broadcast_to([128, NT, 128]), ALU.mult)
        P.tt(oacc, oacc, gnw.unsqueeze(1).broadcast_to([128, NT, 128]), ALU.mult, eng="pool")
        P.tt(yatok, oacc, zs, ALU.mult)
        for i0 in range(0, NT, 8):
            pb = k.pb[(i0 // 8) % 2]
            ni = min(8, NT - i0)
            for i in range(ni):
                P.transpose(pb[:, i * 128:(i + 1) * 128], yatok[:, i0 + i, :], identb)
            pcopy(k, yaT[:, h, i0 * 128:(i0 + ni) * 128], pb[:, 0:ni * 128])
    if is_ctx:
        tap(k, "states", B.states)


def gelu_tanh(k, out, in_, tmp, eng="dve"):
    P = k.P
    P.act(tmp, in_, AF.Square)
    P.ts(tmp, tmp, 0.044715, 1.0, ALU.mult, ALU.add, eng=eng)
    P.tt(tmp, tmp, in_, ALU.mult, eng=eng)
    P.act(tmp, tmp, AF.Sigmoid, scale=1.5957691216057308)
    P.tt(out, tmp, in_, ALU.mult, eng=eng)


def cmlp(k, l, h1T, ybT, T):
    P, B, I = k.P, k.B, k.I
    NT = T // 128
    AF_ = B.AF
    for q in range(2):
        load_w_cols(k, B.wst[q][:], l, OFF_U + q * 256, 256, ("wst", q))
    for q in range(4):
        load_w_cols(k, B.wsm[q][:], l, OFF_VG + q * 128, 128, ("wsm", q))
    sm = B.small
    lnw = sm[:, 512:1024]
    lnb = sm[:, 1024:1536]
    P.dma(lnw, I.lnw_bc[l])
    P.dma(lnb, I.lnb_bc[l])
    bs = sm[:, 176:180]
    P.dma(bs, I.bs_col[l])
    wsT = AF_[:, 8192:8192 + 512].rearrange("p (g c) -> p g c", g=4)
    P.dma(wsT, I.wsT[l].rearrange("g q p -> q g p"))
    ub = AF_[:, 0:512]
    vb = AF_[:, 512:1024]
    t1 = AF_[:, 1024:1536]
    t2 = AF_[:, 1536:2048]
    ybb = B.junk[:, 0:512]
    for i in range(NT):
        pu = k.pf[0]
        pv = k.pf[1]
        for q in range(2):
            for kk in range(KC):
                P.mm(pu[:, q * 256:(q + 1) * 256], h1T[:, kk, i * 128:(i + 1) * 128], B.wst[q][:, kk, :],
                     start=(kk == 0), stop=(kk == KC - 1))
        for q in range(4):
            for kk in range(KC):
                P.mm(pv[:, q * 128:(q + 1) * 128], h1T[:, kk, i * 128:(i + 1) * 128], B.wsm[q][:, kk, :],
                     start=(kk == 0), stop=(kk == KC - 1))
        pcopy(k, ub, pu[:, 0:512], "act")
        pcopy(k, vb, pv[:, 0:512], "dve")
        gelu_tanh(k, ub, ub, t1, eng="dve")
        gelu_tanh(k, vb, vb, t2, eng="dve")
        st = B.st[:, 32:40]
        P.reduce(st[:, 0:1], vb, ALU.add)
        P.ts(st[:, 1:2], st[:, 0:1], -1.0 / 512, None, ALU.mult)
        P.ts(vb, vb, st[:, 1:2], None, ALU.add)
        P.act(t2, vb, AF.Square, accum_out=st[:, 2:3])
        rstd_col(k, st[:, 4:5], st[:, 2:3], 512, st[:, 3:4])
        P.ts(vb, vb, st[:, 4:5], None, ALU.mult)
        P.tt(vb, vb, lnw, ALU.mult, eng="pool")
        P.tt(vb, vb, lnb, ALU.add, eng="pool")
        pm = k.pf[2]
        for g in range(4):
            P.mm(pm[:, g * 128:(g + 1) * 128], wsT[:, g, :], vb[:, g * 128:(g + 1) * 128])
        for g in range(4):
            P.stt(ybb[:, g * 128:(g + 1) * 128], pm[:, g * 128:(g + 1) * 128], bs[:, g:g + 1],
                  ub[:, g * 128:(g + 1) * 128], ALU.add, ALU.mult)
        pb = k.pb[i % 2]
        for g in range(4):
            P.transpose(pb[:, g * 128:(g + 1) * 128], ybb[:, g * 128:(g + 1) * 128], k.cb(C_ID))
        pcopy(k, ybT[:, :, i * 128:(i + 1) * 128], pb[:, 0:512].rearrange("p (g c) -> p g c", g=4))


def pool(k, l, h1T, ycT, T, is_ctx):
    P, B, I = k.P, k.B, k.I
    seg = 256 if is_ctx else 64
    nseg = T // seg
    W = seg + 32
    AF_ = B.AF
    sm = B.small
    psc = sm[:, 180:184]
    P.dma(psc, I.psc_col[l])
    pw = AF_[:, 8192:8192 + 512].rearrange("p (g c) -> p g c", g=4)
    P.dma(pw, I.pool_w[l].rearrange("g c d -> c g d"))
    pwb = B.junk[:, 0:512].rearrange("p (g c) -> p g c", g=4)
    P.copy(pwb, pw, eng="pool")
    rc0 = C_RCC if is_ctx else C_RCX
    rcs = sm[:, 512:512 + 4 * seg]
    P.dma(rcs, I.cst[:, rc0:rc0 + 4 * seg])
    bufs = [AF_[:, i * 3072:(i + 1) * 3072][:, 0:nseg * W].rearrange("p (s w) -> p s w", w=W) for i in range(2)]
    pin = AF_[:, 6144:6144 + T].rearrange("p (s w) -> p s w", w=seg)
    pooled = AF_[:, 8704:8704 + T]
    pooledb = B.AB[:, 49152 - 2048:49152 - 2048 + T]
    for g in range(4):
        def consume(j, t0, n, pap):
            pcopy(k, AF_[:, 6144 + t0:6144 + t0 + n], pap)
        w = B.wsm[g]
        load_w_cols(k, w[:], l, OFF_P + g * 128, 128, ("wsm", g))
        proj_fm(k, h1T, T, w, 128, consume)
        a, bb = bufs
        P.memset(a, 0.0)
        P.memset(bb, 0.0, eng="dve")
        P.copy(a[:, :, 16:16 + seg], pin, eng="pool")
        lo, hi = 8, seg + 24
        P.tt(bb[:, :, lo:hi], a[:, :, lo:hi], a[:, :, lo - 1:hi - 1], ALU.add)
        cur, oth = bb, a
        for lev in range(g):
            sh = 1 << lev
            P.tt(oth[:, :, lo:hi], cur[:, :, lo - sh:hi - sh], cur[:, :, lo + sh:hi + sh], ALU.add, eng="pool")
            cur, oth = oth, cur
        rc = rcs[:, g * seg:(g + 1) * seg]
        pv = pooled.rearrange("p (s w) -> p s w", w=seg)
        P.tt(pv, cur[:, :, 16:16 + seg], rc.unsqueeze(1).broadcast_to([128, nseg, seg]), ALU.mult)
        P.tt(pooledb.rearrange("p (s w) -> p s w", w=seg), pv, pin, ALU.subtract)
        for bi, (t0, n) in enumerate(tokblocks(T)):
            pf = k.pf[4 + bi % 2]
            P.mm(pf[:, 0:n], pwb[:, g, :], pooledb[:, t0:t0 + n])
            P.act(ycT[:, g, t0:t0 + n], pf[:, 0:n], AF.Identity, scale=psc[:, g:g + 1])


def bcast_row(k, out_bc, col8, dg=None):
    P = k.P
    for half in range(2):
        pf = k.pf[half]
        for kk in range(4):
            c = half * 4 + kk
            if dg is None:
                dg = k.B.AF[:, 16384 - 128:16384]
            P.ts(dg, k.c(C_ID), col8[:, c:c + 1], None, ALU.mult)
            P.mm(pf[:, kk * 128:(kk + 1) * 128], k.c(C_ONE), dg)
        pcopy(k, out_bc[:, half * 512:(half + 1) * 512], pf[:, 0:512])


def merge_out(k, b, l, v, src, dst, yaT, ybT, ycT, T):
    P, B, I, S = k.P, k.B, k.I, k.S
    NT = T // 128
    AF_ = B.AF
    mT = B.AB[:, 0:KC * T].rearrange("p (a t) -> p a t", a=KC)
    g1bc = AF_[:, 0:1024]
    bcast_row(k, g1bc, B.mod[:, l, v, 16:24])
    for dc in range(KC):
        wbr = B.wsm[dc % 2]
        wbr2 = B.wsm[2 + dc % 2]
        P.dma(wbr[:], I.w_br_a[l].rearrange("(kk p) c -> p kk c", p=128)[:, :, dc * 128:(dc + 1) * 128],
              eng="pool", semkey=("wsm", dc % 2))
        P.dma(wbr2[:, 0:4, :], I.w_br_b[l].rearrange("(kk p) c -> p kk c", p=128)[:, :, dc * 128:(dc + 1) * 128],
              eng="pool", semkey=("wsm", 2 + dc % 2))
        P.dma(wbr2[:, 4:8, :], I.w_br_c[l].rearrange("(kk p) c -> p kk c", p=128)[:, :, dc * 128:(dc + 1) * 128],
              eng="pool", semkey=("wsm", 2 + dc % 2))
        for bi, (t0, n) in enumerate(tokblocks(T)):
            gts = [AF_[:, 12288 + a * 512:12288 + a * 512 + n] for a in range(3)]
            gtb = [B.junk[:, 0:512], B.junk[:, 512:1024], B.xn[:, 0:512]]
            for a in range(3):
                P.dma(gtb[a][:, 0:n], S.gates[a * 8 + dc, :, t0:t0 + n], semkey=("gld", a))
            acc = AF_[:, 14336:14336 + n]
            for a, (yT, nk_, wsrc, koff) in enumerate(((yaT, 8, wbr, 0), (ybT, 4, wbr2, 0), (ycT, 4, wbr2, 4))):
                pf = k.pf[a]
                for kk in range(nk_):
                    P.mm(pf[:, 0:n], wsrc[:, koff + kk, :], yT[:, kk, t0:t0 + n], start=(kk == 0), stop=(kk == nk_ - 1))
                if a == 0:
                    P.tt(acc, pf[:, 0:n], gtb[a][:, 0:n], ALU.mult)
                else:
                    P.tt(gts[a], pf[:, 0:n], gtb[a][:, 0:n], ALU.mult)
                    P.tt(acc, acc, gts[a], ALU.add)
            P.copy(mT[:, dc, t0:t0 + n], acc, eng="act")
    wv = I.w_out[l].rearrange("(kk p) c -> p kk c", p=128)
    for q in range(4):
        wdst = B.wst[q % 2]
        cs_ = slice(q * 256, (q + 1) * 256)
        for kk in range(KC):
            stg = AF_[:, 2048 + (kk % 2) * 256:2048 + (kk % 2) * 256 + 256]
            P.dma(stg, wv[:, kk, cs_], semkey=("wo", kk % 2))
            P.tt(wdst[:, kk, :], stg, g1bc[:, cs_], ALU.mult)
        for i in range(NT):
            xq = B.xt[i % 2][:, cs_]
            P.dma(xq, src[i * 128:(i + 1) * 128, cs_], semkey=("xt", i % 2))
            pf = k.pf[4 + i % 2]
            for kk in range(KC):
                P.mm(pf[:, 0:256], mT[:, kk, i * 128:(i + 1) * 128], wdst[:, kk, :],
                     start=(kk == 0), stop=(kk == KC - 1))
            P.tt(xq, xq, pf[:, 0:256], ALU.add)
            P.dma(dst[i * 128:(i + 1) * 128, cs_], xq, semkey=("xt", i % 2))


def moe(k, b, l, src, dst, T, is_ctx):
    P, B, I = k.P, k.B, k.I
    NT = T // 128
    cap = 2 * T // NE
    v = 2 if is_ctx else b
    AB, AF_ = B.AB, B.AF
    nsh = (cap + 127) // 128
    sp_ = min(cap, 128)
    xn_tok = AB[:, 0:NT * 1024].rearrange("p (a d) -> p a d", d=1024)
    Sel = AB[:, 16384:16384 + NT * cap].rearrange("p (a s) -> p a s", s=cap)
    SelT = AB[:, 20480:20480 + nsh * T].rearrange("p (a t) -> p a t", a=nsh)
    xeT = AB[:, 24576:24576 + KC * cap].rearrange("p (a s) -> p a s", a=KC)
    hT = AB[:, 26624:26624 + KC * cap].rearrange("p (a s) -> p a s", a=KC)
    ye = AB[:, 28672:28672 + nsh * 1024].rearrange("p (a d) -> p a d", a=nsh)
    wring = [AB[:, 30720 + i * 2048:30720 + (i + 1) * 2048] for i in range(8)]
    ymoe = AF_[:, 0:NT * 1024].rearrange("p (a d) -> p a d", d=1024)
    sm = B.small
    A2 = B.modA[:, l, v, 1, :]
    sh2 = B.mod[:, l, v, 24:32]
    ident, identb = k.c(C_ID), k.cb(C_ID)
    wr = AF_[:, 7168:7296].rearrange("p (a e) -> p a e", a=KC)
    P.dma(wr, I.w_router[l].rearrange("(kk p) e -> p kk e", p=128))
    wrs = AF_[:, 7296:7424].rearrange("p (a e) -> p a e", a=KC)
    P.tt(wrs, wr, A2.unsqueeze(2).broadcast_to([128, KC, NE]), ALU.mult)
    pbias = k.pf[5]
    rep = AF_[:, 7424:7552]
    for kk in range(KC):
        P.ts(rep, k.c(C_ONE), sh2[:, kk:kk + 1], None, ALU.mult)
        P.mm(pbias[:, 0:NE], rep, wr[:, kk, :], start=(kk == 0), stop=(kk == KC - 1))
    rbias = AF_[:, 7552:7568]
    P.copy(rbias, pbias[:, 0:NE])
    afft = sm[:, 1024:1024 + NT * NE].rearrange("p (a e) -> p a e", e=NE)
    maskf = sm[:, 1280:1280 + NT * NE].rearrange("p (a e) -> p a e", e=NE)
    pref = sm[:, 1536:1536 + NT * NE].rearrange("p (a e) -> p a e", e=NE)
    twc = sm[:, 1792:1800]
    xT = AF_[:, 2048:3072].rearrange("p (a c) -> p a c", a=KC)
    xnf = B.xt[0]
    for i in range(NT):
        xt = B.xt[1]
        P.dma(xt[:], src[i * 128:(i + 1) * 128, :], semkey=("xt", 1))
        st = B.st[:, 40:48]
        P.act(B.junk[:], xt[:], AF.Square, accum_out=st[:, 0:1])
        rstd_col(k, st[:, 2:3], st[:, 0:1], D, st[:, 1:2])
        P.ts(xnf[:], xt[:], st[:, 2:3], None, ALU.mult)
        P.copy(xn_tok[:, i, :], xnf[:], eng="act")
        for half in range(2):
            pt = k.pf[half]
            for kk in range(4):
                c = half * 4 + kk
                P.transpose(pt[:, kk * 128:(kk + 1) * 128], xnf[:, c * 128:(c + 1) * 128], ident)
            pcopy(k, xT[:, half * 4:half * 4 + 4, :], pt[:, 0:512].rearrange("p (a c) -> p a c", a=4))
        plog = k.pf[4]
        for kk in range(KC):
            P.mm(plog[:, 0:NE], xT[:, kk, :], wrs[:, kk, :], start=(kk == 0), stop=(kk == KC - 1))
        lg = B.st[:, 48:64]
        P.tt(lg, plog[:, 0:NE], rbias, ALU.add)
        P.reduce(st[:, 3:4], lg, ALU.max)
        P.ts(st[:, 4:5], st[:, 3:4], -1.0, None, ALU.mult)
        P.act(lg, lg, AF.Exp, bias=st[:, 4:5], accum_out=st[:, 5:6])
        P.add("dve", lambda e, st=st: e.reciprocal(out=st[:, 6:7], in_=st[:, 5:6]), [st[:, 5:6]], [st[:, 6:7]])
        P.ts(afft[:, i, :], lg, st[:, 6:7], None, ALU.mult)
    tap(k, "aff", afft)
    affE = AF_[0:NE, 3072:3072 + T]
    wk = AF_[0:NE, 5120:5120 + T]
    for i0 in range(0, NT, 4):
        pt = k.pf[0]
        ni = min(4, NT - i0)
        for i in range(ni):
            P.transpose(pt[0:NE, i * 128:(i + 1) * 128], afft[:, i0 + i, :], ident)
        pcopy(k, affE[:, i0 * 128:(i0 + ni) * 128], pt[0:NE, 0:ni * 128])
    m8 = B.st[0:NE, 8:16]
    for r in range(cap // 8):
        srcv = affE if r == 0 else wk
        P.add("dve", lambda e, srcv=srcv: e.max(out=m8, in_=srcv), [srcv], [m8])
        P.add("dve", lambda e, srcv=srcv: e.match_replace(out=wk, in_to_replace=m8, in_values=srcv, imm_value=-1.0),
              [m8, srcv], [wk])
    maskE = B.wst[0][:].rearrange("p a c -> p (a c)")[0:NE, 0:T]
    P.tt(maskE, affE, wk, ALU.not_equal)
    pm = k.pb[0]
    for i in range(NT):
        P.transpose(pm[:, i * NE:(i + 1) * NE], maskE[:, i * 128:(i + 1) * 128], identb[0:NE, 0:NE])
    maskb = B.junk[:, 0:NT * NE].rearrange("p (a e) -> p a e", e=NE)
    pcopy(k, maskb, pm[:, 0:NT * NE].rearrange("p (a e) -> p a e", e=NE), "act")
    pcopy(k, maskf, pm[:, 0:NT * NE].rearrange("p (a e) -> p a e", e=NE), "dve")
    tap(k, "mask", maskf)
    hl = B.junk[:, 256:256 + NT * NE * 2].rearrange("p (a e h) -> p a e h", e=NE, h=2)
    P.copy(hl[:, :, :, 0], afft)
    hif = B.st[:, 48:64]
    for i in range(NT):
        P.copy(hif, hl[:, i, :, 0], eng="pool")
        P.tt(hl[:, i, :, 1], afft[:, i, :], hif, ALU.subtract, eng="pool")
    for i in range(NT):
        pp = k.pf[1]
        for i2 in range(i + 1):
            P.mm(pp[:, 0:NE], k.cb(C_U) if i2 == i else k.cb(C_ONE), maskb[:, i2, :],
                 start=(i2 == 0), stop=(i2 == i))
        pcopy(k, pref[:, i, :], pp[:, 0:NE], "dve")
    g2bc = None
    for e in range(NE):
        for i in range(NT):
            P.ts(Sel[:, i, :], k.c(C_IOTA, cap), pref[:, i, e:e + 1], maskf[:, i, e:e + 1],
                 ALU.is_equal, ALU.mult)
        for hh in range(nsh):
            for i0 in range(0, NT, 8):
                pb = k.pb[(i0 // 8) % 2]
                ni = min(8, NT - i0)
                for i in range(ni):
                    P.transpose(pb[0:sp_, i * 128:(i + 1) * 128], Sel[:, i0 + i, hh * 128:hh * 128 + sp_], identb)
                pcopy(k, SelT[0:sp_, hh, i0 * 128:(i0 + ni) * 128], pb[0:sp_, 0:ni * 128])
        for kk in range(KC):
            pg = k.pf[kk % 2]
            for i in range(NT):
                P.mm(pg[:, 0:cap], xn_tok[:, i, kk * 128:(kk + 1) * 128], Sel[:, i, :],
                     start=(i == 0), stop=(i == NT - 1))
            P.act(xeT[:, kk, :], pg[:, 0:cap], AF.Identity, scale=A2[:, kk:kk + 1], bias=sh2[:, kk:kk + 1])
        for hh in range(nsh):
            pw_ = k.pf[2]
            for i in range(NT):
                P.mm(pw_[0:sp_, hh * 2:hh * 2 + 2], Sel[:, i, hh * 128:hh * 128 + sp_], hl[:, i, e, :],
                     start=(i == 0), stop=(i == NT - 1))
            P.reduce(twc[0:sp_, hh:hh + 1], pw_[0:sp_, hh * 2:hh * 2 + 2], ALU.add)
        wg_v = I.w_gate[l, e].rearrange("(kk p) f -> p kk f", p=128)
        wu_v = I.w_up[l, e].rearrange("(kk p) f -> p kk f", p=128)
        wd_v = I.w_down[l, e].rearrange("(m p) d -> p m d", p=128)
        for m2 in range(4):
            gp = wring[(m2 % 2) * 2].rearrange("p (a c) -> p a c", a=KC)
            up = wring[(m2 % 2) * 2 + 1].rearrange("p (a c) -> p a c", a=KC)
            P.dma(gp, wg_v[:, :, m2 * 256:(m2 + 1) * 256], eng="pool", semkey=("wr", (m2 % 2) * 2))
            P.dma(up, wu_v[:, :, m2 * 256:(m2 + 1) * 256], eng="pool", semkey=("wr", (m2 % 2) * 2 + 1))
            for mm_ in range(2):
                m = m2 * 2 + mm_
                pg = k.pf[0]
                pu = k.pf[1]
                for kk in range(KC):
                    P.mm(pg[:, 0:cap], gp[:, kk, mm_ * 128:(mm_ + 1) * 128], xeT[:, kk, :],
                         start=(kk == 0), stop=(kk == KC - 1))
                for kk in range(KC):
                    P.mm(pu[:, 0:cap], up[:, kk, mm_ * 128:(mm_ + 1) * 128], xeT[:, kk, :],
                         start=(kk == 0), stop=(kk == KC - 1))
                sg = sm[:, (m % 2) * 256:(m % 2) * 256 + cap]
                P.act(sg, pg[:, 0:cap], AF.Silu)
                P.tt(hT[:, m, :], sg, pu[:, 0:cap], ALU.mult)
        for m2 in range(4):
            dp = wring[4 + m2].rearrange("p (a d) -> p a d", a=2)
            P.dma(dp, wd_v[:, m2 * 2:m2 * 2 + 2, :], eng="pool", semkey=("wr", 4 + m2))
            for mm_ in range(2):
                m = m2 * 2 + mm_
                for hh in range(nsh):
                    for dh in range(2):
                        pd = k.pf[2 + hh * 2 + dh]
                        P.mm(pd[0:sp_, 0:512], hT[:, m, hh * 128:hh * 128 + sp_], dp[:, mm_, dh * 512:(dh + 1) * 512],
                             start=(m == 0), stop=(m == KC - 1))
        for hh in range(nsh):
            for dh in range(2):
                pd = k.pf[2 + hh * 2 + dh]
                P.act(ye[0:sp_, hh, dh * 512:(dh + 1) * 512], pd[0:sp_, 0:512], AF.Identity,
                      scale=twc[0:sp_, hh:hh + 1])
        for i in range(NT):
            for dh in range(2):
                pc = k.pf[(i * 2 + dh) % 2]
                for hh in range(nsh):
                    P.mm(pc[:, 0:512], SelT[0:sp_, hh, i * 128:(i + 1) * 128], ye[0:sp_, hh, dh * 512:(dh + 1) * 512],
                         start=(hh == 0), stop=(hh == nsh - 1))
                dsty = ymoe[:, i, dh * 512:(dh + 1) * 512]
                if e == 0:
                    pcopy(k, dsty, pc[:, 0:512])
                else:
                    P.tt(dsty, dsty, pc[:, 0:512], ALU.add)
    g2bc = sm[:, 0:1024]
    bcast_row(k, g2bc, B.mod[:, l, v, 40:48], dg=sm[:, 1920:2048])
    for i in range(NT):
        xt = B.xt[i % 2]
        P.dma(xt[:], src[i * 128:(i + 1) * 128, :], semkey=("xt", i % 2))
        P.tt(ymoe[:, i, :], ymoe[:, i, :], g2bc, ALU.mult)
        P.tt(xt[:], xt[:], ymoe[:, i, :], ALU.add)
        P.dma(dst[i * 128:(i + 1) * 128, :], xt[:], semkey=("xt", i % 2))


def final_norm(k, b):
    P, B, I, S = k.P, k.B, k.I, k.S
    fw = B.AF[:, 0:1024]
    P.dma(fw, I.fnw_bc)
    for i in range(TX // 128):
        xt = B.xt[i % 2]
        P.dma(xt[:], S.xcur[b][i * 128:(i + 1) * 128, :], semkey=("xt", i % 2))
        st = B.st[:, (i % 2) * 4:(i % 2) * 4 + 4]
        P.act(B.junk[:], xt[:], AF.Square, accum_out=st[:, 0:1])
        rstd_col(k, st[:, 2:3], st[:, 0:1], D, st[:, 1:2])
        P.stt(xt[:], xt[:], st[:, 2:3], fw, ALU.mult, ALU.mult)
        k.fin.append(P.dma(k.out[b][i * 128:(i + 1) * 128, :], xt[:], semkey=("xt", i % 2)))


def dump_dram(k, name, src, T):
    if name not in k.cfg.get("taps", ()):
        return
    o = k.dout("tap_" + name, [T, D])
    for i in range(T // 128):
        xt = k.B.xt[i % 2]
        k.P.dma(xt[:], src[i * 128:(i + 1) * 128, :], semkey=("xt", i % 2))
        k.fin.append(k.P.dma(o[i * 128:(i + 1) * 128, :], xt[:], semkey=("xt", i % 2)))


def col_layout(v, kk):
    return np.ascontiguousarray(np.asarray(v, np.float32).reshape(kk, 128).T)


def bc_layout(v):
    v = np.asarray(v, np.float32).reshape(1, -1)
    return np.ascontiguousarray(np.broadcast_to(v, (128, v.shape[1])))


def shared_inputs(inp):
    f = lambda a: np.ascontiguousarray(np.asarray(a, np.float32))
    sh = {}
    sh["w_ada"] = f(inp["w_ada"])
    sh["b_ada_col"] = np.stack([col_layout(inp["b_ada"][l], 48) for l in range(NL)])
    sh["n1col"] = np.stack([col_layout(inp["norm1_w"][l], KC) for l in range(NL)])
    sh["n2col"] = np.stack([col_layout(inp["norm2_w"][l], KC) for l in range(NL)])
    sh["fnw_bc"] = bc_layout(inp["final_norm_w"])
    sh["w_in"] = f(inp["w_in"])
    cw = np.asarray(inp["qkv_conv_w"], np.float32)
    sh["convw"] = np.ascontiguousarray(cw.reshape(NL, 5, 24, 128).transpose(0, 3, 2, 1))
    sh["alog_bc"] = np.stack([bc_layout(inp["gdn_a_log"][l].reshape(-1)) for l in range(NL)])
    sh["dtb_bc"] = np.stack([bc_layout(inp["gdn_dt_bias"][l].reshape(-1)) for l in range(NL)])
    sh["gnw_bc"] = np.stack([bc_layout(inp["gdn_norm_w"][l]) for l in range(NL)])
    sh["lnw_bc"] = np.stack([bc_layout(inp["cmlp_ln_w"][l]) for l in range(NL)])
    sh["lnb_bc"] = np.stack([bc_layout(inp["cmlp_ln_b"][l]) for l in range(NL)])
    sh["wsT"] = np.ascontiguousarray(np.asarray(inp["cmlp_w_s"], np.float32).transpose(0, 1, 3, 2))
    sh["bs_col"] = np.ascontiguousarray(np.asarray(inp["cmlp_b_s"], np.float32).transpose(0, 2, 1))
    sh["pool_w"] = f(inp["pool_w"])
    sh["psc_col"] = np.stack([col_layout(inp["pool_scale"][l], 4) for l in range(NL)])
    for nm in ("w_br_a", "w_br_b", "w_br_c", "w_out", "w_router", "w_gate", "w_up", "w_down"):
        sh[nm] = f(inp[nm])
    sh["cst"] = make_consts()
    return sh


def core_inputs(inp, sh, core):
    m = dict(sh)
    b0 = 2 * core
    m["x"] = np.ascontiguousarray(np.asarray(inp["x"][b0:b0 + 2], np.float32))
    m["ctx"] = np.ascontiguousarray(np.asarray(inp["ctx"][b0:b0 + 2], np.float32))
    cc = np.stack([np.asarray(inp["c"][b0], np.float32), np.asarray(inp["c"][b0 + 1], np.float32),
                   np.asarray(inp["c_ctx"], np.float32)], axis=-1)
    m["ccol"] = np.ascontiguousarray(cc.reshape(KC, 128, 3).transpose(1, 0, 2))
    return m


_CACHE = {}


def kernel(**inputs):
    n = 8
    if "k" not in _CACHE:
        _CACHE["k"] = build({"batches": [0, 1], "layers": [0, 1], "streams": ["ctx", "x"],
                             "moe": True, "final": True, "taps": []})
    k = _CACHE["k"]
    sh = shared_inputs(inputs)
    in_maps = [core_inputs(inputs, sh, c) for c in range(n)]
    res = run_bass_kernel_spmd(k.nc, in_maps, core_ids=list(range(n)))
    out = np.concatenate([np.asarray(r["out"], np.float32) for r in res.results], axis=0)
    return out
```
